# Optimizing a Trainium2 kernel written in Bass

```python
import math
import jax
import jax.numpy as jnp
from jax import lax
import numpy as np

D_MODEL = 1024
BATCH = 8
SEQ = 8192
DEPTH = 1

CTX_LEN = 256
GRID_W = 64
EPS = 1e-6

HY_WIDTH = D_MODEL
HY_EMB = 33
HY_BANDS = (HY_EMB - 1) // 2
HY_ORDER = 64
HY_FAST_DECAY = 0.3
HY_SLOW_DECAY = 1.5
HY_TARGET = 1e-2

RET_HEADS = 4
RET_DK = D_MODEL // 8
RET_DV = 2 * RET_DK
RET_CHUNK = 128
ROPE_BASE = 10000.0

PEER_HEADS = 8
PEER_N_KEYS = 128
PEER_N_EXPERTS = PEER_N_KEYS * PEER_N_KEYS
PEER_TOPK = 16
PEER_DK = 128
PEER_BLOCK = 128

HY_COLS = 3 * HY_WIDTH
RQ_COLS = RET_HEADS * RET_DK
RV_COLS = RET_HEADS * RET_DV
PROJ_SPLITS = [HY_COLS,
               HY_COLS + RQ_COLS,
               HY_COLS + 2 * RQ_COLS,
               HY_COLS + 2 * RQ_COLS + RV_COLS,
               HY_COLS + 2 * RQ_COLS + 2 * RV_COLS,
               HY_COLS + 2 * RQ_COLS + 2 * RV_COLS + D_MODEL]
PROJ_WIDTH = HY_COLS + 2 * RQ_COLS + 2 * RV_COLS + 2 * D_MODEL

kernel_name = 'hyena_retention_peer_hybrid_dit'


def rms_norm(x, gain):
    xf = x.astype(jnp.float32)
    y = xf * lax.rsqrt(jnp.mean(xf * xf, axis=-1, keepdims=True) + EPS)
    return (y * gain.astype(jnp.float32)).astype(x.dtype)


def modulate(h, shift, scale):
    return h * (1.0 + scale) + shift


def short_conv3(z, w, b):
    zp = jnp.pad(z, ((0, 0), (1, 1), (0, 0)))
    return zp[:, :-2] * w[0] + zp[:, 1:-1] * w[1] + zp[:, 2:] * w[2] + b


def hyena_filter(length, lp):
    f32 = jnp.float32
    t = jnp.linspace(0.0, 1.0, length, dtype=f32)[:, None]
    bands = jnp.linspace(1e-4, HY_BANDS - 1, HY_BANDS, dtype=f32)[None, :]
    w = (2.0 * math.pi / length) * jnp.arange(length, dtype=f32)[:, None]
    z = jnp.concatenate([t, jnp.cos(bands * w), -jnp.sin(bands * w)], axis=-1)
    freq = lp['hy_sin_freq'].astype(f32)
    h = jnp.sin(freq * (z @ lp['hy_fw1'].astype(f32) + lp['hy_fb1'].astype(f32)))
    h = jnp.sin(freq * (h @ lp['hy_fw2'].astype(f32) + lp['hy_fb2'].astype(f32)))
    h = jnp.sin(freq * (h @ lp['hy_fw3'].astype(f32) + lp['hy_fb3'].astype(f32)))
    h = (h @ lp['hy_fw4'].astype(f32)).reshape(length, 2, HY_WIDTH)
    window = jnp.exp(-t * jnp.abs(lp['hy_deltas'].astype(f32)))
    h = h * window[:, None, :]
    kern = jnp.concatenate([h[:, 0], jnp.zeros((1, HY_WIDTH), f32), h[:0:-1, 1]], axis=0)
    return kern / (jnp.sum(jnp.abs(kern), axis=0, keepdims=True) + EPS)


def hyena_long_conv(u, kern, skip):
    length = u.shape[1]
    uf = u.astype(jnp.float32)
    spec = jnp.fft.rfft(uf, n=2 * length, axis=1) * jnp.fft.rfft(kern, n=2 * length, axis=0)[None]
    y = jnp.fft.irfft(spec, n=2 * length, axis=1)[:, :length]
    return (y + uf * skip.astype(jnp.float32)).astype(u.dtype)


def hyena_branch(z, lp):
    length = z.shape[1]
    z = short_conv3(z, lp['hy_conv_w'], lp['hy_conv_b'])
    x0, x1, v = jnp.split(z, 3, axis=-1)
    kern = hyena_filter(length, lp)
    return hyena_long_conv(v * x1, kern, lp['hy_bias']) * x0


def axial_rotary(x, rows, cols):
    half = RET_DK // 2
    nf = half // 2
    inv = ROPE_BASE ** (-jnp.arange(nf, dtype=jnp.float32) / nf)

    def rot(xp, pos):
        ang = pos[:, None] * inv[None, :]
        cos = jnp.cos(ang)[None, :, None, :].astype(x.dtype)
        sin = jnp.sin(ang)[None, :, None, :].astype(x.dtype)
        x1, x2 = xp[..., :nf], xp[..., nf:]
        return jnp.concatenate([x1 * cos - x2 * sin, x1 * sin + x2 * cos], axis=-1)

    return jnp.concatenate([rot(x[..., :half], rows), rot(x[..., half:], cols)], axis=-1)


def retention_scan(q, k, v, log_gamma, state0):
    f32 = jnp.float32
    bsz, length = q.shape[0], q.shape[1]
    n_chunks = length // RET_CHUNK

    def chunks(a):
        return a.astype(f32).reshape(bsz, n_chunks, RET_CHUNK, RET_HEADS, a.shape[-1]).transpose(1, 0, 3, 2, 4)

    lg = log_gamma.astype(f32)[:, None]
    idx = jnp.arange(RET_CHUNK, dtype=f32)
    rel = idx[:, None] - idx[None, :]
    decay_in = jnp.where(rel >= 0, jnp.exp(lg[:, :, None] * jnp.maximum(rel, 0.0)), 0.0)
    xi = jnp.exp(lg * (idx + 1.0))[:, :, None]
    zeta = jnp.exp(lg * (RET_CHUNK - 1.0 - idx))[:, :, None]
    g_chunk = jnp.exp(lg * RET_CHUNK)[:, :, None]

    def step(state, qkv):
        qc, kc, vc = qkv
        scores = jnp.einsum('bhid,bhjd->bhij', qc, kc) * decay_in
        out = (jnp.einsum('bhij,bhjv->bhiv', scores, vc)
               + jnp.einsum('bhid,bhdv->bhiv', qc, state) * xi)
        state = state * g_chunk + jnp.einsum('bhjd,bhjv->bhdv', kc * zeta, vc)
        return state, out

    state, out = lax.scan(step, state0, (chunks(q), chunks(k), chunks(v)))
    out = out.transpose(1, 0, 3, 2, 4).reshape(bsz, length, RET_HEADS, RET_DV)
    return out, state


def bi_retention(q, k, v, lg_f, lg_b, state_f, state_b):
    o_f, s_f = retention_scan(q, k, v, lg_f, state_f)
    o_b, s_b = retention_scan(jnp.flip(q, 1), jnp.flip(k, 1), jnp.flip(v, 1), lg_b, state_b)
    return o_f + jnp.flip(o_b, 1), s_f, s_b


def context_states(k, v, lg_f, lg_b):
    f32 = jnp.float32
    n = k.shape[1]
    pos = jnp.arange(n, dtype=f32)
    w_f = jnp.exp(lg_f.astype(f32)[:, None] * (n - 1.0 - pos))
    w_b = jnp.exp(lg_b.astype(f32)[:, None] * pos)
    kf, vf = k.astype(f32), v.astype(f32)
    s_f = jnp.einsum('blhd,hl,blhv->bhdv', kf, w_f, vf)
    s_b = jnp.einsum('blhd,hl,blhv->bhdv', kf, w_b, vf)
    return s_f, s_b


def retention_readout(o, g):
    bsz, length = o.shape[0], o.shape[1]
    y = o * lax.rsqrt(jnp.mean(o * o, axis=-1, keepdims=True) + EPS)
    gate = jax.nn.silu(g).reshape(bsz, length, RET_HEADS, RET_DV)
    return (y.astype(g.dtype) * gate).reshape(bsz, length, RET_HEADS * RET_DV)


def token_mixer(h_lat, h_ctx, lp, rows, cols, with_ctx_out):
    f32 = jnp.float32
    bsz = h_lat.shape[0]
    hy_l, q_l, k_l, v_l, g_l, ah_l, ar_l = jnp.split(h_lat @ lp['w_in'], PROJ_SPLITS, axis=-1)
    hy_c, q_c, k_c, v_c, g_c, ah_c, ar_c = jnp.split(h_ctx @ lp['w_in'], PROJ_SPLITS, axis=-1)
    scale = RET_DK ** -0.5

    def heads(a, d):
        return a.reshape(a.shape[0], a.shape[1], RET_HEADS, d)

    lg_f, lg_b = lp['ret_log_decay_f'], lp['ret_log_decay_b']
    k_ch = heads(k_c, RET_DK) * scale
    v_ch = heads(v_c, RET_DV)
    if with_ctx_out:
        zero = jnp.zeros((bsz, RET_HEADS, RET_DK, RET_DV), f32)
        o_c, s_f, s_b = bi_retention(heads(q_c, RET_DK), k_ch, v_ch, lg_f, lg_b, zero, zero)
    else:
        s_f, s_b = context_states(k_ch, v_ch, lg_f, lg_b)

    q_lh = axial_rotary(heads(q_l, RET_DK), rows, cols)
    k_lh = axial_rotary(heads(k_l, RET_DK), rows, cols) * scale
    o_l, _, _ = bi_retention(q_lh, k_lh, heads(v_l, RET_DV), lg_f, lg_b, s_f, s_b)

    def merge(hy_proj, o, g, a_h, a_r):
        y_h = hyena_branch(hy_proj, lp) @ lp['w_hy_out']
        y_r = retention_readout(o, g) @ lp['w_ret_out']
        m = jax.nn.sigmoid(a_h) * y_h + jax.nn.sigmoid(a_r) * y_r
        return m @ lp['w_o']

    out_lat = merge(hy_l, o_l, g_l, ah_l, ar_l)
    out_ctx = merge(hy_c, o_c, g_c, ah_c, ar_c) if with_ctx_out else None
    return out_lat, out_ctx


def peer(h, w_query, sub_keys, expert_u, expert_v):
    bsz, length, d = h.shape
    blocks = h.reshape(-1, PEER_BLOCK, d)

    def block_fn(xb):
        q = (xb @ w_query).astype(jnp.float32).reshape(PEER_BLOCK, PEER_HEADS, 2, PEER_DK // 2)
        s = jnp.einsum('phcd,hcnd->phcn', q, sub_keys.astype(jnp.float32))
        s1, i1 = lax.top_k(s[:, :, 0], PEER_TOPK)
        s2, i2 = lax.top_k(s[:, :, 1], PEER_TOPK)
        cand = (s1[..., :, None] + s2[..., None, :]).reshape(PEER_BLOCK, PEER_HEADS, PEER_TOPK * PEER_TOPK)
        cand_idx = (i1[..., :, None] * PEER_N_KEYS + i2[..., None, :]).reshape(PEER_BLOCK, PEER_HEADS, PEER_TOPK * PEER_TOPK)
        top_s, top_pos = lax.top_k(cand, PEER_TOPK)
        eidx = jnp.take_along_axis(cand_idx, top_pos, axis=-1)
        gates = jax.nn.softmax(top_s, axis=-1).astype(xb.dtype)
        u = jnp.take(expert_u, eidx, axis=0)
        act = jax.nn.gelu(jnp.einsum('phkd,pd->phk', u, xb))
        v = jnp.take(expert_v, eidx, axis=0)
        return jnp.einsum('phk,phkd->pd', gates * act, v)

    return lax.map(block_fn, blocks).reshape(bsz, length, d)


def setup_inputs(seed: int = 0) -> dict:
    key = jax.random.key(seed)
    ks = jax.random.split(key, 32)
    f32 = jnp.float32

    def nrm(k, shape, s):
        return jax.random.normal(k, shape, f32) * s

    max_decay = math.log(HY_TARGET) / HY_FAST_DECAY
    min_decay = math.log(HY_TARGET) / HY_SLOW_DECAY
    ret_base = jnp.log1p(-(2.0 ** (-5.0 - jnp.arange(RET_HEADS, dtype=f32))))
    return {
        'x': nrm(ks[0], (BATCH, SEQ, D_MODEL), 1.0),
        'c': nrm(ks[1], (BATCH, D_MODEL), 1.0),
        'ctx': nrm(ks[2], (BATCH, CTX_LEN, D_MODEL), 1.0),
        'c_ctx': nrm(ks[3], (D_MODEL,), 1.0),
        'w_ada': nrm(ks[4], (DEPTH, D_MODEL, 6 * D_MODEL), 0.5 * D_MODEL ** -0.5),
        'b_ada': nrm(ks[5], (DEPTH, 6 * D_MODEL), 0.01),
        'norm1': 1.0 + nrm(ks[6], (DEPTH, D_MODEL), 0.02),
        'norm2': 1.0 + nrm(ks[7], (DEPTH, D_MODEL), 0.02),
        'w_in': nrm(ks[8], (DEPTH, D_MODEL, PROJ_WIDTH), D_MODEL ** -0.5),
        'hy_conv_w': nrm(ks[9], (DEPTH, 3, HY_COLS), 3 ** -0.5),
        'hy_conv_b': nrm(ks[10], (DEPTH, HY_COLS), 0.01),
        'hy_fw1': nrm(ks[11], (DEPTH, HY_EMB, HY_ORDER), HY_EMB ** -0.5),
        'hy_fb1': nrm(ks[12], (DEPTH, HY_ORDER), 0.1),
        'hy_fw2': nrm(ks[13], (DEPTH, HY_ORDER, HY_ORDER), HY_ORDER ** -0.5),
        'hy_fb2': nrm(ks[14], (DEPTH, HY_ORDER), 0.1),
        'hy_fw3': nrm(ks[15], (DEPTH, HY_ORDER, HY_ORDER), HY_ORDER ** -0.5),
        'hy_fb3': nrm(ks[16], (DEPTH, HY_ORDER), 0.1),
        'hy_fw4': nrm(ks[17], (DEPTH, HY_ORDER, 2 * HY_WIDTH), HY_ORDER ** -0.5),
        'hy_sin_freq': 1.0 + nrm(ks[18], (DEPTH, HY_ORDER), 0.02),
        'hy_deltas': jnp.linspace(min_decay, max_decay, HY_WIDTH, dtype=f32)[None, :] * (1.0 + nrm(ks[19], (DEPTH, HY_WIDTH), 0.02)),
        'hy_bias': nrm(ks[20], (DEPTH, HY_WIDTH), 0.5),
        'ret_log_decay_f': ret_base[None, :] * jnp.exp(nrm(ks[21], (DEPTH, RET_HEADS), 0.1)),
        'ret_log_decay_b': ret_base[None, :] * jnp.exp(nrm(ks[22], (DEPTH, RET_HEADS), 0.1)),
        'w_hy_out': nrm(ks[23], (DEPTH, HY_WIDTH, D_MODEL), HY_WIDTH ** -0.5),
        'w_ret_out': nrm(ks[24], (DEPTH, RV_COLS, D_MODEL), RV_COLS ** -0.5),
        'w_o': nrm(ks[25], (DEPTH, D_MODEL, D_MODEL), D_MODEL ** -0.5),
        'peer_w_query': nrm(ks[26], (DEPTH, D_MODEL, PEER_HEADS * PEER_DK), D_MODEL ** -0.5),
        'peer_sub_keys': nrm(ks[27], (DEPTH, PEER_HEADS, 2, PEER_N_KEYS, PEER_DK // 2), (PEER_DK // 2) ** -0.5),
        'peer_u': nrm(ks[28], (DEPTH, PEER_N_EXPERTS, D_MODEL), D_MODEL ** -0.5),
        'peer_v': nrm(ks[29], (DEPTH, PEER_N_EXPERTS, D_MODEL), PEER_HEADS ** -0.5),
        'final_norm': 1.0 + nrm(ks[30], (D_MODEL,), 0.02),
    }


def reference(x, c, ctx, c_ctx, w_ada, b_ada, norm1, norm2, w_in, hy_conv_w, hy_conv_b,
              hy_fw1, hy_fb1, hy_fw2, hy_fb2, hy_fw3, hy_fb3, hy_fw4, hy_sin_freq, hy_deltas, hy_bias,
              ret_log_decay_f, ret_log_decay_b, w_hy_out, w_ret_out, w_o,
              peer_w_query, peer_sub_keys, peer_u, peer_v, final_norm):
    n_rows = x.shape[1] // GRID_W
    rows = jnp.repeat(jnp.arange(n_rows, dtype=jnp.float32), GRID_W)
    cols = jnp.tile(jnp.arange(GRID_W, dtype=jnp.float32), n_rows)
    x_lat, x_ctx = x, ctx
    for layer in range(DEPTH):
        lp = {
            'w_in': w_in[layer], 'hy_conv_w': hy_conv_w[layer], 'hy_conv_b': hy_conv_b[layer],
            'hy_fw1': hy_fw1[layer], 'hy_fb1': hy_fb1[layer], 'hy_fw2': hy_fw2[layer], 'hy_fb2': hy_fb2[layer],
            'hy_fw3': hy_fw3[layer], 'hy_fb3': hy_fb3[layer], 'hy_fw4': hy_fw4[layer],
            'hy_sin_freq': hy_sin_freq[layer], 'hy_deltas': hy_deltas[layer], 'hy_bias': hy_bias[layer],
            'ret_log_decay_f': ret_log_decay_f[layer], 'ret_log_decay_b': ret_log_decay_b[layer],
            'w_hy_out': w_hy_out[layer], 'w_ret_out': w_ret_out[layer], 'w_o': w_o[layer],
        }
        last = layer == DEPTH - 1
        mod_l = (jax.nn.silu(c) @ w_ada[layer] + b_ada[layer])[:, None, :]
        mod_c = (jax.nn.silu(c_ctx) @ w_ada[layer] + b_ada[layer])[None, None, :]
        sh1, sc1, g1, sh2, sc2, g2 = jnp.split(mod_l, 6, axis=-1)
        csh1, csc1, cg1, csh2, csc2, cg2 = jnp.split(mod_c, 6, axis=-1)

        h_lat = modulate(rms_norm(x_lat, norm1[layer]), sh1, sc1)
        h_ctx = modulate(rms_norm(x_ctx, norm1[layer]), csh1, csc1)
        mix_lat, mix_ctx = token_mixer(h_lat, h_ctx, lp, rows, cols, not last)
        x_lat = x_lat + g1 * mix_lat
        h_lat = modulate(rms_norm(x_lat, norm2[layer]), sh2, sc2)
        x_lat = x_lat + g2 * peer(h_lat, peer_w_query[layer], peer_sub_keys[layer], peer_u[layer], peer_v[layer])
        if not last:
            x_ctx = x_ctx + cg1 * mix_ctx
            h_ctx = modulate(rms_norm(x_ctx, norm2[layer]), csh2, csc2)
            x_ctx = x_ctx + cg2 * peer(h_ctx, peer_w_query[layer], peer_sub_keys[layer], peer_u[layer], peer_v[layer])
    return rms_norm(x_lat, final_norm)
```

```python
import math
import numpy as np
import ml_dtypes
from contextlib import ExitStack
import concourse.bass as bass
import concourse.mybir as mybir
from concourse.bass_utils import run_bass_kernel_spmd

F32 = mybir.dt.float32
BF16 = mybir.dt.bfloat16
I32 = mybir.dt.int32
U32 = mybir.dt.uint32
ALU = mybir.AluOpType
AF = mybir.ActivationFunctionType
AX = mybir.AxisListType

D = 1024
L = 8192
NCORES = 8
CTX = 256
EPS = 1e-6
PW = 8192
NT = L // 128
NB = L // 512


class Sched:
    SEM_LIMIT = 30000

    def __init__(self, nc, es, n_dma_sems=40):
        self.nc, self.es = nc, es
        self.engs = {'pe': nc.tensor, 'act': nc.scalar, 'dve': nc.vector,
                     'pool': nc.gpsimd, 'sp': nc.sync}
        self.sems = []
        self.sem_owner = []
        self.cur = {}
        for e in self.engs:
            self._new_sem(e)
        self.dma_ids = []
        for i in range(n_dma_sems):
            self.dma_ids.append(self._alloc(f"dq{i}", 'dma'))
        self.dma_cnt = [0] * n_dma_sems
        self.dma_rr = 0
        self.known = {e: {} for e in self.engs}
        self.lastw = {}
        self.readers = {}
        self.nwaits = 0
        self.nops = 0

    def _alloc(self, name, owner):
        s = self.es.enter_context(self.nc.semaphore(name))
        self.sems.append(s)
        self.sem_owner.append(owner)
        return len(self.sems) - 1

    def _new_sem(self, e):
        sid = self._alloc(f"e_{e}_{len(self.sems)}", e)
        self.cur[e] = [sid, 0]

    def _deps(self, reads, writes):
        deps = []
        for r in reads:
            t = self.lastw.get(r)
            if t is not None:
                deps.append(t)
        for w in writes:
            t = self.lastw.get(w)
            if t is not None:
                deps.append(t)
            deps.extend(self.readers.get(w, ()))
        return deps

    def _wait(self, eng, deps):
        need = {}
        kn = self.known[eng]
        for (s, v) in deps:
            if eng == 'pe' and self.sem_owner[s] == 'pe':
                continue
            if kn.get(s, 0) >= v:
                continue
            if need.get(s, 0) < v:
                need[s] = v
        for s, v in need.items():
            self.engs[eng].wait_ge(self.sems[s], v)
            kn[s] = v
            self.nwaits += 1

    def _commit(self, tok, reads, writes):
        for r in reads:
            self.readers.setdefault(r, []).append(tok)
        for w in writes:
            self.lastw[w] = tok
            self.readers[w] = []

    def op(self, eng, fn, reads=(), writes=()):
        self._wait(eng, self._deps(reads, writes))
        if self.cur[eng][1] >= self.SEM_LIMIT:
            self._new_sem(eng)
        inst = fn(self.engs[eng])
        s, c = self.cur[eng]
        inst.then_inc(self.sems[s], 1)
        c += 1
        self.cur[eng][1] = c
        tok = (s, c)
        self._commit(tok, reads, writes)
        self.nops += 1
        return tok

    def mm(self, fns, reads=(), writes=()):
        self._wait('pe', self._deps(reads, writes))
        if self.cur['pe'][1] >= self.SEM_LIMIT:
            self._new_sem('pe')
        inst = None
        for fn in fns:
            inst = fn(self.engs['pe'])
        s, c = self.cur['pe']
        inst.then_inc(self.sems[s], 1)
        c += 1
        self.cur['pe'][1] = c
        tok = (s, c)
        self._commit(tok, reads, writes)
        self.nops += len(fns)
        return tok

    def _dma_common(self, q, emit, reads, writes):
        deps = self._deps(reads, writes)
        i = self.dma_rr
        self.dma_rr = (i + 1) % len(self.dma_ids)
        s = self.dma_ids[i]
        prev = self.dma_cnt[i]
        if prev > 0:
            deps.append((s, prev))
        self._wait(q, deps)
        inst = emit(self.engs[q])
        inst.then_inc(self.sems[s], 16)
        self.dma_cnt[i] = prev + 16
        tok = (s, prev + 16)
        self._commit(tok, reads, writes)
        self.nops += 1
        return tok

    def dma(self, q, out, in_, reads=(), writes=(), **kw):
        return self._dma_common(q, lambda e: e.dma_start(out=out, in_=in_, **kw), reads, writes)

    def raw_dma(self, q, fn, reads=(), writes=()):
        return self._dma_common(q, fn, reads, writes)

    def barrier(self):
        toks = []
        for e in self.engs:
            s, c = self.cur[e]
            if c > 0:
                toks.append((e, s, c))
        for e in self.engs:
            kn = self.known[e]
            for (o, s, c) in toks:
                if o == e and e == 'pe':
                    continue
                if kn.get(s, 0) < c:
                    self.engs[e].wait_ge(self.sems[s], c)
                    kn[s] = c
            for i, s in enumerate(self.dma_ids):
                v = self.dma_cnt[i]
                if v > 0 and kn.get(s, 0) < v:
                    self.engs[e].wait_ge(self.sems[s], v)
                    kn[s] = v
        self.lastw = {}
        self.readers = {}


def _bf(a):
    return np.ascontiguousarray(a.astype(np.float32)).astype(ml_dtypes.bfloat16)


def make_consts():
    c = {}
    c["ident_bf"] = _bf(np.eye(128))
    c["ident_f"] = np.eye(128, dtype=np.float32)
    a = np.arange(128)
    ang = -2.0 * np.pi * np.outer(a, a) / 128.0
    fre, fim = np.cos(ang), np.sin(ang)
    c["fc1"] = _bf(np.concatenate([fre, fim], 1))
    c["fcj1"] = _bf(np.concatenate([fre, -fim], 1))
    c["fcj2"] = _bf(np.concatenate([fim, fre], 1))
    c["f3"] = _bf(np.stack([fre, fim, -fim, -fre], 0).transpose(1, 0, 2).reshape(128, 512))
    c["fcj1n"] = _bf(np.concatenate([-fre, fim], 1))
    angw = -2.0 * np.pi * np.outer(a, a) / 16384.0
    c["tw"] = np.concatenate([np.cos(angw), np.sin(angw)], 1).astype(np.float32)
    t = np.arange(L)
    rows = (t // 64).astype(np.float32)
    cols = (t % 64).astype(np.float32)
    nf = 32
    inv = (10000.0 ** (-np.arange(nf, dtype=np.float32) / nf)).astype(np.float32)
    cosT = np.zeros((128, L), np.float32)
    sinT = np.zeros((128, L), np.float32)
    for d in range(128):
        pos = rows if d < 64 else cols
        angd = (pos * inv[d % 32]).astype(np.float32)
        cosT[d] = np.cos(angd)
        sinT[d] = np.sin(angd) * (-1.0 if (d % 64) < 32 else 1.0)
    c["rot"] = np.ascontiguousarray(np.stack([cosT, sinT], 1))
    i = np.arange(128, dtype=np.float32)
    jj, ii = np.meshgrid(i, i, indexing="ij")
    ret = np.zeros((128, 6, 128), np.float32)
    ret[:, 0] = np.maximum(ii - jj, 0)
    ret[:, 1] = np.maximum(jj - ii, 0)
    ret[:, 2] = (ii >= jj)
    ret[:, 3] = (jj >= ii)
    ret[:, 4] = (ii + 1.0)
    ret[:, 5] = (128.0 - ii)
    c["rett"] = ret
    colc = np.zeros((128, 8), np.float32)
    colc[:, 0] = 127.0 - i
    colc[:, 1] = i
    colc[:, 2] = 255.0 - i
    colc[:, 3] = 127.0 - i
    colc[:, 4] = i
    colc[:, 5] = 128.0 + i
    colc[:, 6] = 128.0
    c["colc"] = colc
    c["iota16"] = np.tile(np.arange(16, dtype=np.float32)[None, :], (128, 1))
    n = np.arange(2 * L)
    lag = np.where(n < L, n, 2 * L - n).astype(np.int64)
    lag[L] = 0
    tl = np.linspace(0.0, 1.0, L, dtype=np.float32)
    bands = np.linspace(1e-4, 15.0, 16, dtype=np.float32)[None, :]
    w = ((2.0 * math.pi / L) * np.arange(L, dtype=np.float32))[:, None].astype(np.float32)
    zc = np.cos((bands * w).astype(np.float32)).astype(np.float32)
    zs = -np.sin((bands * w).astype(np.float32)).astype(np.float32)
    zf = np.concatenate([zc, zs, tl[:, None]], 1)
    c["zfeat"] = np.ascontiguousarray(zf[lag].T.astype(np.float32))
    lw = tl[lag].astype(np.float32)
    lw[L] = 1e9
    c["lagw"] = lw[None, :].copy()
    return c


CONST_DT = {"ident_bf": BF16, "fc1": BF16, "fcj1": BF16, "fcj2": BF16, "f3": BF16, "fcj1n": BF16}

INPUT_NAMES = ['x', 'c', 'ctx', 'c_ctx', 'w_ada', 'b_ada', 'norm1', 'norm2', 'w_in', 'hy_conv_w', 'hy_conv_b',
               'hy_fw1', 'hy_fb1', 'hy_fw2', 'hy_fb2', 'hy_fw3', 'hy_fb3', 'hy_fw4', 'hy_sin_freq',
               'hy_deltas', 'hy_bias', 'ret_log_decay_f', 'ret_log_decay_b', 'w_hy_out', 'w_ret_out',
               'w_o', 'peer_w_query', 'peer_sub_keys', 'peer_u', 'peer_v', 'final_norm']

PER_CORE_SHAPES = {
    'x': [L, D], 'c': [1, D], 'ctx': [CTX, D], 'c_ctx': [1, D], 'w_ada': [D, 6 * D], 'b_ada': [1, 6 * D],
    'norm1': [1, D], 'norm2': [1, D], 'w_in': [D, PW], 'hy_conv_w': [3, 3 * D], 'hy_conv_b': [1, 3 * D],
    'hy_fw1': [33, 64], 'hy_fb1': [1, 64], 'hy_fw2': [64, 64], 'hy_fb2': [1, 64], 'hy_fw3': [64, 64],
    'hy_fb3': [1, 64], 'hy_fw4': [64, 2 * D], 'hy_sin_freq': [1, 64], 'hy_deltas': [1, D], 'hy_bias': [1, D],
    'ret_log_decay_f': [1, 4], 'ret_log_decay_b': [1, 4], 'w_hy_out': [D, D], 'w_ret_out': [D, D],
    'w_o': [D, D], 'peer_w_query': [D, D], 'peer_sub_keys': [8, 2, 128, 64], 'peer_u': [16384, D],
    'peer_v': [16384, D], 'final_norm': [1, D],
}


def build_nc(consts, stop_after=None, dbg=()):
    nc = bass.Bass("TRN2", target_bir_lowering=False)
    I = {}
    for nme in INPUT_NAMES:
        I[nme] = nc.dram_tensor(nme, PER_CORE_SHAPES[nme], F32, kind="ExternalInput").ap()
    C = {}
    for nme, arr in consts.items():
        C[nme] = nc.dram_tensor("k_" + nme, list(arr.shape), CONST_DT.get(nme, F32), kind="ExternalInput").ap()
    out_ap = nc.dram_tensor("out", [L, D], F32, kind="ExternalOutput").ap()

    def scratch(nme, shape, dt):
        kind = "ExternalOutput" if nme in dbg else "Internal"
        return nc.dram_tensor("s_" + nme, shape, dt, kind=kind).ap()

    zhy = scratch("zhy", [3 * D, L], BF16)
    qT_d = scratch("qT", [512, L], BF16)
    kT_d = scratch("kT", [512, L], BF16)
    v_d = scratch("v", [L, D], BF16)
    gs_d = scratch("gs", [L, D], BF16)
    ahT_d = scratch("ahT", [D, L], BF16)
    arT_d = scratch("arT", [D, L], BF16)
    st0_d = scratch("st0", [128, 2, 4, 256], F32)
    mod_d = scratch("modbc", [7, D], F32)
    ctxmod_d = nc.dram_tensor("s_ctxmod", [2, 128, D], F32).ap()

    with ExitStack() as es:
        S = Sched(nc, es)

        uid = {'n': 0}

        def sb(st, name, shape, dt=F32):
            uid['n'] += 1
            return st.enter_context(nc.sbuf_tensor(f"{name}_{uid['n']}", shape, dt))

        def ps(st, name, shape, dt=F32):
            uid['n'] += 1
            return st.enter_context(nc.psum_tensor(f"{name}_{uid['n']}", shape, dt))

        ident = sb(es, "ident", [128, 128], BF16)
        identf = sb(es, "identf", [128, 128], F32)
        S.dma('sp', ident[:], C["ident_bf"], writes=['ident'])
        S.dma('sp', identf[:], C["ident_f"], writes=['identf'])

        rr = {'i': 0}

        def evac_eng():
            rr['i'] += 1
            return ('act', 'dve')[rr['i'] % 2]

        def copy_op(eng, out, in_, reads, writes, scale=None):
            if eng == 'act':
                if scale is None:
                    return S.op('act', lambda e: e.copy(out, in_), reads, writes)
                return S.op('act', lambda e: e.mul(out, in_, scale), reads, writes)
            if scale is None:
                return S.op(eng, lambda e: e.tensor_copy(out, in_), reads, writes)
            return S.op(eng, lambda e: e.tensor_scalar(out, in_, scale, None, ALU.mult), reads, writes)

        with ExitStack() as p1:
            with ExitStack() as p0:
                A1 = sb(p0, "A1", [128, D]); SH1 = sb(p0, "SH1", [128, D]); G1 = sb(p0, "G1", [128, D])
                A2 = sb(p0, "A2", [128, D]); SH2 = sb(p0, "SH2", [128, D]); G2 = sb(p0, "G2", [128, D])
                FN = sb(p0, "FN", [128, D])
                cc = sb(p0, "cc", [128, 2, 8]); scs = sb(p0, "scs", [128, 2, 8])
                screp = sb(p0, "screp", [128, 2, 8, 128])
                wst = [sb(p0, f"wst{i}", [128, 8, 512]) for i in range(2)]
                brow = sb(p0, "brow", [1, 6 * D]); ones1 = sb(p0, "ones1", [1, 128])
                MODL = sb(p0, "MODL", [128, 6 * D]); MODC = sb(p0, "MODC", [128, 2 * D])
                nrm = sb(p0, "nrm", [128, 3, D])
                CA1 = sb(p0, "CA1", [128, D]); CSH1 = sb(p0, "CSH1", [128, D])
                pm = [ps(p0, f"pm{i}", [128, 512]) for i in range(4)]
                S.dma('sp', cc[:, 0, :], I['c'].rearrange("o (k p) -> p (o k)", p=128), writes=['cc'], allow_slow_non_contiguous=True)
                S.dma('sp', cc[:, 1, :], I['c_ctx'].rearrange("o (k p) -> p (o k)", p=128), writes=['cc'], allow_slow_non_contiguous=True)
                S.dma('sp', brow[:], I['b_ada'], writes=['brow'])
                S.dma('sp', nrm[:, 0, :], I['norm1'].to_broadcast([128, D]), writes=['nrm0'])
                S.dma('sp', nrm[:, 1, :], I['norm2'].to_broadcast([128, D]), writes=['nrm1'])
                S.dma('sp', FN[:], I['final_norm'].to_broadcast([128, D]), writes=['FN'])
                S.op('pool', lambda e: e.memset(ones1[:], 1.0), writes=['ones1'])
                S.op('act', lambda e: e.activation(scs[:], cc[:], AF.Silu), reads=['cc'], writes=['scs'])
                S.op('dve', lambda e: e.tensor_copy(screp[:], scs[:].unsqueeze(3).to_broadcast([128, 2, 8, 128])),
                     reads=['scs'], writes=['screp'])
                for cb in range(12):
                    w = wst[cb % 2]
                    wn = f"wst{cb % 2}"
                    S.dma('sp', w[:], I['w_ada'][:, cb * 512:(cb + 1) * 512].rearrange("(k p) n -> p k n", p=128),
                          writes=[wn])
                    srcs = (0, 1) if cb < 4 else (0,)
                    for si in srcs:
                        pt = pm[(cb * 2 + si) % 4]
                        pn = f"pm{(cb * 2 + si) % 4}"
                        fns = [(lambda e, k=k, si=si, pt=pt, w=w: e.matmul(pt[:], screp[:, si, k, :], w[:, k, :],
                                                                             start=(k == 0), stop=False))
                               for k in range(8)]
                        fns.append(lambda e, pt=pt, cb=cb: e.matmul(pt[:], ones1[:], brow[:, cb * 512:(cb + 1) * 512],
                                                                    start=False, stop=True))
                        S.mm(fns, reads=[wn, 'screp', 'ones1', 'brow'], writes=[pn])
                        dst = MODL[:, cb * 512:(cb + 1) * 512] if si == 0 else MODC[:, cb * 512:(cb + 1) * 512]
                        copy_op(evac_eng(), dst, pt[:], [pn], [('MODL' if si == 0 else 'MODC') + str(cb)])
                modl_all = ['MODL' + str(i) for i in range(12)]
                modc_all = ['MODC' + str(i) for i in range(4)]
                S.op('dve', lambda e: e.scalar_tensor_tensor(A1[:], MODL[:, D:2 * D], 1.0, nrm[:, 0, :], ALU.add, ALU.mult),
                     reads=modl_all + ['nrm0'], writes=['A1'])
                S.op('dve', lambda e: e.scalar_tensor_tensor(A2[:], MODL[:, 4 * D:5 * D], 1.0, nrm[:, 1, :], ALU.add, ALU.mult),
                     reads=modl_all + ['nrm1'], writes=['A2'])
                S.op('dve', lambda e: e.scalar_tensor_tensor(CA1[:], MODC[:, D:2 * D], 1.0, nrm[:, 0, :], ALU.add, ALU.mult),
                     reads=modc_all + ['nrm0'], writes=['CA1'])
                S.op('act', lambda e: e.copy(SH1[:], MODL[:, 0:D]), reads=modl_all, writes=['SH1'])
                S.op('act', lambda e: e.copy(G1[:], MODL[:, 2 * D:3 * D]), reads=modl_all, writes=['G1'])
                S.op('act', lambda e: e.copy(SH2[:], MODL[:, 3 * D:4 * D]), reads=modl_all, writes=['SH2'])
                S.op('act', lambda e: e.copy(G2[:], MODL[:, 5 * D:6 * D]), reads=modl_all, writes=['G2'])
                S.op('act', lambda e: e.copy(CSH1[:], MODC[:, 0:D]), reads=modc_all, writes=['CSH1'])
                if True:
                    for i, (t, nme) in enumerate([(A1, 'A1'), (SH1, 'SH1'), (G1, 'G1'), (A2, 'A2'), (SH2, 'SH2'), (G2, 'G2'), (FN, 'FN')]):
                        S.dma('sp', mod_d[i:i + 1, :], t[0:1, :], reads=[nme], writes=['mod_d'])
                S.dma('sp', ctxmod_d[0], CA1[:], reads=['CA1'], writes=['ctxmod'])
                S.dma('sp', ctxmod_d[1], CSH1[:], reads=['CSH1'], writes=['ctxmod'])
                S.barrier()
            W1 = sb(p1, "W1", [128, 8, 9216], BF16)
            with ExitStack() as p0:
                stg = [sb(p0, f"stg{i}", [128, 2048]) for i in range(3)]
                kscale = 128.0 ** -0.5
                n = 0
                for k in range(8):
                    for cb in range(4):
                        st = stg[n % 3]
                        sn = f"stg{n % 3}"
                        S.dma('sp', st[:], I['w_in'][k * 128:(k + 1) * 128, cb * 2048:(cb + 1) * 2048], writes=[sn])
                        eng = ('act', 'dve', 'pool')[n % 3]
                        if cb == 1:
                            copy_op(eng, W1[:, k, 2048:3584], st[:, 0:1536], [sn], [f"W1_{k}_{cb}a"])
                            copy_op(eng, W1[:, k, 3584:4096], st[:, 1536:2048], [sn], [f"W1_{k}_{cb}b"], scale=kscale)
                        else:
                            copy_op(eng, W1[:, k, cb * 2048:(cb + 1) * 2048], st[:], [sn], [f"W1_{k}_{cb}"])
                        n += 1
                S.barrier()
                srcv = W1[:, :, 3072:4096].rearrange("p k (b s i) -> p k b s i", s=2, i=32)
                dstv = W1[:, :, 8192:9216].rearrange("p k (b s i) -> p k b s i", s=2, i=32)
                for k in range(8):
                    S.op('dve', lambda e, k=k: e.tensor_copy(dstv[:, k, :, 0, :], srcv[:, k, :, 1, :]), writes=[f'W1sw{k}a'])
                    S.op('pool', lambda e, k=k: e.tensor_copy(dstv[:, k, :, 1, :], srcv[:, k, :, 0, :]), writes=[f'W1sw{k}b'])
                S.barrier()

            xin = [sb(p1, f"xin{i}", [128, D]) for i in range(2)]
            junk = sb(p1, "junk", [128, D], BF16)
            htmp = sb(p1, "htmp", [128, D])
            hb = [sb(p1, f"hb{i}", [128, D], BF16) for i in range(2)]
            hT = [sb(p1, f"hT{i}", [128, 8, 512], BF16) for i in range(2)]
            stat = sb(p1, "stat", [128, 8])
            A1 = sb(p1, "A1", [128, D]); SH1 = sb(p1, "SH1", [128, D])
            S.dma('sp', A1[:], ctxmod_d[0], writes=['A1'])
            S.dma('sp', SH1[:], ctxmod_d[1], writes=['SH1'])
            pF = [ps(p1, f"pF{i}", [128, 512]) for i in range(6)]
            pT = [ps(p1, f"pT{i}", [128, 8, 128], BF16) for i in range(2)]
            cnt = {'x': 0, 'pF': 0, 'ob': 0, 'pT': 0, 'rt': 0}

            def norm_T(src_rows, Abc, SHbc, an, shn, hTt, hTn, col):
                if isinstance(src_rows, tuple):
                    xi = src_rows[0]
                    xt, xn = xin[xi], f"xin{xi}"
                else:
                    xi = cnt['x'] % 2
                    cnt['x'] += 1
                    xt, xn = xin[xi], f"xin{xi}"
                    S.dma('sp', xt[:], src_rows, writes=[xn])
                S.op('act', lambda e: e.activation(junk[:], xt[:], AF.Square, accum_out=stat[:, 0:1]),
                     reads=[xn], writes=['junk', 'stat0'])
                S.op('dve', lambda e: e.tensor_scalar(stat[:, 1:2], stat[:, 0:1], 1.0 / D, EPS, ALU.mult, ALU.add),
                     reads=['stat0'], writes=['stat1'])
                S.op('act', lambda e: e.sqrt(stat[:, 2:3], stat[:, 1:2]), reads=['stat1'], writes=['stat2'])
                S.op('dve', lambda e: e.reciprocal(stat[:, 3:4], stat[:, 2:3]), reads=['stat2'], writes=['stat3'])
                S.op('dve', lambda e: e.scalar_tensor_tensor(htmp[:], xt[:], stat[:, 3:4], Abc[:], ALU.mult, ALU.mult),
                     reads=[xn, 'stat3', an], writes=['htmp'])
                hbt, hbn = hb[xi], f"hb{xi}"
                S.op('pool', lambda e: e.tensor_tensor(hbt[:], htmp[:], SHbc[:], ALU.add),
                     reads=['htmp', shn], writes=[hbn])
                pi = cnt['pT'] % 2
                cnt['pT'] += 1
                S.mm([(lambda e, k=k: e.transpose(pT[pi][:, k, :], hbt[:, k * 128:(k + 1) * 128], ident[:]))
                      for k in range(8)], reads=[hbn, 'ident'], writes=[f"pT{pi}"])
                copy_op(evac_eng(), hTt[:, :, col:col + 128], pT[pi][:], [f"pT{pi}"], [hTn + f"_{col}"])

            def next_pF():
                i = cnt['pF'] % 6
                cnt['pF'] += 1
                return pF[i], f"pF{i}"

            def next_ob():
                i = cnt['ob'] % NOB
                cnt['ob'] += 1
                return ob[i], f"ob{i}"

            def proj_fm(hTt, hTreads, c0, pt, pn, ntok=512):
                S.mm([(lambda e, k=k: e.matmul(pt[:, 0:ntok], W1[:, k, c0:c0 + 128], hTt[:, k, 0:ntok],
                                               start=(k == 0), stop=(k == 7))) for k in range(8)],
                     reads=hTreads, writes=[pn])

            def proj_tm(hTt, hTreads, tcol, c0, pt, pn):
                S.mm([(lambda e, k=k: e.matmul(pt[:], hTt[:, k, tcol:tcol + 128], W1[:, k, c0:c0 + 512],
                                               start=(k == 0), stop=(k == 7))) for k in range(8)],
                     reads=hTreads, writes=[pn])

            with ExitStack() as pc:
                kc = sb(pc, "kc", [128, 2, 512], BF16)
                vcs = sb(pc, "vcs", [128, 2, 2, D], BF16)
                lgb = sb(pc, "lgb", [128, 2, 4])
                colc = sb(pc, "colc", [128, 8])
                wfb = sb(pc, "wfb", [128, 2, 2, 4])
                st0 = sb(pc, "st0", [128, 2, 4, 256])
                S.dma('sp', lgb[:, 0, :], I['ret_log_decay_f'].to_broadcast([128, 4]), writes=['lgb'])
                S.dma('sp', lgb[:, 1, :], I['ret_log_decay_b'].to_broadcast([128, 4]), writes=['lgb'])
                S.dma('sp', colc[:], C['colc'], writes=['colc'])
                for ti in range(2):
                    for di in range(2):
                        cidx = 2 + di * 2 + ti
                        S.op('act', lambda e, ti=ti, di=di, cidx=cidx: e.activation(
                            wfb[:, ti, di, :], lgb[:, di, :], AF.Exp, scale=colc[:, cidx:cidx + 1]),
                            reads=['lgb', 'colc'], writes=[f'wfb{ti}{di}'])
                hTc = hT[0]
                for ti in range(2):
                    norm_T(I['ctx'][ti * 128:(ti + 1) * 128, :], A1, SH1, 'A1', 'SH1', hTc, 'hTc', ti * 128)
                hreads = ['hTc_0', 'hTc_128']
                for ti in range(2):
                    pt, pn = next_pF()
                    proj_tm(hTc, hreads, ti * 128, 3584, pt, pn)
                    copy_op(evac_eng(), kc[:, ti, :], pt[:], [pn], [f'kc{ti}'])
                    for hf in range(2):
                        pt, pn = next_pF()
                        proj_tm(hTc, hreads, ti * 128, 4096 + hf * 512, pt, pn)
                        for di in range(2):
                            for hh in range(2):
                                h = hf * 2 + hh
                                S.op('dve', lambda e, ti=ti, di=di, h=h, hh=hh, pt=pt: e.tensor_scalar(
                                    vcs[:, ti, di, h * 256:(h + 1) * 256], pt[:, hh * 256:(hh + 1) * 256],
                                    wfb[:, ti, di, h:h + 1], None, ALU.mult),
                                    reads=[pn, f'wfb{ti}{di}'], writes=[f'vcs{ti}{di}{h}'])
                for di in range(2):
                    for h in range(4):
                        pt, pn = next_pF()
                        S.mm([(lambda e, ti=ti, pt=pt: e.matmul(pt[:, 0:256], kc[:, ti, h * 128:(h + 1) * 128],
                                                                 vcs[:, ti, di, h * 256:(h + 1) * 256],
                                                                 start=(ti == 0), stop=(ti == 1))) for ti in range(2)],
                             reads=['kc0', 'kc1'] + [f'vcs{ti}{di}{h}' for ti in range(2)], writes=[pn])
                        copy_op(evac_eng(), st0[:, di, h, :], pt[:, 0:256], [pn], [f'st0_{di}{h}'])
                S.dma('sp', st0_d, st0[:], reads=[f'st0_{di}{h}' for di in range(2) for h in range(4)])
                S.barrier()

            if stop_after == 'ctx':
                S.barrier()
                return nc

            S.dma('sp', A1[:], mod_d[0:1, :].to_broadcast([128, D]), writes=['A1'])
            S.dma('sp', SH1[:], mod_d[1:2, :].to_broadcast([128, D]), writes=['SH1'])
            rot = [sb(p1, f"rot{i}", [128, 2, 512]) for i in range(2)]
            NOB = 4
            ob = [sb(p1, f"ob{i}", [128, 512], BF16) for i in range(NOB)]
            rt = [sb(p1, f"rt{i}", [128, 512]) for i in range(2)]
            nblocks = NB if stop_after != 'p1small' else 2
            def xload(b, ti):
                xi = cnt['x'] % 2
                cnt['x'] += 1
                r0 = b * 512 + ti * 128
                S.dma('sp', xin[xi][:], I['x'][r0:r0 + 128, :], writes=[f"xin{xi}"])
                return xi

            def norm_steps(b):
                st = {}

                def step(k):
                    if k == 0:
                        st[0] = xload(b, 0)
                    if k + 1 < 4:
                        st[k + 1] = xload(b, k + 1)
                    norm_T((st[k],), A1, SH1, 'A1', 'SH1', hT[b % 2], f"hT{b % 2}", k * 128)
                return [lambda k=k: step(k) for k in range(4)]

            S.dma('sp', rot[0][:], C['rot'][:, :, 0:512], writes=["rot0"])
            for th in norm_steps(0):
                th()
            for b in range(nblocks):
                hTt, hTn = hT[b % 2], f"hT{b % 2}"
                rtab, rn = rot[b % 2], f"rot{b % 2}"
                nsteps = norm_steps(b + 1) if b + 1 < nblocks else []
                if b + 1 < nblocks:
                    S.dma('sp', rot[(b + 1) % 2][:], C['rot'][:, :, (b + 1) * 512:(b + 2) * 512], writes=[f"rot{(b + 1) % 2}"])
                hreads = [hTn + f"_{c}" for c in (0, 128, 256, 384)]
                tsl = slice(b * 512, (b + 1) * 512)
                for cc_ in range(24):
                    pt, pn = next_pF()
                    proj_fm(hTt, hreads, cc_ * 128, pt, pn)
                    o, on = next_ob()
                    copy_op(evac_eng(), o[:], pt[:], [pn], [on])
                    S.dma('sp', zhy[cc_ * 128:(cc_ + 1) * 128, tsl], o[:], reads=[on])
                    if cc_ % 6 == 5 and nsteps:
                        nsteps[cc_ // 6]()
                for qk in range(2):
                    for h in range(4):
                        c0 = 3072 + qk * 512 + h * 128
                        c1 = 8192 + qk * 512 + h * 128
                        pa, pan = next_pF()
                        proj_fm(hTt, hreads, c0, pa, pan)
                        pb, pbn = next_pF()
                        proj_fm(hTt, hreads, c1, pb, pbn)
                        ta, tan = rt[0], "rt0"
                        tb, tbn = rt[1], "rt1"
                        S.op('dve', lambda e, ta=ta, pa=pa: e.tensor_tensor(ta[:], pa[:], rtab[:, 0, :], ALU.mult),
                             reads=[pan, rn], writes=[tan])
                        S.op('dve', lambda e, tb=tb, pb=pb: e.tensor_tensor(tb[:], pb[:], rtab[:, 1, :], ALU.mult),
                             reads=[pbn, rn], writes=[tbn])
                        o, on = next_ob()
                        S.op('dve', lambda e, o=o, ta=ta, tb=tb: e.tensor_tensor(o[:], ta[:], tb[:], ALU.add),
                             reads=[tan, tbn], writes=[on])
                        dst = (qT_d if qk == 0 else kT_d)[h * 128:(h + 1) * 128, tsl]
                        S.dma('sp', dst, o[:], reads=[on])
                for gi in range(16):
                    pt, pn = next_pF()
                    proj_fm(hTt, hreads, 6144 + gi * 128, pt, pn)
                    o, on = next_ob()
                    S.op('act', lambda e, o=o, pt=pt: e.activation(o[:], pt[:], AF.Sigmoid), reads=[pn], writes=[on])
                    dst = (ahT_d if gi < 8 else arT_d)[(gi % 8) * 128:(gi % 8 + 1) * 128, tsl]
                    S.dma('sp', dst, o[:], reads=[on])
                for ti in range(4):
                    r0 = b * 512 + ti * 128
                    for hf in range(2):
                        pt, pn = next_pF()
                        proj_tm(hTt, hreads, ti * 128, 4096 + hf * 512, pt, pn)
                        o, on = next_ob()
                        copy_op(evac_eng(), o[:], pt[:], [pn], [on])
                        S.dma('sp', v_d[r0:r0 + 128, hf * 512:(hf + 1) * 512], o[:], reads=[on])
                    for hf in range(2):
                        pt, pn = next_pF()
                        proj_tm(hTt, hreads, ti * 128, 5120 + hf * 512, pt, pn)
                        o, on = next_ob()
                        S.op('act', lambda e, o=o, pt=pt: e.activation(o[:], pt[:], AF.Silu), reads=[pn], writes=[on])
                        S.dma('sp', gs_d[r0:r0 + 128, hf * 512:(hf + 1) * 512], o[:], reads=[on])
            S.barrier()
        if stop_after in ('p1', 'p1small'):
            return nc

        def run_interleaved(chains, K):
            it = iter(chains)
            active = []
            done = False
            while True:
                while not done and len(active) < K:
                    try:
                        active.append([next(it), 0])
                    except StopIteration:
                        done = True
                if not active:
                    break
                for a in list(active):
                    a[0][a[1]]()
                    a[1] += 1
                    if a[1] >= len(a[0]):
                        active.remove(a)

        sb_d = scratch("sbst", [64, 128, D], BF16)
        uvb_d = scratch("uvb", [16384, 2 * D], BF16)
        yrT_d = scratch("yrT", [D, L], BF16)
        with ExitStack() as p2:
            rett = sb(p2, "rett", [128, 6, 128]); colc = sb(p2, "colc", [128, 8]); lgb = sb(p2, "lgb", [128, 2, 4])
            MT = sb(p2, "MT", [128, 4, 128]); XIF = sb(p2, "XIF", [128, 4, 128]); XIB = sb(p2, "XIB", [128, 4, 128])
            mtmp = sb(p2, "mtmp", [128, 2, 128])
            zeta = sb(p2, "zeta", [128, 2, 4]); gch = sb(p2, "gch", [128, 2, 4])
            St = sb(p2, "St", [128, 2, 4, 256])
            Sfb = sb(p2, "Sfb", [128, 4, 256], BF16)
            qTb = [sb(p2, f"qTb{i}", [128, 4, 512], BF16) for i in range(2)]
            kTb = [sb(p2, f"kTb{i}", [128, 4, 512], BF16) for i in range(2)]
            vb = [sb(p2, f"vb{i}", [128, 4, D], BF16) for i in range(2)]
            gsb = [sb(p2, f"gsb{i}", [128, 4, D], BF16) for i in range(2)]
            sbl = [sb(p2, f"sbl{i}", [128, 4, D], BF16) for i in range(2)]
            yst = [sb(p2, f"yst{i}", [128, 8, 512], BF16) for i in range(2)]
            ktok = [sb(p2, f"ktok{i}", [128, 128], BF16) for i in range(4)]
            vz = [sb(p2, f"vz{i}", [128, 256], BF16) for i in range(4)]
            PTt = [sb(p2, f"PT{i}", [128, 128], BF16) for i in range(4)]
            qfb = [sb(p2, f"qfb{i}", [128, 2, 128], BF16) for i in range(4)]
            yr = [sb(p2, f"yr{i}", [128, 256], BF16) for i in range(4)]
            junk2 = [sb(p2, f"junk2{i}", [128, 256], BF16) for i in range(4)]
            rst = [sb(p2, f"rst{i}", [128, 4]) for i in range(4)]
            cst = [sb(p2, f"cst{i}", [128, 4, D]) for i in range(2)]
            cbf = [sb(p2, f"cbf{i}", [128, 4, D], BF16) for i in range(2)]
            bkA = [ps(p2, f"bkA{i}", [128, 512]) for i in range(4)]
            bkB = [ps(p2, f"bkB{i}", [128, 512]) for i in range(4)]

            class _Slots:
                def __init__(self, f):
                    self.f = f

                def __getitem__(self, key):
                    return self.f(key[1])[(key[0],) + tuple(key[2:])]
            pK = _Slots(lambda i: bkA[i][:, 0:64].bitcast(BF16))
            pS = _Slots(lambda i: bkA[i][:, 64:192])
            pY = _Slots(lambda i: bkA[i][:, 192:320].bitcast(BF16).rearrange("p (a b) -> p a b", a=2))
            pO = _Slots(lambda i: bkB[i][:, 0:256])
            pD = _Slots(lambda i: bkB[i][:, 256:512])
            S.dma('sp', rett[:], C['rett'], writes=['rett'])
            S.dma('sp', colc[:], C['colc'], writes=['colc'])
            S.dma('sp', lgb[:, 0, :], I['ret_log_decay_f'].to_broadcast([128, 4]), writes=['lgb'])
            S.dma('sp', lgb[:, 1, :], I['ret_log_decay_b'].to_broadcast([128, 4]), writes=['lgb'])
            S.dma('sp', St[:], st0_d, writes=['St0', 'St1'])
            for h in range(4):
                S.op('act', lambda e, h=h: e.activation(mtmp[:, 0, :], rett[:, 0, :], AF.Exp, scale=lgb[:, 0, h:h + 1]),
                     reads=['rett', 'lgb'], writes=['mtmp0'])
                S.op('act', lambda e, h=h: e.activation(mtmp[:, 1, :], rett[:, 1, :], AF.Exp, scale=lgb[:, 1, h:h + 1]),
                     reads=['rett', 'lgb'], writes=['mtmp1'])
                S.op('dve', lambda e, h=h: e.tensor_tensor(mtmp[:, 0, :], mtmp[:, 0, :], rett[:, 2, :], ALU.mult),
                     reads=['mtmp0', 'rett'], writes=['mtmp0'])
                S.op('dve', lambda e, h=h: e.tensor_tensor(mtmp[:, 1, :], mtmp[:, 1, :], rett[:, 3, :], ALU.mult),
                     reads=['mtmp1', 'rett'], writes=['mtmp1'])
                S.op('dve', lambda e, h=h: e.tensor_tensor(MT[:, h, :], mtmp[:, 0, :], mtmp[:, 1, :], ALU.add),
                     reads=['mtmp0', 'mtmp1'], writes=['MT'])
                S.op('act', lambda e, h=h: e.activation(XIF[:, h, :], rett[:, 4, :], AF.Exp, scale=lgb[:, 0, h:h + 1]),
                     reads=['rett', 'lgb'], writes=['XIF'])
                S.op('act', lambda e, h=h: e.activation(XIB[:, h, :], rett[:, 5, :], AF.Exp, scale=lgb[:, 1, h:h + 1]),
                     reads=['rett', 'lgb'], writes=['XIB'])
            for di in range(2):
                S.op('act', lambda e, di=di: e.activation(zeta[:, di, :], lgb[:, di, :], AF.Exp, scale=colc[:, di:di + 1]),
                     reads=['lgb', 'colc'], writes=['zeta'])
                S.op('act', lambda e, di=di: e.activation(gch[:, di, :], lgb[:, di, :], AF.Exp, scale=128.0),
                     reads=['lgb'], writes=['gch'])
            S.barrier()
            nblk = NB if stop_after != 'retsmall' else 2
            kTv = kT_d.rearrange("(h d) t -> d h t", d=128)
            qTv = qT_d.rearrange("(h d) t -> d h t", d=128)

            def loadA(blk, bi):
                tsl = slice(blk * 512, (blk + 1) * 512)
                S.dma('sp', kTb[bi][:], kTv[:, :, tsl], writes=[f'kTb{bi}'])
                S.dma('sp', vb[bi][:], v_d[tsl, :].rearrange("(c j) f -> j c f", j=128), writes=[f'vb{bi}'])

            def loadB(blk, bi):
                tsl = slice(blk * 512, (blk + 1) * 512)
                S.dma('sp', kTb[bi][:], kTv[:, :, tsl], writes=[f'kTb{bi}'])
                S.dma('sp', qTb[bi][:], qTv[:, :, tsl], writes=[f'qTb{bi}'])
                S.dma('sp', vb[bi][:], v_d[tsl, :].rearrange("(c j) f -> j c f", j=128), writes=[f'vb{bi}'])
                S.dma('sp', gsb[bi][:], gs_d[tsl, :].rearrange("(c j) f -> j c f", j=128), writes=[f'gsb{bi}'])
                S.dma('sp', sbl[bi][:], sb_d[blk * 4:(blk + 1) * 4].rearrange("n d f -> d n f"), writes=[f'sbl{bi}'])

            def k_tok(A, s_, kt, ktn, h, c):
                A(lambda: S.mm([lambda e: e.transpose(pK[:, s_, :], kt[:, h, c * 128:(c + 1) * 128], ident[:])],
                               reads=[ktn, 'ident'], writes=[f'pK{s_}', f'bkA{s_}']))
                A(lambda: copy_op('act', ktok[s_][:], pK[:, s_, :], [f'pK{s_}'], [f'ktok{s_}', f'bkA{s_}']))

            def state_update(A, s_, di, h, vt, vtn, c):
                A(lambda: S.op('dve', lambda e: e.tensor_scalar(vz[s_][:], vt[:, c, h * 256:(h + 1) * 256], zeta[:, di, h:h + 1], None, ALU.mult),
                               reads=[vtn], writes=[f'vz{s_}']))
                A(lambda: S.mm([lambda e: e.matmul(pD[:, s_, :], ktok[s_][:], vz[s_][:], start=True, stop=True)],
                               reads=[f'ktok{s_}', f'vz{s_}'], writes=[f'pD{s_}', f'bkB{s_}']))
                A(lambda: S.op('dve', lambda e: e.scalar_tensor_tensor(St[:, di, h, :], St[:, di, h, :], gch[:, di, h:h + 1], pD[:, s_, :],
                                                                       ALU.mult, ALU.add),
                               reads=[f'pD{s_}', f'St{di}{h}'], writes=[f'St{di}{h}', f'bkB{s_}']))

            bdone = {}

            def chainsA():
                for bi_, blk in enumerate(range(nblk - 1, -1, -1)):
                    bi = bi_ % 2
                    kt, ktn = kTb[bi], f'kTb{bi}'
                    vt, vtn = vb[bi], f'vb{bi}'
                    sst, sstn = sbl[bi], f'sbl{bi}'
                    for c in range(3, -1, -1):
                        for h in range(4):
                            ops = []
                            A = ops.append
                            A(lambda sst=sst, sstn=sstn, c=c, h=h: copy_op('act', sst[:, c, h * 256:(h + 1) * 256], St[:, 1, h, :], [f'St1{h}'], [sstn + f'_{c}{h}']))
                            k_tok(A, h, kt, ktn, h, c)
                            state_update(A, h, 1, h, vt, vtn, c)

                            def fin(bi_=bi_, blk=blk, sst=sst, sstn=sstn):
                                bdone[bi_] = bdone.get(bi_, 0) + 1
                                if bdone[bi_] == 16:
                                    S.dma('sp', sb_d[blk * 4:(blk + 1) * 4].rearrange("n d f -> d n f"), sst[:],
                                          reads=[sstn + f'_{c2}{h2}' for c2 in range(4) for h2 in range(4)], writes=['sb_d'])
                                    if bi_ + 2 < nblk:
                                        loadA(nblk - 1 - (bi_ + 2), bi_ % 2)
                            A(fin)
                            yield ops

            loadA(nblk - 1, 0)
            if nblk > 1:
                loadA(nblk - 2, 1)
            import os as _os2
            KR = int(_os2.environ.get('RET_K', 4))
            run_interleaved(chainsA(), KR)
            S.barrier()
            if stop_after == 'retA':
                return nc
            for h in range(4):
                copy_op('act', Sfb[:, h, :], St[:, 0, h, :], [], [f'Sfb{h}'])
            bdone = {}

            def chainsB():
                for blk in range(nblk):
                    bb = blk % 2
                    kt, ktn = kTb[bb], f'kTb{bb}'
                    qt, qtn = qTb[bb], f'qTb{bb}'
                    vt, vtn = vb[bb], f'vb{bb}'
                    gt, gtn = gsb[bb], f'gsb{bb}'
                    sst, sstn = sbl[bb], f'sbl{bb}'
                    ys, ysn = yst[bb], f'yst{bb}'
                    for c in range(4):
                        csl = slice(c * 128, (c + 1) * 128)
                        for h in range(4):
                            ops = []
                            A = ops.append
                            s_ = h
                            k_tok(A, s_, kt, ktn, h, c)
                            A(lambda kt=kt, qt=qt, ktn=ktn, qtn=qtn, h=h, csl=csl, s_=s_: S.mm(
                                [lambda e: e.matmul(pS[:, s_, :], kt[:, h, csl], qt[:, h, csl], start=True, stop=True)],
                                reads=[ktn, qtn], writes=[f'pS{s_}', f'bkA{s_}']))
                            A(lambda h=h, s_=s_: S.op('dve', lambda e: e.tensor_tensor(PTt[s_][:], pS[:, s_, :], MT[:, h, :], ALU.mult),
                                                      reads=[f'pS{s_}'], writes=[f'PT{s_}', f'bkA{s_}']))
                            A(lambda qt=qt, qtn=qtn, h=h, csl=csl, s_=s_: S.op('pool', lambda e: e.tensor_tensor(qfb[s_][:, 0, :], qt[:, h, csl], XIF[:, h, :], ALU.mult),
                                                                               reads=[qtn], writes=[f'qf{s_}']))
                            A(lambda qt=qt, qtn=qtn, h=h, csl=csl, s_=s_: S.op('pool', lambda e: e.tensor_tensor(qfb[s_][:, 1, :], qt[:, h, csl], XIB[:, h, :], ALU.mult),
                                                                               reads=[qtn], writes=[f'qb{s_}']))
                            A(lambda vt=vt, vtn=vtn, sst=sst, sstn=sstn, h=h, c=c, s_=s_: S.mm(
                                [lambda e: e.matmul(pO[:, s_, :], PTt[s_][:], vt[:, c, h * 256:(h + 1) * 256], start=True, stop=False),
                                 lambda e: e.matmul(pO[:, s_, :], qfb[s_][:, 0, :], Sfb[:, h, :], start=False, stop=False),
                                 lambda e: e.matmul(pO[:, s_, :], qfb[s_][:, 1, :], sst[:, c, h * 256:(h + 1) * 256], start=False, stop=True)],
                                reads=[f'PT{s_}', vtn, f'qf{s_}', f'qb{s_}', f'Sfb{h}', sstn], writes=[f'pO{s_}', f'bkB{s_}']))
                            r = rst[s_]
                            rn = f'rst{s_}'
                            A(lambda r=r, rn=rn, s_=s_: S.op('act', lambda e: e.activation(junk2[s_][:], pO[:, s_, :], AF.Square, accum_out=r[:, 0:1]),
                                                             reads=[f'pO{s_}'], writes=[f'junk2{s_}', rn + 'a', f'bkB{s_}']))
                            A(lambda r=r, rn=rn: S.op('dve', lambda e: e.tensor_scalar(r[:, 1:2], r[:, 0:1], 1.0 / 256.0, EPS, ALU.mult, ALU.add),
                                                      reads=[rn + 'a'], writes=[rn + 'b']))
                            A(lambda r=r, rn=rn: S.op('act', lambda e: e.sqrt(r[:, 2:3], r[:, 1:2]), reads=[rn + 'b'], writes=[rn + 'c']))
                            A(lambda r=r, rn=rn: S.op('dve', lambda e: e.reciprocal(r[:, 3:4], r[:, 2:3]), reads=[rn + 'c'], writes=[rn + 'd']))
                            A(lambda r=r, rn=rn, gt=gt, gtn=gtn, h=h, c=c, s_=s_: S.op(
                                'dve', lambda e: e.scalar_tensor_tensor(yr[s_][:], pO[:, s_, :], r[:, 3:4], gt[:, c, h * 256:(h + 1) * 256], ALU.mult, ALU.mult),
                                reads=[f'pO{s_}', rn + 'd', gtn], writes=[f'yr{s_}', f'bkB{s_}']))
                            A(lambda s_=s_: S.mm([lambda e: e.transpose(pY[:, s_, 0, :], yr[s_][:, 0:128], ident[:]),
                                                  lambda e: e.transpose(pY[:, s_, 1, :], yr[s_][:, 128:256], ident[:])],
                                                 reads=[f'yr{s_}', 'ident'], writes=[f'pY{s_}', f'bkA{s_}']))
                            A(lambda ys=ys, ysn=ysn, h=h, c=c, csl=csl, s_=s_: copy_op('act', ys[:, h * 2:h * 2 + 2, csl], pY[:, s_, :, :], [f'pY{s_}'], [ysn + f'_{c}{h}', f'bkA{s_}']))
                            state_update(A, s_, 0, h, vt, vtn, c)
                            A(lambda h=h: copy_op('act', Sfb[:, h, :], St[:, 0, h, :], [f'St0{h}'], [f'Sfb{h}']))

                            def fin(blk=blk, ys=ys, ysn=ysn):
                                bdone[blk] = bdone.get(blk, 0) + 1
                                if bdone[blk] == 16:
                                    tsl = slice(blk * 512, (blk + 1) * 512)
                                    S.dma('sp', yrT_d.rearrange("(g f) t -> f g t", f=128)[:, :, tsl], ys[:],
                                          reads=[ysn + f'_{c2}{h2}' for c2 in range(4) for h2 in range(4)], writes=['yrT_d'])
                                    if blk + 2 < nblk:
                                        loadB(blk + 2, blk % 2)
                            A(fin)
                            yield ops

            cast_list = []
            for (src_, dst_) in ((I['peer_u'], uvb_d[:, 0:D]), (I['peer_v'], uvb_d[:, D:2 * D])):
                sv = src_.rearrange("(n g p) d -> n p g d", g=4, p=128)
                dv = dst_.rearrange("(n g p) d -> n p g d", g=4, p=128)
                for c in range(32):
                    cast_list.append((sv[c], dv[c]))

            def mixedB():
                for i, ch in enumerate(chainsB()):
                    n = i // 4
                    if n < len(cast_list):
                        sv_c, dv_c = cast_list[n]
                        bi = n % 2
                        if i % 4 == 0:
                            ch.insert(0, lambda bi=bi, sv_c=sv_c: S.dma('sp', cst[bi][:], sv_c, writes=[f'cst{bi}']))
                        if i % 4 == 3:
                            ch.insert(len(ch) - 1, lambda bi=bi, n=n: copy_op(('act', 'dve')[n % 2], cbf[bi][:], cst[bi][:], [f'cst{bi}'], [f'cbf{bi}']))
                            ch.insert(len(ch) - 1, lambda bi=bi, dv_c=dv_c: S.dma('sp', dv_c, cbf[bi][:], reads=[f'cbf{bi}'], writes=['tab']))
                    yield ch

            loadB(0, 0)
            if nblk > 1:
                loadB(1, 1)
            run_interleaved(mixedB(), KR)
            S.barrier()
        if stop_after in ('ret', 'retsmall'):
            return nc

        kern_d = scratch("kern", [D, 2 * L], BF16)
        ksp_d = scratch("ksp", [128, D, 2, 128], F32)
        u_d = scratch("u", [D, L], BF16)
        yconv_d = scratch("yconv", [D, L], F32)
        hyT_d = scratch("hyT", [D, L], BF16)
        invn_d = scratch("invn", [128, 8], F32)
        TWO_PI = 2.0 * math.pi
        MAGIC = 12582912.0
        with ExitStack() as ph:
            fw1 = sb(ph, "fw1", [33, 64]); fw2 = sb(ph, "fw2", [64, 64]); fw3 = sb(ph, "fw3", [64, 64])
            fw4 = sb(ph, "fw4", [64, 2 * D])
            fbc = sb(ph, "fbc", [64, 4]); fq2 = sb(ph, "fq2", [64, 1])
            dl = sb(ph, "dl", [128, 8]); nda = sb(ph, "nda", [128, 8])
            asum = sb(ph, "asum", [128, 8, 32]); invn = sb(ph, "invn", [128, 8]); atot = sb(ph, "atot", [128, 8])
            zb = [sb(ph, f"zb{i}", [33, 512]) for i in range(2)]
            lw = [sb(ph, f"lw{i}", [128, 512]) for i in range(2)]
            hu = [sb(ph, f"hu{i}", [64, 512]) for i in range(2)]; hk = [sb(ph, f"hk{i}", [64, 512]) for i in range(2)]
            hf = [sb(ph, f"hf{i}", [64, 512]) for i in range(2)]
            hh = [[sb(ph, f"hh{c}_{i}", [64, 512]) for i in range(3)] for c in range(2)]
            wn = [[sb(ph, f"wn{c}_{i}", [128, 512]) for i in range(2)] for c in range(2)]
            kf = [[sb(ph, f"kf{c}_{i}", [128, 512]) for i in range(2)] for c in range(2)]
            kbb = [[sb(ph, f"kbb{c}_{i}", [128, 512], BF16) for i in range(2)] for c in range(2)]
            pM = [ps(ph, f"pM{i}", [128, 512]) for i in range(2)]
            pG = [[ps(ph, f"pG{c}_{i}", [128, 512]) for i in range(2)] for c in range(2)]
            S.dma('sp', fw1[0:32, :], I['hy_fw1'][1:33, :], writes=['fw1'])
            S.dma('sp', fw1[32:33, :], I['hy_fw1'][0:1, :], writes=['fw1'])
            S.dma('sp', fw2[:], I['hy_fw2'], writes=['fw2'])
            S.dma('sp', fw3[:], I['hy_fw3'], writes=['fw3'])
            S.dma('sp', fw4[:], I['hy_fw4'], writes=['fw4'])
            for li, nme in enumerate(['hy_fb1', 'hy_fb2', 'hy_fb3', 'hy_sin_freq']):
                S.dma('sp', fbc[:, li:li + 1], I[nme].rearrange("o j -> j o"), writes=['fbc'], allow_slow_non_contiguous=True)
            S.dma('sp', dl[:], I['hy_deltas'].rearrange("o (g p) -> p (o g)", p=128), writes=['dl'], allow_slow_non_contiguous=True)
            S.op('dve', lambda e: e.tensor_scalar(fq2[:], fbc[:, 3:4], 1.0 / TWO_PI, None, ALU.mult), reads=['fbc'], writes=['fq2'])
            S.op('dve', lambda e: e.tensor_scalar(atot[:], dl[:], -1.0, None, ALU.mult), reads=['dl'], writes=['atot'])
            S.op('dve', lambda e: e.tensor_tensor(nda[:], dl[:], atot[:], ALU.min), reads=['dl', 'atot'], writes=['nda'])
            S.barrier()
            fws = [fw1, fw2, fw3]

            def fload(blk):
                cs = blk % 2
                nsl = slice(blk * 512, (blk + 1) * 512)
                S.dma('sp', zb[cs][:], C['zfeat'][:, nsl], writes=[f'zb{cs}'])
                S.dma('sp', lw[cs][:], C['lagw'][0:1, nsl].to_broadcast([128, 512]), writes=[f'lw{cs}'])

            def filt_chains():
                for blk in range(32):
                    cs = blk % 2
                    ops = []
                    A = ops.append
                    z, zn = zb[cs], f'zb{cs}'
                    lwt, lwn = lw[cs], f'lw{cs}'
                    nsl = slice(blk * 512, (blk + 1) * 512)
                    prev, prevn, kdim = z, zn, 33
                    pt, pn = pM[cs], f'pM{cs}'
                    HU, HK, HF = hu[cs], hk[cs], hf[cs]
                    for li in range(3):
                        A(lambda pt=pt, pn=pn, li=li, prev=prev, prevn=prevn, kdim=kdim: S.mm(
                            [lambda e: e.matmul(pt[0:64, :], fws[li][0:kdim, :], prev[0:kdim, :], start=True, stop=True)], reads=[prevn], writes=[pn]))
                        A(lambda pt=pt, pn=pn, li=li, HU=HU, cs=cs: S.op('dve', lambda e: e.tensor_scalar(HU[:], pt[0:64, :], fbc[:, li:li + 1], fq2[:, 0:1], ALU.add, ALU.mult),
                                                                   reads=[pn], writes=[f'hu{cs}']))
                        A(lambda HU=HU, HK=HK, cs=cs: S.op('dve', lambda e: e.tensor_scalar(HK[:], HU[:], MAGIC, MAGIC, ALU.add, ALU.subtract), reads=[f'hu{cs}'], writes=[f'hk{cs}']))
                        A(lambda HU=HU, HK=HK, HF=HF, cs=cs: S.op('dve', lambda e: e.tensor_tensor(HF[:], HU[:], HK[:], ALU.subtract), reads=[f'hu{cs}', f'hk{cs}'], writes=[f'hf{cs}']))
                        A(lambda HF=HF, li=li, cs=cs: S.op('act', lambda e: e.activation(hh[cs][li][:], HF[:], AF.Sin, scale=TWO_PI), reads=[f'hf{cs}'], writes=[f'hh{cs}_{li}']))
                        prev, prevn, kdim = hh[cs][li], f'hh{cs}_{li}', 64
                    dbase = 0 if blk < 16 else D
                    for g in range(8):
                        gi = g % 2
                        ptg, png = pG[cs][gi], f'pG{cs}_{gi}'
                        WN, KF, KB_ = wn[cs][gi], kf[cs][gi], kbb[cs][gi]
                        A(lambda ptg=ptg, png=png, g=g, cs=cs, dbase=dbase: S.mm(
                            [lambda e: e.matmul(ptg[:], fw4[:, dbase + g * 128:dbase + (g + 1) * 128], hh[cs][2][:], start=True, stop=True)],
                            reads=[f'hh{cs}_2'], writes=[png]))
                        A(lambda WN=WN, lwt=lwt, lwn=lwn, g=g, cs=cs, gi=gi: S.op('act', lambda e: e.activation(WN[:], lwt[:], AF.Exp, scale=nda[:, g:g + 1]),
                                                                                reads=[lwn], writes=[f'wn{cs}_{gi}']))
                        A(lambda KF=KF, WN=WN, ptg=ptg, png=png, cs=cs, gi=gi: S.op('dve', lambda e: e.tensor_tensor(KF[:], ptg[:], WN[:], ALU.mult),
                                                                                  reads=[png, f'wn{cs}_{gi}'], writes=[f'kf{cs}_{gi}']))
                        A(lambda KF=KF, g=g, blk=blk, cs=cs, gi=gi: S.op('dve', lambda e: e.tensor_reduce(asum[:, g, blk:blk + 1], KF[:], AX.X, ALU.add, apply_absolute_value=True),
                                                                       reads=[f'kf{cs}_{gi}'], writes=[f'asum{g}_{blk}']))
                        A(lambda KF=KF, KB_=KB_, cs=cs, gi=gi: copy_op('act', KB_[:], KF[:], [f'kf{cs}_{gi}'], [f'kbb{cs}_{gi}']))
                        A(lambda KB_=KB_, g=g, nsl=nsl, cs=cs, gi=gi: S.dma('sp', kern_d[g * 128:(g + 1) * 128, nsl], KB_[:], reads=[f'kbb{cs}_{gi}'], writes=['kern_d']))

                    def fin(blk=blk):
                        if blk + 2 < 32:
                            fload(blk + 2)
                    A(fin)
                    yield ops

            fload(0)
            fload(1)
            run_interleaved(filt_chains(), 2)
            S.op('dve', lambda e: e.tensor_reduce(atot[:], asum[:], AX.X, ALU.add), reads=[f'asum{g}_{b}' for g in range(8) for b in range(32)], writes=['atot'])
            S.op('dve', lambda e: e.tensor_scalar(atot[:], atot[:], EPS, None, ALU.add), reads=['atot'], writes=['atot'])
            S.op('dve', lambda e: e.reciprocal(invn[:], atot[:]), reads=['atot'], writes=['invn'])
            S.dma('sp', invn_d, invn[:], reads=['invn'], writes=['invn_d'])
            S.barrier()
        if stop_after == 'hfilt':
            return nc

        def conv3(e_out, zt, ztn, cw, cbb, s, g, outn):
            S.op('dve', lambda e: e.tensor_scalar(e_out[:], zt[:], cw[:, 1, s, g:g + 1], cbb[:, s, g:g + 1], ALU.mult, ALU.add),
                 reads=[ztn], writes=[outn])
            S.op('dve', lambda e: e.scalar_tensor_tensor(e_out[:, 1:L], zt[:, 0:L - 1], cw[:, 0, s, g:g + 1], e_out[:, 1:L], ALU.mult, ALU.add),
                 reads=[ztn, outn], writes=[outn])
            S.op('dve', lambda e: e.scalar_tensor_tensor(e_out[:, 0:L - 1], zt[:, 1:L], cw[:, 2, s, g:g + 1], e_out[:, 0:L - 1], ALU.mult, ALU.add),
                 reads=[ztn, outn], writes=[outn])

        with ExitStack() as ph:
            cw = sb(ph, "cw", [128, 3, 3, 8]); cbb = sb(ph, "cbb", [128, 3, 8])
            z1 = [sb(ph, f"z1{i}", [128, L], BF16) for i in range(2)]
            z2 = [sb(ph, f"z2{i}", [128, L], BF16) for i in range(2)]
            t1 = sb(ph, "t1", [128, L]); t2 = sb(ph, "t2", [128, L])
            ub = [sb(ph, f"ub{i}", [128, L], BF16) for i in range(2)]
            for k in range(3):
                S.dma('sp', cw[:, k, :, :], I['hy_conv_w'][k:k + 1, :].rearrange("o (s g p) -> p (o s) g", p=128, g=8),
                      writes=['cw'], allow_slow_non_contiguous=True)
            S.dma('sp', cbb[:], I['hy_conv_b'].rearrange("o (s g p) -> p (o s) g", p=128, g=8), writes=['cw'], allow_slow_non_contiguous=True)
            for g in range(8):
                a, an = z1[g % 2], f'z1{g % 2}'
                b_, bn = z2[g % 2], f'z2{g % 2}'
                S.dma('sp', a[:], zhy[D + g * 128:D + (g + 1) * 128, :], writes=[an])
                S.dma('sp', b_[:], zhy[2 * D + g * 128:2 * D + (g + 1) * 128, :], writes=[bn])
                conv3(t1, a, an, cw, cbb, 1, g, 't1')
                conv3(t2, b_, bn, cw, cbb, 2, g, 't2')
                S.op('dve', lambda e, g=g: e.tensor_tensor(ub[g % 2][:], t1[:], t2[:], ALU.mult), reads=['t1', 't2'], writes=[f'ub{g % 2}'])
                S.dma('sp', u_d[g * 128:(g + 1) * 128, :], ub[g % 2][:], reads=[f'ub{g % 2}'], writes=['u_d'])
            S.barrier()
        if stop_after == 'hu':
            return nc

        with ExitStack() as ph:
            fc1 = sb(ph, "fc1", [128, 256], BF16); fcj1 = sb(ph, "fcj1", [128, 256], BF16); fcj2 = sb(ph, "fcj2", [128, 256], BF16)
            fcj1n = sb(ph, "fcj1n", [128, 256], BF16)
            f3 = sb(ph, "f3", [128, 4, 128], BF16)
            tw = sb(ph, "tw", [128, 2, 128])
            src = [sb(ph, f"src{i}", [128, 16, 128], BF16) for i in range(2)]
            ksp = [sb(ph, f"ksp{i}", [128, 16, 2, 128]) for i in range(2)]
            yo = [sb(ph, f"yo{i}", [64, 16, 128]) for i in range(2)]
            KQ = 4
            p1t = [sb(ph, f"p1t{i}", [128, 2, 4, 128], BF16) for i in range(KQ)]
            p2t = [sb(ph, f"p2t{i}", [128, 2, 4, 128], BF16) for i in range(KQ)]
            m4 = [sb(ph, f"m4{i}", [128, 4, 4, 128]) for i in range(KQ)]
            Yt = [sb(ph, f"Yt{i}", [128, 2, 4, 128], BF16) for i in range(KQ)]
            q1t = [sb(ph, f"q1t{i}", [128, 2, 4, 128], BF16) for i in range(KQ)]
            q2t = [sb(ph, f"q2t{i}", [128, 2, 4, 128], BF16) for i in range(KQ)]
            pAC = [ps(ph, f"pAC{i}", [128, 4, 256]) for i in range(KQ)]
            pX = [pAC[i][:].rearrange("p c (r k) -> p r (c k)", r=2) if False else None for i in range(KQ)]
            S.dma('sp', fc1[:], C['fc1'], writes=['c']); S.dma('sp', fcj1[:], C['fcj1'], writes=['c'])
            S.dma('sp', fcj2[:], C['fcj2'], writes=['c']); S.dma('sp', fcj1n[:], C['fcj1n'], writes=['c'])
            S.dma('sp', f3[:], C['f3'].rearrange("p (a b) -> p a b", a=4), writes=['c'])
            S.dma('sp', tw[:], C['tw'].rearrange("p (a b) -> p a b", a=2), writes=['c'])
            S.barrier()
            Wre_b = tw[:, 0, :].unsqueeze(1).unsqueeze(1).to_broadcast([128, 4, 2, 128])
            Wim_b = tw[:, 1, :].unsqueeze(1).unsqueeze(1).to_broadcast([128, 4, 2, 128])
            FRE, FIM, NFIM, NFRE = 0, 1, 2, 3

            def twid_products(A, pin, pinn, P1, P1n, P2, P2n):
                pv = pin[:].rearrange("p c (r k) -> p c r k", r=2)
                A(lambda: S.op('dve', lambda e: e.tensor_tensor(P1[:].rearrange("p r c k -> p c r k"), pv, Wre_b, ALU.mult), reads=[pinn], writes=[P1n]))
                A(lambda: S.op('dve', lambda e: e.tensor_tensor(P2[:].rearrange("p r c k -> p c r k"), pv, Wim_b, ALU.mult), reads=[pinn], writes=[P2n]))

            def fl(t, r):
                return t[:, r, :, :].rearrange("p c k -> p (c k)")

            class _V:
                def __init__(self, t):
                    self.t = t

                def __getitem__(self, key):
                    return self.t[:].rearrange("p (r c) k -> p r (c k)", r=2)[key]

            def PXV(b):
                return _V(pAC[b])

            def fft_fwd(A, b, st, stn, q, kdim):
                pA, pAn = pAC[b], f'pAC{b}'
                pXb, pXn = PXV(b), f'pAC{b}'
                A(lambda: S.mm([(lambda e, ci=ci: e.matmul(pA[:, ci, :], st[0:kdim, q * 4 + ci, :], fc1[0:kdim, :], start=True, stop=True))
                                for ci in range(4)], reads=[stn], writes=[pAn]))
                P1, P2 = p1t[b], p2t[b]
                twid_products(A, pA, pAn, P1, f'p1t{b}', P2, f'p2t{b}')
                A(lambda: S.mm([lambda e: e.matmul(pXb[:, 0, :], f3[:, FRE, :], fl(P1, 0), start=True, stop=False),
                                lambda e: e.matmul(pXb[:, 0, :], f3[:, NFRE, :], fl(P2, 1), start=False, stop=False),
                                lambda e: e.matmul(pXb[:, 0, :], f3[:, NFIM, :], fl(P2, 0), start=False, stop=False),
                                lambda e: e.matmul(pXb[:, 0, :], f3[:, NFIM, :], fl(P1, 1), start=False, stop=True),
                                lambda e: e.matmul(pXb[:, 1, :], f3[:, FIM, :], fl(P1, 0), start=True, stop=False),
                                lambda e: e.matmul(pXb[:, 1, :], f3[:, NFIM, :], fl(P2, 1), start=False, stop=False),
                                lambda e: e.matmul(pXb[:, 1, :], f3[:, FRE, :], fl(P2, 0), start=False, stop=False),
                                lambda e: e.matmul(pXb[:, 1, :], f3[:, FRE, :], fl(P1, 1), start=False, stop=True)],
                               reads=[f'p1t{b}', f'p2t{b}'], writes=[pXn]))

            ngrp = 64 if stop_after != 'hsmall' else 2

            def kload(g):
                S.dma('sp', src[g % 2][:], kern_d[g * 16:(g + 1) * 16, :].rearrange("c (h l) -> h c l", l=128), writes=[f'src{g % 2}'])

            def cload(g):
                S.dma('sp', src[g % 2][0:64, :, :], u_d[g * 16:(g + 1) * 16, :].rearrange("c (h l) -> h c l", l=128), writes=[f'src{g % 2}'])
                S.dma('sp', ksp[g % 2][:], ksp_d[:, g * 16:(g + 1) * 16, :, :], writes=[f'ksp{g % 2}'])

            def ksp_chains():
                n = 0
                for gq in range(ngrp):
                    st, stn = src[gq % 2], f'src{gq % 2}'
                    kk_, kn = ksp[gq % 2], f'ksp{gq % 2}'
                    for q in range(4):
                        b = n % KQ
                        n += 1
                        ops = []
                        A = ops.append
                        fft_fwd(A, b, st, stn, q, 128)
                        pXb, pXn = PXV(b), f'pAC{b}'
                        A(lambda q=q, kk_=kk_, kn=kn, pXb=pXb, pXn=pXn: copy_op('act', kk_[:, q * 4:(q + 1) * 4, 0, :], pXb[:, 0, :].rearrange("p (c k) -> p c k", c=4), [pXn], [kn + f'_{q}r']))
                        A(lambda q=q, kk_=kk_, kn=kn, pXb=pXb, pXn=pXn: copy_op('act', kk_[:, q * 4:(q + 1) * 4, 1, :], pXb[:, 1, :].rearrange("p (c k) -> p c k", c=4), [pXn], [kn + f'_{q}i']))
                        def fin(gq=gq, kk_=kk_, kn=kn):
                            gdone[gq] = gdone.get(gq, 0) + 1
                            if gdone[gq] == 4:
                                S.dma('sp', ksp_d[:, gq * 16:(gq + 1) * 16, :, :], kk_[:],
                                      reads=[kn + f'_{q2}{x}' for q2 in range(4) for x in 'ri'], writes=['ksp_d'])
                                if gq + 2 < ngrp:
                                    kload(gq + 2)
                        A(fin)
                        yield ops

            gdone = {}
            kload(0)
            if ngrp > 1:
                kload(1)
            run_interleaved(ksp_chains(), KQ)
            S.barrier()
            if stop_after == 'hksp':
                return nc

            def conv_chains():
                n = 0
                for gq in range(ngrp):
                    st, stn = src[gq % 2], f'src{gq % 2}'
                    kk_, kn = ksp[gq % 2], f'ksp{gq % 2}'
                    yt, yn = yo[gq % 2], f'yo{gq % 2}'
                    for q in range(4):
                        b = n % KQ
                        n += 1
                        ops = []
                        A = ops.append
                        fft_fwd(A, b, st, stn, q, 64)
                        pXb, pXn = PXV(b), f'pAC{b}'
                        pC, pCn = pAC[b], f'pAC{b}'
                        M4, m4n = m4[b], f'm4{b}'
                        kq = kk_[:, q * 4:(q + 1) * 4, :, :]
                        Xre = pXb[:, 0, :].rearrange("p (c k) -> p c k", c=4)
                        Xim = pXb[:, 1, :].rearrange("p (c k) -> p c k", c=4)
                        A(lambda M4=M4, Xre=Xre, kq=kq, pXn=pXn, kn=kn, m4n=m4n: S.op('dve', lambda e: e.tensor_tensor(M4[:, 0], Xre, kq[:, :, 0, :], ALU.mult), reads=[pXn, kn], writes=[m4n + '0']))
                        A(lambda M4=M4, Xim=Xim, kq=kq, pXn=pXn, kn=kn, m4n=m4n: S.op('dve', lambda e: e.tensor_tensor(M4[:, 1], Xim, kq[:, :, 1, :], ALU.mult), reads=[pXn, kn], writes=[m4n + '1']))
                        A(lambda M4=M4, Xre=Xre, kq=kq, pXn=pXn, kn=kn, m4n=m4n: S.op('dve', lambda e: e.tensor_tensor(M4[:, 2], Xre, kq[:, :, 1, :], ALU.mult), reads=[pXn, kn], writes=[m4n + '2']))
                        A(lambda M4=M4, Xim=Xim, kq=kq, pXn=pXn, kn=kn, m4n=m4n: S.op('dve', lambda e: e.tensor_tensor(M4[:, 3], Xim, kq[:, :, 0, :], ALU.mult), reads=[pXn, kn], writes=[m4n + '3']))

                        Y, Yn = Yt[b], f'Yt{b}'
                        A(lambda M4=M4, Y=Y, Yn=Yn, m4n=m4n: S.op('pool', lambda e: e.tensor_tensor(Y[:, 0, :, :], M4[:, 0], M4[:, 1], ALU.subtract), reads=[m4n + '0', m4n + '1'], writes=[Yn + 'r']))
                        A(lambda M4=M4, Y=Y, Yn=Yn, m4n=m4n: S.op('pool', lambda e: e.tensor_tensor(Y[:, 1, :, :], M4[:, 2], M4[:, 3], ALU.add), reads=[m4n + '2', m4n + '3'], writes=[Yn + 'i']))

                        def step5(Y=Y, Yn=Yn, pC=pC, pCn=pCn):
                            fns = []
                            for ci in range(4):
                                fns.append(lambda e, ci=ci: e.matmul(pC[:, ci, :], Y[:, 0, ci, :], fcj1[:], start=True, stop=False))
                                fns.append(lambda e, ci=ci: e.matmul(pC[:, ci, :], Y[:, 1, ci, :], fcj2[:], start=False, stop=True))
                            S.mm(fns, reads=[Yn + 'r', Yn + 'i'], writes=[pCn])
                        A(step5)
                        Q1, Q2 = q1t[b], q2t[b]
                        twid_products(A, pC, pCn, Q1, f'q1t{b}', Q2, f'q2t{b}')
                        A(lambda pXb=pXb, pXn=pXn, Q1=Q1, Q2=Q2, b=b: S.mm(
                            [lambda e: e.matmul(pXb[0:64, 0, :], f3[:, FRE, 0:64], fl(Q1, 0), start=True, stop=False),
                             lambda e: e.matmul(pXb[0:64, 0, :], f3[:, FRE, 0:64], fl(Q2, 1), start=False, stop=False),
                             lambda e: e.matmul(pXb[0:64, 0, :], f3[:, FIM, 0:64], fl(Q1, 1), start=False, stop=False),
                             lambda e: e.matmul(pXb[0:64, 0, :], f3[:, NFIM, 0:64], fl(Q2, 0), start=False, stop=True)],
                            reads=[f'q1t{b}', f'q2t{b}'], writes=[pXn]))
                        A(lambda q=q, yt=yt, yn=yn, pXb=pXb, pXn=pXn: copy_op('act', yt[:, q * 4:(q + 1) * 4, :], pXb[0:64, 0, :].rearrange("p (c k) -> p c k", c=4),
                                                                            [pXn], [yn + f'_{q}'], scale=1.0 / 16384.0))
                        def fin(gq=gq, yt=yt, yn=yn):
                            gdone[gq] = gdone.get(gq, 0) + 1
                            if gdone[gq] == 4:
                                S.dma('sp', yconv_d[gq * 16:(gq + 1) * 16, :].rearrange("c (h l) -> h c l", l=128), yt[:],
                                      reads=[yn + f'_{q2}' for q2 in range(4)], writes=['yconv_d'])
                                if gq + 2 < ngrp:
                                    cload(gq + 2)
                        A(fin)
                        yield ops

            gdone = {}
            cload(0)
            if ngrp > 1:
                cload(1)
            run_interleaved(conv_chains(), KQ)
            S.barrier()
        if stop_after in ('hconv', 'hsmall'):
            return nc

        with ExitStack() as ph:
            cw = sb(ph, "cw", [128, 3, 3, 8]); cbb = sb(ph, "cbb", [128, 3, 8])
            hbias = sb(ph, "hbias", [128, 8]); invn = sb(ph, "invn", [128, 8])
            z0 = [sb(ph, f"z0{i}", [128, L], BF16) for i in range(2)]
            uu = [sb(ph, f"uu{i}", [128, L], BF16) for i in range(2)]
            yc = [sb(ph, f"yc{i}", [128, L]) for i in range(2)]
            t1 = sb(ph, "t1", [128, L])
            ho = [sb(ph, f"ho{i}", [128, L], BF16) for i in range(2)]
            for k in range(3):
                S.dma('sp', cw[:, k, :, :], I['hy_conv_w'][k:k + 1, :].rearrange("o (s g p) -> p (o s) g", p=128, g=8),
                      writes=['cw'], allow_slow_non_contiguous=True)
            S.dma('sp', cbb[:], I['hy_conv_b'].rearrange("o (s g p) -> p (o s) g", p=128, g=8), writes=['cw'], allow_slow_non_contiguous=True)
            S.dma('sp', hbias[:], I['hy_bias'].rearrange("o (g p) -> p (o g)", p=128), writes=['hbias'], allow_slow_non_contiguous=True)
            S.dma('sp', invn[:], invn_d, writes=['invn'])
            for g in range(8):
                gi = g % 2
                S.dma('sp', z0[gi][:], zhy[g * 128:(g + 1) * 128, :], writes=[f'z0{gi}'])
                S.dma('sp', uu[gi][:], u_d[g * 128:(g + 1) * 128, :], writes=[f'uu{gi}'])
                S.dma('sp', yc[gi][:], yconv_d[g * 128:(g + 1) * 128, :], writes=[f'yc{gi}'])
                conv3(t1, z0[gi], f'z0{gi}', cw, cbb, 0, g, 't1')
                S.op('dve', lambda e, g=g, gi=gi: e.tensor_scalar(yc[gi][:], yc[gi][:], invn[:, g:g + 1], None, ALU.mult),
                     reads=[f'yc{gi}', 'invn'], writes=[f'yc{gi}'])
                S.op('dve', lambda e, g=g, gi=gi: e.scalar_tensor_tensor(yc[gi][:], uu[gi][:], hbias[:, g:g + 1], yc[gi][:], ALU.mult, ALU.add),
                     reads=[f'yc{gi}', f'uu{gi}', 'hbias'], writes=[f'yc{gi}'])
                S.op('dve', lambda e, gi=gi: e.tensor_tensor(ho[gi][:], yc[gi][:], t1[:], ALU.mult), reads=[f'yc{gi}', 't1'], writes=[f'ho{gi}'])
                S.dma('sp', hyT_d[g * 128:(g + 1) * 128, :], ho[gi][:], reads=[f'ho{gi}'], writes=['hyT_d'])
            S.barrier()
        if stop_after == 'hy':
            return nc

        xl_d = scratch("xl", [L, D], F32)
        h2_d = scratch("h2", [L, D], F32)

        def load_cast_w(st, Wt, wname, src):
            for k in range(8):
                i = k % 2
                S.dma('sp', st[i][:], src[k * 128:(k + 1) * 128, :], writes=[f'wstg{i}'])
                copy_op(('act', 'dve')[k % 2], Wt[:, k, :], st[i][:], [f'wstg{i}'], [f'{wname}{k}'])

        def rms_rstd(r, rn, src, srcn, jk):
            S.op('act', lambda e: e.activation(jk[:], src[:], AF.Square, accum_out=r[:, 0:1]), reads=[srcn], writes=['jk', rn + 'a'])
            S.op('dve', lambda e: e.tensor_scalar(r[:, 1:2], r[:, 0:1], 1.0 / D, EPS, ALU.mult, ALU.add), reads=[rn + 'a'], writes=[rn + 'b'])
            S.op('act', lambda e: e.sqrt(r[:, 2:3], r[:, 1:2]), reads=[rn + 'b'], writes=[rn + 'c'])
            S.op('dve', lambda e: e.reciprocal(r[:, 3:4], r[:, 2:3]), reads=[rn + 'c'], writes=[rn + 'd'])

        with ExitStack() as pm_:
            Why = sb(pm_, "Why", [128, 8, D], BF16); Wret = sb(pm_, "Wret", [128, 8, D], BF16); Wo = sb(pm_, "Wo", [128, 8, D], BF16)
            G1 = sb(pm_, "G1", [128, D]); A2 = sb(pm_, "A2", [128, D]); SH2 = sb(pm_, "SH2", [128, D])
            wstg = [sb(pm_, f"wstg{i}", [128, D]) for i in range(2)]
            S.dma('sp', G1[:], mod_d[2:3, :].to_broadcast([128, D]), writes=['G1'])
            S.dma('sp', A2[:], mod_d[3:4, :].to_broadcast([128, D]), writes=['A2'])
            S.dma('sp', SH2[:], mod_d[4:5, :].to_broadcast([128, D]), writes=['SH2'])
            load_cast_w(wstg, Why, 'Why', I['w_hy_out'])
            load_cast_w(wstg, Wret, 'Wret', I['w_ret_out'])
            load_cast_w(wstg, Wo, 'Wo', I['w_o'])
            S.barrier()
            blkt = [[sb(pm_, f"blk{i}_{j}", [128, 8, 512], BF16) for j in range(4)] for i in range(2)]
            mT = sb(pm_, "mT", [128, 8, 512], BF16)
            ta = [sb(pm_, f"ta{i}", [128, 512]) for i in range(2)]
            tb = [sb(pm_, f"tb{i}", [128, 512]) for i in range(2)]
            xt = [sb(pm_, f"xt{i}", [128, D]) for i in range(4)]
            xl = [sb(pm_, f"xl{i}", [128, D]) for i in range(2)]
            h2t = [sb(pm_, f"h2t{i}", [128, D]) for i in range(2)]
            jk = sb(pm_, "jk", [128, D], BF16)
            rs = [sb(pm_, f"rs{i}", [128, 4]) for i in range(2)]
            pH = [ps(pm_, f"pH{i}", [128, 512]) for i in range(2)]
            pR = [ps(pm_, f"pR{i}", [128, 512]) for i in range(2)]
            pMx = [ps(pm_, f"pMx{i}", [128, 512]) for i in range(4)]
            srcs = [hyT_d, yrT_d, ahT_d, arT_d]
            nblk = NB if stop_after != 'mergesmall' else 2
            tcount = 0
            for b in range(nblk):
                tsl = slice(b * 512, (b + 1) * 512)
                bt = blkt[b % 2]
                bn = [f'blk{b % 2}_{j}' for j in range(4)]
                if b == 0:
                    for j in range(4):
                        S.dma('sp', bt[j][:], srcs[j].rearrange("(g f) t -> f g t", f=128)[:, :, tsl], writes=[bn[j]])
                if b + 1 < nblk:
                    tsl2 = slice((b + 1) * 512, (b + 2) * 512)
                    for j in range(4):
                        S.dma('sp', blkt[(b + 1) % 2][j][:], srcs[j].rearrange("(g f) t -> f g t", f=128)[:, :, tsl2],
                              writes=[f'blk{(b + 1) % 2}_{j}'])
                for ti in range(4):
                    S.dma('sp', xt[ti][:], I['x'][b * 512 + ti * 128:b * 512 + (ti + 1) * 128, :], writes=[f'xt{ti}'])
                for n_ in range(8):
                    i2 = n_ % 2
                    S.mm([(lambda e, k=k: e.matmul(pH[i2][:], Why[:, k, n_ * 128:(n_ + 1) * 128], bt[0][:, k, :], start=(k == 0), stop=(k == 7)))
                          for k in range(8)], reads=[bn[0]], writes=[f'pH{i2}'])
                    S.mm([(lambda e, k=k: e.matmul(pR[i2][:], Wret[:, k, n_ * 128:(n_ + 1) * 128], bt[1][:, k, :], start=(k == 0), stop=(k == 7)))
                          for k in range(8)], reads=[bn[1]], writes=[f'pR{i2}'])
                    S.op('dve', lambda e: e.tensor_tensor(ta[i2][:], pH[i2][:], bt[2][:, n_, :], ALU.mult), reads=[f'pH{i2}', bn[2]], writes=[f'ta{i2}'])
                    S.op('dve', lambda e: e.tensor_tensor(tb[i2][:], pR[i2][:], bt[3][:, n_, :], ALU.mult), reads=[f'pR{i2}', bn[3]], writes=[f'tb{i2}'])
                    S.op('dve', lambda e: e.tensor_tensor(mT[:, n_, :], ta[i2][:], tb[i2][:], ALU.add), reads=[f'ta{i2}', f'tb{i2}'], writes=[f'mT{n_}'])
                mreads = [f'mT{n_}' for n_ in range(8)]
                for ti in range(4):
                    r0 = b * 512 + ti * 128
                    i2 = tcount % 2
                    tcount += 1
                    for hf in range(2):
                        pp = pMx[(tcount * 2 + hf) % 4]
                        ppn = f'pMx{(tcount * 2 + hf) % 4}'
                        S.mm([(lambda e, k=k: e.matmul(pp[:], mT[:, k, ti * 128:(ti + 1) * 128], Wo[:, k, hf * 512:(hf + 1) * 512],
                                                       start=(k == 0), stop=(k == 7))) for k in range(8)], reads=mreads, writes=[ppn])
                        S.op('dve', lambda e: e.tensor_tensor(xl[i2][:, hf * 512:(hf + 1) * 512], pp[:], G1[:, hf * 512:(hf + 1) * 512], ALU.mult),
                             reads=[ppn], writes=[f'xl{i2}_{hf}'])
                    S.op('pool', lambda e: e.tensor_tensor(xl[i2][:], xl[i2][:], xt[ti][:], ALU.add),
                         reads=[f'xl{i2}_0', f'xl{i2}_1', f'xt{ti}'], writes=[f'xl{i2}'])
                    S.dma('sp', xl_d[r0:r0 + 128, :], xl[i2][:], reads=[f'xl{i2}'], writes=['xl_d'])
                    rms_rstd(rs[i2], f'rs{i2}', xl[i2], f'xl{i2}', jk)
                    S.op('dve', lambda e: e.scalar_tensor_tensor(h2t[i2][:], xl[i2][:], rs[i2][:, 3:4], A2[:], ALU.mult, ALU.mult),
                         reads=[f'xl{i2}', f'rs{i2}d'], writes=[f'h2t{i2}'])
                    S.op('pool', lambda e: e.tensor_tensor(h2t[i2][:], h2t[i2][:], SH2[:], ALU.add), reads=[f'h2t{i2}'], writes=[f'h2t{i2}'])
                    S.dma('sp', h2_d[r0:r0 + 128, :], h2t[i2][:], reads=[f'h2t{i2}'], writes=['h2_d'])
            S.barrier()
        if stop_after in ('merge', 'mergesmall'):
            return nc

        NEG = -1.0e30
        import os as _os
        ABL = _os.environ.get('PEER_ABL', '')
        with ExitStack() as pp_:
            Wq = sb(pp_, "Wq", [128, 8, D], BF16)
            KB = sb(pp_, "KB", [128, 8, 256])
            G2 = sb(pp_, "G2", [128, D]); FN = sb(pp_, "FN", [128, D])
            iota16 = sb(pp_, "iota16", [128, 16])
            S.dma('sp', G2[:], mod_d[5:6, :].to_broadcast([128, D]), writes=['G2'])
            S.dma('sp', FN[:], mod_d[6:7, :].to_broadcast([128, D]), writes=['FN'])
            S.dma('sp', iota16[:], C['iota16'], writes=['iota16'])
            with ExitStack() as pq_:
                wstg = [sb(pq_, f"wstg{i}", [128, D]) for i in range(2)]
                SK = sb(pq_, "SK", [128, 16, 64])
                pk = ps(pq_, "pk", [128, 128])
                load_cast_w(wstg, Wq, 'Wq', I['peer_w_query'])
                S.dma('sp', SK[:], I['peer_sub_keys'].rearrange("h c n d -> n (h c) d"), writes=['SK'])
                S.op('pool', lambda e: e.memset(KB[:], 0.0), writes=['KB'])
                for h in range(8):
                    S.mm([lambda e, h=h: e.transpose(pk[:], SK[:, 2 * h:2 * h + 2, :].rearrange("p a d -> p (a d)"), identf[:])],
                         reads=['SK', 'identf'], writes=['pk'])
                    copy_op('act', KB[0:64, h, 0:128], pk[0:64, :], ['pk'], ['KB'])
                    copy_op('act', KB[64:128, h, 128:256], pk[64:128, :], ['pk'], ['KB'])
                S.barrier()
            NG = 16
            gb = [sb(pp_, f"gb{i}", [128, 2 * D], BF16) for i in range(NG)]
            gl = sb(pp_, "gl", [128, 128])
            ND = 8
            Dg = [sb(pp_, f"Dg{i}", [128, 128], BF16) for i in range(ND)]
            h2 = [sb(pp_, f"h2_{i}", [128, D]) for i in range(2)]
            xlp = [sb(pp_, f"xlp{i}", [128, D]) for i in range(2)]
            h2b = sb(pp_, "h2b", [128, D], BF16)
            h2T = sb(pp_, "h2T", [128, 8, 128], BF16)
            qTs = sb(pp_, "qTs", [128, 4, 128])
            sall = sb(pp_, "sall", [128, 8, 2, 128])
            srep = sb(pp_, "srep", [128, 128])
            vv = sb(pp_, "vv", [128, 8, 2, 16]); iu = sb(pp_, "iu", [128, 8, 2, 16], U32); idf = sb(pp_, "idf", [128, 8, 2, 16])
            cand = sb(pp_, "cand", [128, 8, 16, 16]); crep = sb(pp_, "crep", [128, 256])
            tops = sb(pp_, "tops", [128, 8, 16]); pos = sb(pp_, "pos", [128, 8, 16], U32)
            pab = sb(pp_, "pab", [128, 2, 128], U32); pabf = sb(pp_, "pabf", [128, 2, 128])
            oh = sb(pp_, "oh", [128, 128, 16]); isel = sb(pp_, "isel", [128, 2, 128])
            eidf = sb(pp_, "eidf", [128, 128]); EIDX = [sb(pp_, f"EIDX{i}", [128, 128], I32) for i in range(2)]
            GATE = [sb(pp_, f"GATE{i}", [128, 8, 16]) for i in range(2)]
            ex = sb(pp_, "ex", [128, 8, 16]); sm = sb(pp_, "sm", [128, 2, 8])
            actp = sb(pp_, "actp", [128, 128]); coef = sb(pp_, "coef", [128, 128])
            jk2 = [sb(pp_, f"jk2{i}", [128, D], BF16) for i in range(4)]; jk3 = sb(pp_, "jk3", [128, D], BF16)
            xo = sb(pp_, "xo", [128, D]); ot = [sb(pp_, f"ot{i}", [128, D]) for i in range(2)]
            rs = sb(pp_, "rsp", [128, 4])
            pT2 = ps(pp_, "pT2", [128, 8, 128], BF16)
            pq = ps(pp_, "pq", [128, 4, 128])
            psc = ps(pp_, "psc", [128, 4, 256])
            pacc = ps(pp_, "pacc", [128, D])
            ntile = NT if stop_after != 'peersmall' else 1
            ntile = int(_os.environ.get('PEER_TILES', ntile))

            def front_ops(t):
                r0 = t * 128
                i2 = t % 2
                hh_, hn = h2[i2], f'h2_{i2}'
                EI, EIn = EIDX[i2], f'EIDX{i2}'
                GT, GTn = GATE[i2], f'GATE{i2}'
                ops = []
                A = ops.append
                A(lambda: S.dma('sp', hh_[:], h2_d[r0:r0 + 128, :], writes=[hn]))
                A(lambda: S.dma('sp', xlp[i2][:], xl_d[r0:r0 + 128, :], writes=[f'xlp{i2}']))
                A(lambda: copy_op('act', h2b[:], hh_[:], [hn], ['h2b']))
                A(lambda: S.mm([(lambda e, k=k: e.transpose(pT2[:, k, :], h2b[:, k * 128:(k + 1) * 128], ident[:])) for k in range(8)],
                               reads=['h2b', 'ident'], writes=['pT2']))
                A(lambda: copy_op('act', h2T[:], pT2[:], ['pT2'], ['h2T']))
                for hg in range(2):
                    def qproj(hg=hg):
                        fns = []
                        for hh in range(4):
                            h = hg * 4 + hh
                            for k in range(8):
                                fns.append(lambda e, h=h, hh=hh, k=k: e.matmul(pq[:, hh, :], Wq[:, k, h * 128:(h + 1) * 128], h2T[:, k, :],
                                                                             start=(k == 0), stop=(k == 7)))
                        S.mm(fns, reads=['h2T'], writes=['pq'])
                    A(qproj)
                    A(lambda: copy_op('act', qTs[:], pq[:], ['pq'], ['qTs']))
                    A(lambda hg=hg: S.mm([(lambda e, hh=hh: e.matmul(psc[:, hh, :], qTs[:, hh, :], KB[:, hg * 4 + hh, :], start=True, stop=True))
                                          for hh in range(4)], reads=['qTs'], writes=['psc']))
                    A(lambda hg=hg: copy_op('act', sall[:, hg * 4:(hg + 1) * 4, :, :].rearrange("p h c n -> p h (c n)"), psc[:], ['psc'], [f'sall{hg}']))
                for h in range(8):
                    sn = f'sall{h // 4}'
                    for c in range(2):
                        sc_ = sall[:, h, c, :]
                        A(lambda h=h, c=c, sc_=sc_, sn=sn: S.op('dve', lambda e: e.max(out=vv[:, h, c, 0:8], in_=sc_), reads=[sn], writes=[f'vv{h}{c}a']))
                        A(lambda h=h, c=c, sc_=sc_, sn=sn: S.op('dve', lambda e: e.match_replace(out=srep[:], in_to_replace=vv[:, h, c, 0:8], in_values=sc_, imm_value=NEG),
                                                                reads=[sn, f'vv{h}{c}a'], writes=['srep']))
                        A(lambda h=h, c=c: S.op('dve', lambda e: e.max(out=vv[:, h, c, 8:16], in_=srep[:]), reads=['srep'], writes=[f'vv{h}{c}b']))
                        A(lambda h=h, c=c, sc_=sc_, sn=sn: S.op('dve', lambda e: e.max_index(out=iu[:, h, c, 0:8], in_max=vv[:, h, c, 0:8], in_values=sc_),
                                                                reads=[sn, f'vv{h}{c}a'], writes=[f'iu{h}{c}a']))
                        A(lambda h=h, c=c: S.op('dve', lambda e: e.max_index(out=iu[:, h, c, 8:16], in_max=vv[:, h, c, 8:16], in_values=srep[:]),
                                                reads=['srep', f'vv{h}{c}b'], writes=[f'iu{h}{c}b']))
                vall = [f'vv{h}{c}{x}' for h in range(8) for c in range(2) for x in 'ab']
                iall = [f'iu{h}{c}{x}' for h in range(8) for c in range(2) for x in 'ab']
                A(lambda: S.op('dve', lambda e: e.tensor_copy(idf[:], iu[:]), reads=iall, writes=['idf']))
                A(lambda: S.op('dve', lambda e: e.tensor_tensor(cand[:], vv[:, :, 0, :].unsqueeze(3).to_broadcast([128, 8, 16, 16]),
                                                                vv[:, :, 1, :].unsqueeze(2).to_broadcast([128, 8, 16, 16]), ALU.add),
                               reads=vall, writes=['cand']))
                for h in range(8):
                    cf = cand[:, h, :, :].rearrange("p a b -> p (a b)")
                    A(lambda h=h, cf=cf: S.op('dve', lambda e: e.max(out=tops[:, h, 0:8], in_=cf), reads=['cand'], writes=[f'tops{h}a']))
                    A(lambda h=h, cf=cf: S.op('dve', lambda e: e.match_replace(out=crep[:], in_to_replace=tops[:, h, 0:8], in_values=cf, imm_value=NEG),
                                              reads=['cand', f'tops{h}a'], writes=['crep']))
                    A(lambda h=h: S.op('dve', lambda e: e.max(out=tops[:, h, 8:16], in_=crep[:]), reads=['crep'], writes=[f'tops{h}b']))
                    A(lambda h=h, cf=cf: S.op('dve', lambda e: e.max_index(out=pos[:, h, 0:8], in_max=tops[:, h, 0:8], in_values=cf),
                                              reads=['cand', f'tops{h}a'], writes=[f'pos{h}a']))
                    A(lambda h=h: S.op('dve', lambda e: e.max_index(out=pos[:, h, 8:16], in_max=tops[:, h, 8:16], in_values=crep[:]),
                                       reads=['crep', f'tops{h}b'], writes=[f'pos{h}b']))
                tall = [f'tops{h}{x}' for h in range(8) for x in 'ab']
                pall = [f'pos{h}{x}' for h in range(8) for x in 'ab']
                pf = pos[:].rearrange("p h k -> p (h k)")
                A(lambda: S.op('dve', lambda e: e.tensor_single_scalar(pab[:, 0, :], pf, 4, ALU.logical_shift_right), reads=pall, writes=['pab0']))
                A(lambda: S.op('dve', lambda e: e.tensor_single_scalar(pab[:, 1, :], pf, 15, ALU.bitwise_and), reads=pall, writes=['pab1']))
                A(lambda: S.op('dve', lambda e: e.tensor_copy(pabf[:], pab[:]), reads=['pab0', 'pab1'], writes=['pabf']))
                for ab in range(2):
                    A(lambda ab=ab: S.op('dve', lambda e: e.tensor_tensor(oh[:], pabf[:, ab, :].unsqueeze(2).to_broadcast([128, 128, 16]),
                                                                          iota16[:].unsqueeze(1).to_broadcast([128, 128, 16]), ALU.is_equal),
                                         reads=['pabf'], writes=['oh']))
                    A(lambda ab=ab: S.op('dve', lambda e: e.tensor_tensor(oh[:].rearrange("p (h k) a -> p h k a", h=8),
                                                                          oh[:].rearrange("p (h k) a -> p h k a", h=8),
                                                                          idf[:, :, ab, :].unsqueeze(2).to_broadcast([128, 8, 16, 16]), ALU.mult),
                                         reads=['oh', 'idf'], writes=['oh']))
                    A(lambda ab=ab: S.op('dve', lambda e: e.tensor_reduce(isel[:, ab, :], oh[:], AX.X, ALU.add), reads=['oh'], writes=[f'isel{ab}']))
                A(lambda: S.op('dve', lambda e: e.scalar_tensor_tensor(eidf[:], isel[:, 0, :], 128.0, isel[:, 1, :], ALU.mult, ALU.add),
                               reads=['isel0', 'isel1'], writes=['eidf']))
                A(lambda: S.op('dve', lambda e: e.tensor_copy(EI[:], eidf[:]), reads=['eidf'], writes=[EIn]))
                A(lambda: S.op('dve', lambda e: e.tensor_tensor(ex[:], tops[:], tops[:, :, 0:1].to_broadcast([128, 8, 16]), ALU.subtract),
                               reads=tall, writes=['ex']))
                A(lambda: S.op('act', lambda e: e.activation(ex[:], ex[:], AF.Exp), reads=['ex'], writes=['ex']))
                A(lambda: S.op('dve', lambda e: e.tensor_reduce(sm[:, 0, :], ex[:], AX.X, ALU.add), reads=['ex'], writes=['sm0']))
                A(lambda: S.op('dve', lambda e: e.reciprocal(sm[:, 1, :], sm[:, 0, :]), reads=['sm0'], writes=['sm1']))
                A(lambda: S.op('dve', lambda e: e.tensor_tensor(GT[:], ex[:], sm[:, 1, :].unsqueeze(2).to_broadcast([128, 8, 16]), ALU.mult),
                               reads=['ex', 'sm1'], writes=[GTn]))
                return ops

            pending = front_ops(0)
            for op_ in pending:
                op_()
            gcnt = 0
            dcnt = 0
            for t in range(ntile):
                r0 = t * 128
                i2 = t % 2
                hh_, hn = h2[i2], f'h2_{i2}'
                EI, EIn = EIDX[i2], f'EIDX{i2}'
                GT, GTn = GATE[i2], f'GATE{i2}'
                nxt = front_ops(t + 1) if t + 1 < ntile else []
                ni = 0
                per = (len(nxt) + 127) // 128 if nxt else 0
                GTf = GT[:].rearrange("p h k -> p (h k)")
                for j in range(128):
                    g_ = gcnt % NG
                    gcnt += 1
                    d_ = dcnt % ND
                    dcnt += 1
                    if 'nog' not in ABL:
                        S.raw_dma('pool', lambda e, g_=g_, j=j: e.indirect_dma_start(
                            out=gb[g_][:], out_offset=None, in_=uvb_d,
                            in_offset=bass.IndirectOffsetOnAxis(ap=EI[:, j:j + 1], axis=0)), reads=[EIn], writes=[f'gb{g_}'])
                    if 'nod' not in ABL:
                        S.op('dve', lambda e, g_=g_, j=j: e.scalar_tensor_tensor(jk2[j % 4][:], gb[g_][:, 0:D], 1.0, hh_[:], ALU.mult, ALU.mult,
                                                                                 accum_out=actp[:, j:j + 1]),
                             reads=[f'gb{g_}', hn], writes=[f'jk2{j % 4}', f'actp{j % 4}'])
                    S.op('act', lambda e, j=j: e.activation(gl[:, j:j + 1], actp[:, j:j + 1], AF.Gelu), reads=[f'actp{j % 4}'], writes=[f'gl{j % 4}'])
                    S.op('act', lambda e, j=j: e.mul(coef[:, j:j + 1], gl[:, j:j + 1], GTf[:, j:j + 1]), reads=[f'gl{j % 4}', GTn], writes=[f'coef{j % 4}'])
                    S.op('act', lambda e, d_=d_, j=j: e.mul(Dg[d_][:], ident[:], coef[:, j:j + 1]), reads=[f'coef{j % 4}'], writes=[f'Dg{d_}'])
                    S.mm([lambda e, d_=d_, g_=g_, j=j: e.matmul(pacc[:, 0:512], Dg[d_][:], gb[g_][:, D:D + 512], start=(j == 0), stop=(j == 127)),
                          lambda e, d_=d_, g_=g_, j=j: e.matmul(pacc[:, 512:1024], Dg[d_][:], gb[g_][:, D + 512:2 * D], start=(j == 0), stop=(j == 127))],
                         reads=[f'Dg{d_}', f'gb{g_}'], writes=['pacc'])
                    for _ in range(per):
                        if ni < len(nxt):
                            nxt[ni]()
                            ni += 1
                while ni < len(nxt):
                    nxt[ni]()
                    ni += 1
                S.op('dve', lambda e: e.tensor_tensor(xo[:], pacc[:], G2[:], ALU.mult), reads=['pacc'], writes=['xo'])
                S.op('pool', lambda e: e.tensor_tensor(xo[:], xo[:], xlp[i2][:], ALU.add), reads=['xo', f'xlp{i2}'], writes=['xo'])
                rms_rstd(rs, 'rsp', xo, 'xo', jk3)
                S.op('dve', lambda e: e.scalar_tensor_tensor(ot[i2][:], xo[:], rs[:, 3:4], FN[:], ALU.mult, ALU.mult),
                     reads=['xo', 'rspd'], writes=[f'ot{i2}'])
                S.dma('sp', out_ap[r0:r0 + 128, :], ot[i2][:], reads=[f'ot{i2}'], writes=['out'])
            S.barrier()

        S.barrier()
        print("ops", S.nops, "waits", S.nwaits, flush=True)
    return nc


def make_in_maps(inputs, consts):
    maps = []
    shared = {}
    for k in INPUT_NAMES:
        if k in ('x', 'c', 'ctx'):
            continue
        a = np.asarray(inputs[k], dtype=np.float32)
        if k in ('c_ctx', 'final_norm'):
            a = a.reshape(1, D)
        elif a.shape[0] == 1:
            a = a[0]
        a = a.reshape(PER_CORE_SHAPES[k])
        shared[k] = np.ascontiguousarray(a)
    for k, v in consts.items():
        shared["k_" + k] = v
    for b in range(NCORES):
        m = dict(shared)
        m['x'] = np.ascontiguousarray(inputs['x'][b])
        m['c'] = np.ascontiguousarray(inputs['c'][b:b + 1])
        m['ctx'] = np.ascontiguousarray(inputs['ctx'][b])
        maps.append(m)
    return maps


def kernel(**inputs):
    consts = make_consts()
    nc = build_nc(consts)
    maps = make_in_maps(inputs, consts)
    res = run_bass_kernel_spmd(nc, maps, core_ids=list(range(NCORES)))
    return np.stack([r["out"] for r in res.results], axis=0).astype(np.float32)
```

```python
import math
import numpy as np
import ml_dtypes
from contextlib import ExitStack
import concourse.bass as bass
import concourse.mybir as mybir
from concourse.bass_utils import run_bass_kernel_spmd

F32 = mybir.dt.float32
BF16 = mybir.dt.bfloat16
I32 = mybir.dt.int32
U32 = mybir.dt.uint32
ALU = mybir.AluOpType
AF = mybir.ActivationFunctionType
AX = mybir.AxisListType

D = 1024
L = 8192
NCORES = 8
CTX = 256
EPS = 1e-6
PW = 8192
NT = L // 128
NB = L // 512


class Sched:
    SEM_LIMIT = 30000

    def __init__(self, nc, es, n_dma_sems=40):
        self.nc, self.es = nc, es
        self.engs = {'pe': nc.tensor, 'act': nc.scalar, 'dve': nc.vector,
                     'pool': nc.gpsimd, 'sp': nc.sync}
        self.sems = []
        self.sem_owner = []
        self.cur = {}
        for e in self.engs:
            self._new_sem(e)
        self.dma_ids = []
        for i in range(n_dma_sems):
            self.dma_ids.append(self._alloc(f"dq{i}", 'dma'))
        self.dma_cnt = [0] * n_dma_sems
        self.dma_rr = 0
        self.known = {e: {} for e in self.engs}
        self.lastw = {}
        self.readers = {}
        self.nwaits = 0
        self.nops = 0

    def _alloc(self, name, owner):
        s = self.es.enter_context(self.nc.semaphore(name))
        self.sems.append(s)
        self.sem_owner.append(owner)
        return len(self.sems) - 1

    def _new_sem(self, e):
        sid = self._alloc(f"e_{e}_{len(self.sems)}", e)
        self.cur[e] = [sid, 0]

    def _deps(self, reads, writes):
        deps = []
        for r in reads:
            t = self.lastw.get(r)
            if t is not None:
                deps.append(t)
        for w in writes:
            t = self.lastw.get(w)
            if t is not None:
                deps.append(t)
            deps.extend(self.readers.get(w, ()))
        return deps

    def _wait(self, eng, deps):
        need = {}
        kn = self.known[eng]
        for (s, v) in deps:
            if eng == 'pe' and self.sem_owner[s] == 'pe':
                continue
            if kn.get(s, 0) >= v:
                continue
            if need.get(s, 0) < v:
                need[s] = v
        for s, v in need.items():
            self.engs[eng].wait_ge(self.sems[s], v)
            kn[s] = v
            self.nwaits += 1

    def _commit(self, tok, reads, writes):
        for r in reads:
            self.readers.setdefault(r, []).append(tok)
        for w in writes:
            self.lastw[w] = tok
            self.readers[w] = []

    def op(self, eng, fn, reads=(), writes=()):
        self._wait(eng, self._deps(reads, writes))
        if self.cur[eng][1] >= self.SEM_LIMIT:
            self._new_sem(eng)
        inst = fn(self.engs[eng])
        s, c = self.cur[eng]
        inst.then_inc(self.sems[s], 1)
        c += 1
        self.cur[eng][1] = c
        tok = (s, c)
        self._commit(tok, reads, writes)
        self.nops += 1
        return tok

    def mm(self, fns, reads=(), writes=()):
        self._wait('pe', self._deps(reads, writes))
        if self.cur['pe'][1] >= self.SEM_LIMIT:
            self._new_sem('pe')
        inst = None
        for fn in fns:
            inst = fn(self.engs['pe'])
        s, c = self.cur['pe']
        inst.then_inc(self.sems[s], 1)
        c += 1
        self.cur['pe'][1] = c
        tok = (s, c)
        self._commit(tok, reads, writes)
        self.nops += len(fns)
        return tok

    def _dma_common(self, q, emit, reads, writes):
        deps = self._deps(reads, writes)
        i = self.dma_rr
        self.dma_rr = (i + 1) % len(self.dma_ids)
        s = self.dma_ids[i]
        prev = self.dma_cnt[i]
        if prev > 0:
            deps.append((s, prev))
        self._wait(q, deps)
        inst = emit(self.engs[q])
        inst.then_inc(self.sems[s], 16)
        self.dma_cnt[i] = prev + 16
        tok = (s, prev + 16)
        self._commit(tok, reads, writes)
        self.nops += 1
        return tok

    def dma(self, q, out, in_, reads=(), writes=(), **kw):
        return self._dma_common(q, lambda e: e.dma_start(out=out, in_=in_, **kw), reads, writes)

    def raw_dma(self, q, fn, reads=(), writes=()):
        return self._dma_common(q, fn, reads, writes)

    def barrier(self):
        toks = []
        for e in self.engs:
            s, c = self.cur[e]
            if c > 0:
                toks.append((e, s, c))
        for e in self.engs:
            kn = self.known[e]
            for (o, s, c) in toks:
                if o == e and e == 'pe':
                    continue
                if kn.get(s, 0) < c:
                    self.engs[e].wait_ge(self.sems[s], c)
                    kn[s] = c
            for i, s in enumerate(self.dma_ids):
                v = self.dma_cnt[i]
                if v > 0 and kn.get(s, 0) < v:
                    self.engs[e].wait_ge(self.sems[s], v)
                    kn[s] = v
        self.lastw = {}
        self.readers = {}


def _bf(a):
    return np.ascontiguousarray(a.astype(np.float32)).astype(ml_dtypes.bfloat16)


def make_consts():
    c = {}
    c["ident_bf"] = _bf(np.eye(128))
    c["ident_f"] = np.eye(128, dtype=np.float32)
    a = np.arange(128)
    ang = -2.0 * np.pi * np.outer(a, a) / 128.0
    fre, fim = np.cos(ang), np.sin(ang)
    c["fc1"] = _bf(np.concatenate([fre, fim], 1))
    c["fcj1"] = _bf(np.concatenate([fre, -fim], 1))
    c["fcj2"] = _bf(np.concatenate([fim, fre], 1))
    c["f3"] = _bf(np.stack([fre, fim, -fim, -fre], 0).transpose(1, 0, 2).reshape(128, 512))
    c["fcj1n"] = _bf(np.concatenate([-fre, fim], 1))
    angw = -2.0 * np.pi * np.outer(a, a) / 16384.0
    c["tw"] = np.concatenate([np.cos(angw), np.sin(angw)], 1).astype(np.float32)
    t = np.arange(L)
    rows = (t // 64).astype(np.float32)
    cols = (t % 64).astype(np.float32)
    nf = 32
    inv = (10000.0 ** (-np.arange(nf, dtype=np.float32) / nf)).astype(np.float32)
    cosT = np.zeros((128, L), np.float32)
    sinT = np.zeros((128, L), np.float32)
    for d in range(128):
        pos = rows if d < 64 else cols
        angd = (pos * inv[d % 32]).astype(np.float32)
        cosT[d] = np.cos(angd)
        sinT[d] = np.sin(angd) * (-1.0 if (d % 64) < 32 else 1.0)
    c["rot"] = np.ascontiguousarray(np.stack([cosT, sinT], 1))
    i = np.arange(128, dtype=np.float32)
    jj, ii = np.meshgrid(i, i, indexing="ij")
    ret = np.zeros((128, 6, 128), np.float32)
    ret[:, 0] = np.maximum(ii - jj, 0)
    ret[:, 1] = np.maximum(jj - ii, 0)
    ret[:, 2] = (ii >= jj)
    ret[:, 3] = (jj >= ii)
    ret[:, 4] = (ii + 1.0)
    ret[:, 5] = (128.0 - ii)
    c["rett"] = ret
    colc = np.zeros((128, 8), np.float32)
    colc[:, 0] = 127.0 - i
    colc[:, 1] = i
    colc[:, 2] = 255.0 - i
    colc[:, 3] = 127.0 - i
    colc[:, 4] = i
    colc[:, 5] = 128.0 + i
    colc[:, 6] = 128.0
    c["colc"] = colc
    c["iota16"] = np.tile(np.arange(16, dtype=np.float32)[None, :], (128, 1))
    n = np.arange(2 * L)
    lag = np.where(n < L, n, 2 * L - n).astype(np.int64)
    lag[L] = 0
    tl = np.linspace(0.0, 1.0, L, dtype=np.float32)
    bands = np.linspace(1e-4, 15.0, 16, dtype=np.float32)[None, :]
    w = ((2.0 * math.pi / L) * np.arange(L, dtype=np.float32))[:, None].astype(np.float32)
    zc = np.cos((bands * w).astype(np.float32)).astype(np.float32)
    zs = -np.sin((bands * w).astype(np.float32)).astype(np.float32)
    zf = np.concatenate([zc, zs, tl[:, None]], 1)
    c["zfeat"] = np.ascontiguousarray(zf[lag].T.astype(np.float32))
    lw = tl[lag].astype(np.float32)
    lw[L] = 1e9
    c["lagw"] = lw[None, :].copy()
    return c


CONST_DT = {"ident_bf": BF16, "fc1": BF16, "fcj1": BF16, "fcj2": BF16, "f3": BF16, "fcj1n": BF16}

INPUT_NAMES = ['x', 'c', 'ctx', 'c_ctx', 'w_ada', 'b_ada', 'norm1', 'norm2', 'w_in', 'hy_conv_w', 'hy_conv_b',
               'hy_fw1', 'hy_fb1', 'hy_fw2', 'hy_fb2', 'hy_fw3', 'hy_fb3', 'hy_fw4', 'hy_sin_freq',
               'hy_deltas', 'hy_bias', 'ret_log_decay_f', 'ret_log_decay_b', 'w_hy_out', 'w_ret_out',
               'w_o', 'peer_w_query', 'peer_sub_keys', 'peer_u', 'peer_v', 'final_norm']

PER_CORE_SHAPES = {
    'x': [L, D], 'c': [1, D], 'ctx': [CTX, D], 'c_ctx': [1, D], 'w_ada': [D, 6 * D], 'b_ada': [1, 6 * D],
    'norm1': [1, D], 'norm2': [1, D], 'w_in': [D, PW], 'hy_conv_w': [3, 3 * D], 'hy_conv_b': [1, 3 * D],
    'hy_fw1': [33, 64], 'hy_fb1': [1, 64], 'hy_fw2': [64, 64], 'hy_fb2': [1, 64], 'hy_fw3': [64, 64],
    'hy_fb3': [1, 64], 'hy_fw4': [64, 2 * D], 'hy_sin_freq': [1, 64], 'hy_deltas': [1, D], 'hy_bias': [1, D],
    'ret_log_decay_f': [1, 4], 'ret_log_decay_b': [1, 4], 'w_hy_out': [D, D], 'w_ret_out': [D, D],
    'w_o': [D, D], 'peer_w_query': [D, D], 'peer_sub_keys': [8, 2, 128, 64], 'peer_u': [16384, D],
    'peer_v': [16384, D], 'final_norm': [1, D],
}


def build_nc(consts, stop_after=None, dbg=()):
    nc = bass.Bass("TRN2", target_bir_lowering=False)
    I = {}
    for nme in INPUT_NAMES:
        I[nme] = nc.dram_tensor(nme, PER_CORE_SHAPES[nme], F32, kind="ExternalInput").ap()
    C = {}
    for nme, arr in consts.items():
        C[nme] = nc.dram_tensor("k_" + nme, list(arr.shape), CONST_DT.get(nme, F32), kind="ExternalInput").ap()
    out_ap = nc.dram_tensor("out", [L, D], F32, kind="ExternalOutput").ap()

    def scratch(nme, shape, dt):
        kind = "ExternalOutput" if nme in dbg else "Internal"
        return nc.dram_tensor("s_" + nme, shape, dt, kind=kind).ap()

    zhy = scratch("zhy", [3 * D, L], BF16)
    qT_d = scratch("qT", [512, L], BF16)
    kT_d = scratch("kT", [512, L], BF16)
    v_d = scratch("v", [L, D], BF16)
    gs_d = scratch("gs", [L, D], BF16)
    ahT_d = scratch("ahT", [D, L], BF16)
    arT_d = scratch("arT", [D, L], BF16)
    st0_d = scratch("st0", [128, 2, 4, 256], F32)
    mod_d = scratch("modbc", [7, D], F32)
    ctxmod_d = nc.dram_tensor("s_ctxmod", [2, 128, D], F32).ap()

    with ExitStack() as es:
        S = Sched(nc, es)

        uid = {'n': 0}

        def sb(st, name, shape, dt=F32):
            uid['n'] += 1
            return st.enter_context(nc.sbuf_tensor(f"{name}_{uid['n']}", shape, dt))

        def ps(st, name, shape, dt=F32):
            uid['n'] += 1
            return st.enter_context(nc.psum_tensor(f"{name}_{uid['n']}", shape, dt))

        ident = sb(es, "ident", [128, 128], BF16)
        identf = sb(es, "identf", [128, 128], F32)
        S.dma('sp', ident[:], C["ident_bf"], writes=['ident'])
        S.dma('sp', identf[:], C["ident_f"], writes=['identf'])

        rr = {'i': 0}

        def evac_eng():
            rr['i'] += 1
            return ('act', 'dve')[rr['i'] % 2]

        def copy_op(eng, out, in_, reads, writes, scale=None):
            if eng == 'act':
                if scale is None:
                    return S.op('act', lambda e: e.copy(out, in_), reads, writes)
                return S.op('act', lambda e: e.mul(out, in_, scale), reads, writes)
            if scale is None:
                return S.op(eng, lambda e: e.tensor_copy(out, in_), reads, writes)
            return S.op(eng, lambda e: e.tensor_scalar(out, in_, scale, None, ALU.mult), reads, writes)

        with ExitStack() as p1:
            with ExitStack() as p0:
                A1 = sb(p0, "A1", [128, D]); SH1 = sb(p0, "SH1", [128, D]); G1 = sb(p0, "G1", [128, D])
                A2 = sb(p0, "A2", [128, D]); SH2 = sb(p0, "SH2", [128, D]); G2 = sb(p0, "G2", [128, D])
                FN = sb(p0, "FN", [128, D])
                cc = sb(p0, "cc", [128, 2, 8]); scs = sb(p0, "scs", [128, 2, 8])
                screp = sb(p0, "screp", [128, 2, 8, 128])
                wst = [sb(p0, f"wst{i}", [128, 8, 512]) for i in range(2)]
                brow = sb(p0, "brow", [1, 6 * D]); ones1 = sb(p0, "ones1", [1, 128])
                MODL = sb(p0, "MODL", [128, 6 * D]); MODC = sb(p0, "MODC", [128, 2 * D])
                nrm = sb(p0, "nrm", [128, 3, D])
                CA1 = sb(p0, "CA1", [128, D]); CSH1 = sb(p0, "CSH1", [128, D])
                pm = [ps(p0, f"pm{i}", [128, 512]) for i in range(4)]
                S.dma('sp', cc[:, 0, :], I['c'].rearrange("o (k p) -> p (o k)", p=128), writes=['cc'], allow_slow_non_contiguous=True)
                S.dma('sp', cc[:, 1, :], I['c_ctx'].rearrange("o (k p) -> p (o k)", p=128), writes=['cc'], allow_slow_non_contiguous=True)
                S.dma('sp', brow[:], I['b_ada'], writes=['brow'])
                S.dma('sp', nrm[:, 0, :], I['norm1'].to_broadcast([128, D]), writes=['nrm0'])
                S.dma('sp', nrm[:, 1, :], I['norm2'].to_broadcast([128, D]), writes=['nrm1'])
                S.dma('sp', FN[:], I['final_norm'].to_broadcast([128, D]), writes=['FN'])
                S.op('pool', lambda e: e.memset(ones1[:], 1.0), writes=['ones1'])
                S.op('act', lambda e: e.activation(scs[:], cc[:], AF.Silu), reads=['cc'], writes=['scs'])
                S.op('dve', lambda e: e.tensor_copy(screp[:], scs[:].unsqueeze(3).to_broadcast([128, 2, 8, 128])),
                     reads=['scs'], writes=['screp'])
                for cb in range(12):
                    w = wst[cb % 2]
                    wn = f"wst{cb % 2}"
                    S.dma('sp', w[:], I['w_ada'][:, cb * 512:(cb + 1) * 512].rearrange("(k p) n -> p k n", p=128),
                          writes=[wn])
                    srcs = (0, 1) if cb < 4 else (0,)
                    for si in srcs:
                        pt = pm[(cb * 2 + si) % 4]
                        pn = f"pm{(cb * 2 + si) % 4}"
                        fns = [(lambda e, k=k, si=si, pt=pt, w=w: e.matmul(pt[:], screp[:, si, k, :], w[:, k, :],
                                                                             start=(k == 0), stop=False))
                               for k in range(8)]
                        fns.append(lambda e, pt=pt, cb=cb: e.matmul(pt[:], ones1[:], brow[:, cb * 512:(cb + 1) * 512],
                                                                    start=False, stop=True))
                        S.mm(fns, reads=[wn, 'screp', 'ones1', 'brow'], writes=[pn])
                        dst = MODL[:, cb * 512:(cb + 1) * 512] if si == 0 else MODC[:, cb * 512:(cb + 1) * 512]
                        copy_op(evac_eng(), dst, pt[:], [pn], [('MODL' if si == 0 else 'MODC') + str(cb)])
                modl_all = ['MODL' + str(i) for i in range(12)]
                modc_all = ['MODC' + str(i) for i in range(4)]
                S.op('dve', lambda e: e.scalar_tensor_tensor(A1[:], MODL[:, D:2 * D], 1.0, nrm[:, 0, :], ALU.add, ALU.mult),
                     reads=modl_all + ['nrm0'], writes=['A1'])
                S.op('dve', lambda e: e.scalar_tensor_tensor(A2[:], MODL[:, 4 * D:5 * D], 1.0, nrm[:, 1, :], ALU.add, ALU.mult),
                     reads=modl_all + ['nrm1'], writes=['A2'])
                S.op('dve', lambda e: e.scalar_tensor_tensor(CA1[:], MODC[:, D:2 * D], 1.0, nrm[:, 0, :], ALU.add, ALU.mult),
                     reads=modc_all + ['nrm0'], writes=['CA1'])
                S.op('act', lambda e: e.copy(SH1[:], MODL[:, 0:D]), reads=modl_all, writes=['SH1'])
                S.op('act', lambda e: e.copy(G1[:], MODL[:, 2 * D:3 * D]), reads=modl_all, writes=['G1'])
                S.op('act', lambda e: e.copy(SH2[:], MODL[:, 3 * D:4 * D]), reads=modl_all, writes=['SH2'])
                S.op('act', lambda e: e.copy(G2[:], MODL[:, 5 * D:6 * D]), reads=modl_all, writes=['G2'])
                S.op('act', lambda e: e.copy(CSH1[:], MODC[:, 0:D]), reads=modc_all, writes=['CSH1'])
                if True:
                    for i, (t, nme) in enumerate([(A1, 'A1'), (SH1, 'SH1'), (G1, 'G1'), (A2, 'A2'), (SH2, 'SH2'), (G2, 'G2'), (FN, 'FN')]):
                        S.dma('sp', mod_d[i:i + 1, :], t[0:1, :], reads=[nme], writes=['mod_d'])
                S.dma('sp', ctxmod_d[0], CA1[:], reads=['CA1'], writes=['ctxmod'])
                S.dma('sp', ctxmod_d[1], CSH1[:], reads=['CSH1'], writes=['ctxmod'])
                S.barrier()
            W1 = sb(p1, "W1", [128, 8, 9216], BF16)
            with ExitStack() as p0:
                stg = [sb(p0, f"stg{i}", [128, 2048]) for i in range(3)]
                kscale = 128.0 ** -0.5
                n = 0
                for k in range(8):
                    for cb in range(4):
                        st = stg[n % 3]
                        sn = f"stg{n % 3}"
                        S.dma('sp', st[:], I['w_in'][k * 128:(k + 1) * 128, cb * 2048:(cb + 1) * 2048], writes=[sn])
                        eng = ('act', 'dve', 'pool')[n % 3]
                        if cb == 1:
                            copy_op(eng, W1[:, k, 2048:3584], st[:, 0:1536], [sn], [f"W1_{k}_{cb}a"])
                            copy_op(eng, W1[:, k, 3584:4096], st[:, 1536:2048], [sn], [f"W1_{k}_{cb}b"], scale=kscale)
                        else:
                            copy_op(eng, W1[:, k, cb * 2048:(cb + 1) * 2048], st[:], [sn], [f"W1_{k}_{cb}"])
                        n += 1
                S.barrier()
                srcv = W1[:, :, 3072:4096].rearrange("p k (b s i) -> p k b s i", s=2, i=32)
                dstv = W1[:, :, 8192:9216].rearrange("p k (b s i) -> p k b s i", s=2, i=32)
                for k in range(8):
                    S.op('dve', lambda e, k=k: e.tensor_copy(dstv[:, k, :, 0, :], srcv[:, k, :, 1, :]), writes=[f'W1sw{k}a'])
                    S.op('pool', lambda e, k=k: e.tensor_copy(dstv[:, k, :, 1, :], srcv[:, k, :, 0, :]), writes=[f'W1sw{k}b'])
                S.barrier()

            xin = [sb(p1, f"xin{i}", [128, D]) for i in range(2)]
            junk = sb(p1, "junk", [128, D], BF16)
            htmp = sb(p1, "htmp", [128, D])
            hb = [sb(p1, f"hb{i}", [128, D], BF16) for i in range(2)]
            hT = [sb(p1, f"hT{i}", [128, 8, 512], BF16) for i in range(2)]
            stat = sb(p1, "stat", [128, 8])
            A1 = sb(p1, "A1", [128, D]); SH1 = sb(p1, "SH1", [128, D])
            S.dma('sp', A1[:], ctxmod_d[0], writes=['A1'])
            S.dma('sp', SH1[:], ctxmod_d[1], writes=['SH1'])
            pF = [ps(p1, f"pF{i}", [128, 512]) for i in range(6)]
            pT = [ps(p1, f"pT{i}", [128, 8, 128], BF16) for i in range(2)]
            cnt = {'x': 0, 'pF': 0, 'ob': 0, 'pT': 0, 'rt': 0}

            def norm_T(src_rows, Abc, SHbc, an, shn, hTt, hTn, col):
                if isinstance(src_rows, tuple):
                    xi = src_rows[0]
                    xt, xn = xin[xi], f"xin{xi}"
                else:
                    xi = cnt['x'] % 2
                    cnt['x'] += 1
                    xt, xn = xin[xi], f"xin{xi}"
                    S.dma('sp', xt[:], src_rows, writes=[xn])
                S.op('act', lambda e: e.activation(junk[:], xt[:], AF.Square, accum_out=stat[:, 0:1]),
                     reads=[xn], writes=['junk', 'stat0'])
                S.op('dve', lambda e: e.tensor_scalar(stat[:, 1:2], stat[:, 0:1], 1.0 / D, EPS, ALU.mult, ALU.add),
                     reads=['stat0'], writes=['stat1'])
                S.op('act', lambda e: e.sqrt(stat[:, 2:3], stat[:, 1:2]), reads=['stat1'], writes=['stat2'])
                S.op('dve', lambda e: e.reciprocal(stat[:, 3:4], stat[:, 2:3]), reads=['stat2'], writes=['stat3'])
                S.op('dve', lambda e: e.scalar_tensor_tensor(htmp[:], xt[:], stat[:, 3:4], Abc[:], ALU.mult, ALU.mult),
                     reads=[xn, 'stat3', an], writes=['htmp'])
                hbt, hbn = hb[xi], f"hb{xi}"
                S.op('pool', lambda e: e.tensor_tensor(hbt[:], htmp[:], SHbc[:], ALU.add),
                     reads=['htmp', shn], writes=[hbn])
                pi = cnt['pT'] % 2
                cnt['pT'] += 1
                S.mm([(lambda e, k=k: e.transpose(pT[pi][:, k, :], hbt[:, k * 128:(k + 1) * 128], ident[:]))
                      for k in range(8)], reads=[hbn, 'ident'], writes=[f"pT{pi}"])
                copy_op(evac_eng(), hTt[:, :, col:col + 128], pT[pi][:], [f"pT{pi}"], [hTn + f"_{col}"])

            def next_pF():
                i = cnt['pF'] % 6
                cnt['pF'] += 1
                return pF[i], f"pF{i}"

            def next_ob():
                i = cnt['ob'] % NOB
                cnt['ob'] += 1
                return ob[i], f"ob{i}"

            def proj_fm(hTt, hTreads, c0, pt, pn, ntok=512):
                S.mm([(lambda e, k=k: e.matmul(pt[:, 0:ntok], W1[:, k, c0:c0 + 128], hTt[:, k, 0:ntok],
                                               start=(k == 0), stop=(k == 7))) for k in range(8)],
                     reads=hTreads, writes=[pn])

            def proj_tm(hTt, hTreads, tcol, c0, pt, pn):
                S.mm([(lambda e, k=k: e.matmul(pt[:], hTt[:, k, tcol:tcol + 128], W1[:, k, c0:c0 + 512],
                                               start=(k == 0), stop=(k == 7))) for k in range(8)],
                     reads=hTreads, writes=[pn])

            with ExitStack() as pc:
                kc = sb(pc, "kc", [128, 2, 512], BF16)
                vcs = sb(pc, "vcs", [128, 2, 2, D], BF16)
                lgb = sb(pc, "lgb", [128, 2, 4])
                colc = sb(pc, "colc", [128, 8])
                wfb = sb(pc, "wfb", [128, 2, 2, 4])
                st0 = sb(pc, "st0", [128, 2, 4, 256])
                S.dma('sp', lgb[:, 0, :], I['ret_log_decay_f'].to_broadcast([128, 4]), writes=['lgb'])
                S.dma('sp', lgb[:, 1, :], I['ret_log_decay_b'].to_broadcast([128, 4]), writes=['lgb'])
                S.dma('sp', colc[:], C['colc'], writes=['colc'])
                for ti in range(2):
                    for di in range(2):
                        cidx = 2 + di * 2 + ti
                        S.op('act', lambda e, ti=ti, di=di, cidx=cidx: e.activation(
                            wfb[:, ti, di, :], lgb[:, di, :], AF.Exp, scale=colc[:, cidx:cidx + 1]),
                            reads=['lgb', 'colc'], writes=[f'wfb{ti}{di}'])
                hTc = hT[0]
                for ti in range(2):
                    norm_T(I['ctx'][ti * 128:(ti + 1) * 128, :], A1, SH1, 'A1', 'SH1', hTc, 'hTc', ti * 128)
                hreads = ['hTc_0', 'hTc_128']
                for ti in range(2):
                    pt, pn = next_pF()
                    proj_tm(hTc, hreads, ti * 128, 3584, pt, pn)
                    copy_op(evac_eng(), kc[:, ti, :], pt[:], [pn], [f'kc{ti}'])
                    for hf in range(2):
                        pt, pn = next_pF()
                        proj_tm(hTc, hreads, ti * 128, 4096 + hf * 512, pt, pn)
                        for di in range(2):
                            for hh in range(2):
                                h = hf * 2 + hh
                                S.op('dve', lambda e, ti=ti, di=di, h=h, hh=hh, pt=pt: e.tensor_scalar(
                                    vcs[:, ti, di, h * 256:(h + 1) * 256], pt[:, hh * 256:(hh + 1) * 256],
                                    wfb[:, ti, di, h:h + 1], None, ALU.mult),
                                    reads=[pn, f'wfb{ti}{di}'], writes=[f'vcs{ti}{di}{h}'])
                for di in range(2):
                    for h in range(4):
                        pt, pn = next_pF()
                        S.mm([(lambda e, ti=ti, pt=pt: e.matmul(pt[:, 0:256], kc[:, ti, h * 128:(h + 1) * 128],
                                                                 vcs[:, ti, di, h * 256:(h + 1) * 256],
                                                                 start=(ti == 0), stop=(ti == 1))) for ti in range(2)],
                             reads=['kc0', 'kc1'] + [f'vcs{ti}{di}{h}' for ti in range(2)], writes=[pn])
                        copy_op(evac_eng(), st0[:, di, h, :], pt[:, 0:256], [pn], [f'st0_{di}{h}'])
                S.dma('sp', st0_d, st0[:], reads=[f'st0_{di}{h}' for di in range(2) for h in range(4)])
                S.barrier()

            if stop_after == 'ctx':
                S.barrier()
                return nc

            S.dma('sp', A1[:], mod_d[0:1, :].to_broadcast([128, D]), writes=['A1'])
            S.dma('sp', SH1[:], mod_d[1:2, :].to_broadcast([128, D]), writes=['SH1'])
            rot = [sb(p1, f"rot{i}", [128, 2, 512]) for i in range(2)]
            NOB = 4
            ob = [sb(p1, f"ob{i}", [128, 512], BF16) for i in range(NOB)]
            rt = [sb(p1, f"rt{i}", [128, 512]) for i in range(2)]
            nblocks = NB if stop_after != 'p1small' else 2
            def xload(b, ti):
                xi = cnt['x'] % 2
                cnt['x'] += 1
                r0 = b * 512 + ti * 128
                S.dma('sp', xin[xi][:], I['x'][r0:r0 + 128, :], writes=[f"xin{xi}"])
                return xi

            def norm_steps(b):
                st = {}

                def step(k):
                    if k == 0:
                        st[0] = xload(b, 0)
                    if k + 1 < 4:
                        st[k + 1] = xload(b, k + 1)
                    norm_T((st[k],), A1, SH1, 'A1', 'SH1', hT[b % 2], f"hT{b % 2}", k * 128)
                return [lambda k=k: step(k) for k in range(4)]

            S.dma('sp', rot[0][:], C['rot'][:, :, 0:512], writes=["rot0"])
            for th in norm_steps(0):
                th()
            for b in range(nblocks):
                hTt, hTn = hT[b % 2], f"hT{b % 2}"
                rtab, rn = rot[b % 2], f"rot{b % 2}"
                nsteps = norm_steps(b + 1) if b + 1 < nblocks else []
                if b + 1 < nblocks:
                    S.dma('sp', rot[(b + 1) % 2][:], C['rot'][:, :, (b + 1) * 512:(b + 2) * 512], writes=[f"rot{(b + 1) % 2}"])
                hreads = [hTn + f"_{c}" for c in (0, 128, 256, 384)]
                tsl = slice(b * 512, (b + 1) * 512)
                for cc_ in range(24):
                    pt, pn = next_pF()
                    proj_fm(hTt, hreads, cc_ * 128, pt, pn)
                    o, on = next_ob()
                    copy_op(evac_eng(), o[:], pt[:], [pn], [on])
                    S.dma('sp', zhy[cc_ * 128:(cc_ + 1) * 128, tsl], o[:], reads=[on])
                    if cc_ % 6 == 5 and nsteps:
                        nsteps[cc_ // 6]()
                for qk in range(2):
                    for h in range(4):
                        c0 = 3072 + qk * 512 + h * 128
                        c1 = 8192 + qk * 512 + h * 128
                        pa, pan = next_pF()
                        proj_fm(hTt, hreads, c0, pa, pan)
                        pb, pbn = next_pF()
                        proj_fm(hTt, hreads, c1, pb, pbn)
                        ta, tan = rt[0], "rt0"
                        tb, tbn = rt[1], "rt1"
                        S.op('dve', lambda e, ta=ta, pa=pa: e.tensor_tensor(ta[:], pa[:], rtab[:, 0, :], ALU.mult),
                             reads=[pan, rn], writes=[tan])
                        S.op('dve', lambda e, tb=tb, pb=pb: e.tensor_tensor(tb[:], pb[:], rtab[:, 1, :], ALU.mult),
                             reads=[pbn, rn], writes=[tbn])
                        o, on = next_ob()
                        S.op('dve', lambda e, o=o, ta=ta, tb=tb: e.tensor_tensor(o[:], ta[:], tb[:], ALU.add),
                             reads=[tan, tbn], writes=[on])
                        dst = (qT_d if qk == 0 else kT_d)[h * 128:(h + 1) * 128, tsl]
                        S.dma('sp', dst, o[:], reads=[on])
                for gi in range(16):
                    pt, pn = next_pF()
                    proj_fm(hTt, hreads, 6144 + gi * 128, pt, pn)
                    o, on = next_ob()
                    S.op('act', lambda e, o=o, pt=pt: e.activation(o[:], pt[:], AF.Sigmoid), reads=[pn], writes=[on])
                    dst = (ahT_d if gi < 8 else arT_d)[(gi % 8) * 128:(gi % 8 + 1) * 128, tsl]
                    S.dma('sp', dst, o[:], reads=[on])
                for ti in range(4):
                    r0 = b * 512 + ti * 128
                    for hf in range(2):
                        pt, pn = next_pF()
                        proj_tm(hTt, hreads, ti * 128, 4096 + hf * 512, pt, pn)
                        o, on = next_ob()
                        copy_op(evac_eng(), o[:], pt[:], [pn], [on])
                        S.dma('sp', v_d[r0:r0 + 128, hf * 512:(hf + 1) * 512], o[:], reads=[on])
                    for hf in range(2):
                        pt, pn = next_pF()
                        proj_tm(hTt, hreads, ti * 128, 5120 + hf * 512, pt, pn)
                        o, on = next_ob()
                        S.op('act', lambda e, o=o, pt=pt: e.activation(o[:], pt[:], AF.Silu), reads=[pn], writes=[on])
                        S.dma('sp', gs_d[r0:r0 + 128, hf * 512:(hf + 1) * 512], o[:], reads=[on])
            S.barrier()
        if stop_after in ('p1', 'p1small'):
            return nc

        def run_interleaved(chains, K):
            it = iter(chains)
            active = []
            done = False
            while True:
                while not done and len(active) < K:
                    try:
                        active.append([next(it), 0])
                    except StopIteration:
                        done = True
                if not active:
                    break
                for a in list(active):
                    a[0][a[1]]()
                    a[1] += 1
                    if a[1] >= len(a[0]):
                        active.remove(a)

        sb_d = scratch("sbst", [64, 128, D], BF16)
        uvb_d = scratch("uvb", [16384, 2 * D], BF16)
        yrT_d = scratch("yrT", [D, L], BF16)
        with ExitStack() as p2:
            rett = sb(p2, "rett", [128, 6, 128]); colc = sb(p2, "colc", [128, 8]); lgb = sb(p2, "lgb", [128, 2, 4])
            MT = sb(p2, "MT", [128, 4, 128]); XIF = sb(p2, "XIF", [128, 4, 128]); XIB = sb(p2, "XIB", [128, 4, 128])
            mtmp = sb(p2, "mtmp", [128, 2, 128])
            zeta = sb(p2, "zeta", [128, 2, 4]); gch = sb(p2, "gch", [128, 2, 4])
            St = sb(p2, "St", [128, 2, 4, 256])
            Sfb = sb(p2, "Sfb", [128, 4, 256], BF16)
            qTb = [sb(p2, f"qTb{i}", [128, 4, 512], BF16) for i in range(2)]
            kTb = [sb(p2, f"kTb{i}", [128, 4, 512], BF16) for i in range(2)]
            vb = [sb(p2, f"vb{i}", [128, 4, D], BF16) for i in range(2)]
            gsb = [sb(p2, f"gsb{i}", [128, 4, D], BF16) for i in range(2)]
            sbl = [sb(p2, f"sbl{i}", [128, 4, D], BF16) for i in range(2)]
            yst = [sb(p2, f"yst{i}", [128, 8, 512], BF16) for i in range(2)]
            ktok = [sb(p2, f"ktok{i}", [128, 128], BF16) for i in range(4)]
            vz = [sb(p2, f"vz{i}", [128, 256], BF16) for i in range(4)]
            PTt = [sb(p2, f"PT{i}", [128, 128], BF16) for i in range(4)]
            qfb = [sb(p2, f"qfb{i}", [128, 2, 128], BF16) for i in range(4)]
            yr = [sb(p2, f"yr{i}", [128, 256], BF16) for i in range(4)]
            junk2 = [sb(p2, f"junk2{i}", [128, 256], BF16) for i in range(4)]
            rst = [sb(p2, f"rst{i}", [128, 4]) for i in range(4)]
            cst = [sb(p2, f"cst{i}", [128, 4, D]) for i in range(2)]
            cbf = [sb(p2, f"cbf{i}", [128, 4, D], BF16) for i in range(2)]
            bkA = [ps(p2, f"bkA{i}", [128, 512]) for i in range(4)]
            bkB = [ps(p2, f"bkB{i}", [128, 512]) for i in range(4)]

            class _Slots:
                def __init__(self, f):
                    self.f = f

                def __getitem__(self, key):
                    return self.f(key[1])[(key[0],) + tuple(key[2:])]
            pK = _Slots(lambda i: bkA[i][:, 0:64].bitcast(BF16))
            pS = _Slots(lambda i: bkA[i][:, 64:192])
            pY = _Slots(lambda i: bkA[i][:, 192:320].bitcast(BF16).rearrange("p (a b) -> p a b", a=2))
            pO = _Slots(lambda i: bkB[i][:, 0:256])
            pD = _Slots(lambda i: bkB[i][:, 256:512])
            S.dma('sp', rett[:], C['rett'], writes=['rett'])
            S.dma('sp', colc[:], C['colc'], writes=['colc'])
            S.dma('sp', lgb[:, 0, :], I['ret_log_decay_f'].to_broadcast([128, 4]), writes=['lgb'])
            S.dma('sp', lgb[:, 1, :], I['ret_log_decay_b'].to_broadcast([128, 4]), writes=['lgb'])
            S.dma('sp', St[:], st0_d, writes=['St0', 'St1'])
            for h in range(4):
                S.op('act', lambda e, h=h: e.activation(mtmp[:, 0, :], rett[:, 0, :], AF.Exp, scale=lgb[:, 0, h:h + 1]),
                     reads=['rett', 'lgb'], writes=['mtmp0'])
                S.op('act', lambda e, h=h: e.activation(mtmp[:, 1, :], rett[:, 1, :], AF.Exp, scale=lgb[:, 1, h:h + 1]),
                     reads=['rett', 'lgb'], writes=['mtmp1'])
                S.op('dve', lambda e, h=h: e.tensor_tensor(mtmp[:, 0, :], mtmp[:, 0, :], rett[:, 2, :], ALU.mult),
                     reads=['mtmp0', 'rett'], writes=['mtmp0'])
                S.op('dve', lambda e, h=h: e.tensor_tensor(mtmp[:, 1, :], mtmp[:, 1, :], rett[:, 3, :], ALU.mult),
                     reads=['mtmp1', 'rett'], writes=['mtmp1'])
                S.op('dve', lambda e, h=h: e.tensor_tensor(MT[:, h, :], mtmp[:, 0, :], mtmp[:, 1, :], ALU.add),
                     reads=['mtmp0', 'mtmp1'], writes=['MT'])
                S.op('act', lambda e, h=h: e.activation(XIF[:, h, :], rett[:, 4, :], AF.Exp, scale=lgb[:, 0, h:h + 1]),
                     reads=['rett', 'lgb'], writes=['XIF'])
                S.op('act', lambda e, h=h: e.activation(XIB[:, h, :], rett[:, 5, :], AF.Exp, scale=lgb[:, 1, h:h + 1]),
                     reads=['rett', 'lgb'], writes=['XIB'])
            for di in range(2):
                S.op('act', lambda e, di=di: e.activation(zeta[:, di, :], lgb[:, di, :], AF.Exp, scale=colc[:, di:di + 1]),
                     reads=['lgb', 'colc'], writes=['zeta'])
                S.op('act', lambda e, di=di: e.activation(gch[:, di, :], lgb[:, di, :], AF.Exp, scale=128.0),
                     reads=['lgb'], writes=['gch'])
            S.barrier()
            nblk = NB if stop_after != 'retsmall' else 2
            kTv = kT_d.rearrange("(h d) t -> d h t", d=128)
            qTv = qT_d.rearrange("(h d) t -> d h t", d=128)

            def loadA(blk, bi):
                tsl = slice(blk * 512, (blk + 1) * 512)
                S.dma('sp', kTb[bi][:], kTv[:, :, tsl], writes=[f'kTb{bi}'])
                S.dma('sp', vb[bi][:], v_d[tsl, :].rearrange("(c j) f -> j c f", j=128), writes=[f'vb{bi}'])

            def loadB(blk, bi):
                tsl = slice(blk * 512, (blk + 1) * 512)
                S.dma('sp', kTb[bi][:], kTv[:, :, tsl], writes=[f'kTb{bi}'])
                S.dma('sp', qTb[bi][:], qTv[:, :, tsl], writes=[f'qTb{bi}'])
                S.dma('sp', vb[bi][:], v_d[tsl, :].rearrange("(c j) f -> j c f", j=128), writes=[f'vb{bi}'])
                S.dma('sp', gsb[bi][:], gs_d[tsl, :].rearrange("(c j) f -> j c f", j=128), writes=[f'gsb{bi}'])
                S.dma('sp', sbl[bi][:], sb_d[blk * 4:(blk + 1) * 4].rearrange("n d f -> d n f"), writes=[f'sbl{bi}'])

            def k_tok(A, s_, kt, ktn, h, c):
                A(lambda: S.mm([lambda e: e.transpose(pK[:, s_, :], kt[:, h, c * 128:(c + 1) * 128], ident[:])],
                               reads=[ktn, 'ident'], writes=[f'pK{s_}', f'bkA{s_}']))
                A(lambda: copy_op('act', ktok[s_][:], pK[:, s_, :], [f'pK{s_}'], [f'ktok{s_}', f'bkA{s_}']))

            def state_update(A, s_, di, h, vt, vtn, c):
                A(lambda: S.op('dve', lambda e: e.tensor_scalar(vz[s_][:], vt[:, c, h * 256:(h + 1) * 256], zeta[:, di, h:h + 1], None, ALU.mult),
                               reads=[vtn], writes=[f'vz{s_}']))
                A(lambda: S.mm([lambda e: e.matmul(pD[:, s_, :], ktok[s_][:], vz[s_][:], start=True, stop=True)],
                               reads=[f'ktok{s_}', f'vz{s_}'], writes=[f'pD{s_}', f'bkB{s_}']))
                A(lambda: S.op('dve', lambda e: e.scalar_tensor_tensor(St[:, di, h, :], St[:, di, h, :], gch[:, di, h:h + 1], pD[:, s_, :],
                                                                       ALU.mult, ALU.add),
                               reads=[f'pD{s_}', f'St{di}{h}'], writes=[f'St{di}{h}', f'bkB{s_}']))

            bdone = {}

            def chainsA():
                for bi_, blk in enumerate(range(nblk - 1, -1, -1)):
                    bi = bi_ % 2
                    kt, ktn = kTb[bi], f'kTb{bi}'
                    vt, vtn = vb[bi], f'vb{bi}'
                    sst, sstn = sbl[bi], f'sbl{bi}'
                    for c in range(3, -1, -1):
                        for h in range(4):
                            ops = []
                            A = ops.append
                            A(lambda sst=sst, sstn=sstn, c=c, h=h: copy_op('act', sst[:, c, h * 256:(h + 1) * 256], St[:, 1, h, :], [f'St1{h}'], [sstn + f'_{c}{h}']))
                            k_tok(A, h, kt, ktn, h, c)
                            state_update(A, h, 1, h, vt, vtn, c)

                            def fin(bi_=bi_, blk=blk, sst=sst, sstn=sstn):
                                bdone[bi_] = bdone.get(bi_, 0) + 1
                                if bdone[bi_] == 16:
                                    S.dma('sp', sb_d[blk * 4:(blk + 1) * 4].rearrange("n d f -> d n f"), sst[:],
                                          reads=[sstn + f'_{c2}{h2}' for c2 in range(4) for h2 in range(4)], writes=['sb_d'])
                                    if bi_ + 2 < nblk:
                                        loadA(nblk - 1 - (bi_ + 2), bi_ % 2)
                            A(fin)
                            yield ops

            loadA(nblk - 1, 0)
            if nblk > 1:
                loadA(nblk - 2, 1)
            import os as _os2
            KR = int(_os2.environ.get('RET_K', 4))
            run_interleaved(chainsA(), KR)
            S.barrier()
            if stop_after == 'retA':
                return nc
            for h in range(4):
                copy_op('act', Sfb[:, h, :], St[:, 0, h, :], [], [f'Sfb{h}'])
            bdone = {}

            def chainsB():
                for blk in range(nblk):
                    bb = blk % 2
                    kt, ktn = kTb[bb], f'kTb{bb}'
                    qt, qtn = qTb[bb], f'qTb{bb}'
                    vt, vtn = vb[bb], f'vb{bb}'
                    gt, gtn = gsb[bb], f'gsb{bb}'
                    sst, sstn = sbl[bb], f'sbl{bb}'
                    ys, ysn = yst[bb], f'yst{bb}'
                    for c in range(4):
                        csl = slice(c * 128, (c + 1) * 128)
                        for h in range(4):
                            ops = []
                            A = ops.append
                            s_ = h
                            k_tok(A, s_, kt, ktn, h, c)
                            A(lambda kt=kt, qt=qt, ktn=ktn, qtn=qtn, h=h, csl=csl, s_=s_: S.mm(
                                [lambda e: e.matmul(pS[:, s_, :], kt[:, h, csl], qt[:, h, csl], start=True, stop=True)],
                                reads=[ktn, qtn], writes=[f'pS{s_}', f'bkA{s_}']))
                            A(lambda h=h, s_=s_: S.op('dve', lambda e: e.tensor_tensor(PTt[s_][:], pS[:, s_, :], MT[:, h, :], ALU.mult),
                                                      reads=[f'pS{s_}'], writes=[f'PT{s_}', f'bkA{s_}']))
                            A(lambda qt=qt, qtn=qtn, h=h, csl=csl, s_=s_: S.op('pool', lambda e: e.tensor_tensor(qfb[s_][:, 0, :], qt[:, h, csl], XIF[:, h, :], ALU.mult),
                                                                               reads=[qtn], writes=[f'qf{s_}']))
                            A(lambda qt=qt, qtn=qtn, h=h, csl=csl, s_=s_: S.op('pool', lambda e: e.tensor_tensor(qfb[s_][:, 1, :], qt[:, h, csl], XIB[:, h, :], ALU.mult),
                                                                               reads=[qtn], writes=[f'qb{s_}']))
                            A(lambda vt=vt, vtn=vtn, sst=sst, sstn=sstn, h=h, c=c, s_=s_: S.mm(
                                [lambda e: e.matmul(pO[:, s_, :], PTt[s_][:], vt[:, c, h * 256:(h + 1) * 256], start=True, stop=False),
                                 lambda e: e.matmul(pO[:, s_, :], qfb[s_][:, 0, :], Sfb[:, h, :], start=False, stop=False),
                                 lambda e: e.matmul(pO[:, s_, :], qfb[s_][:, 1, :], sst[:, c, h * 256:(h + 1) * 256], start=False, stop=True)],
                                reads=[f'PT{s_}', vtn, f'qf{s_}', f'qb{s_}', f'Sfb{h}', sstn], writes=[f'pO{s_}', f'bkB{s_}']))
                            r = rst[s_]
                            rn = f'rst{s_}'
                            A(lambda r=r, rn=rn, s_=s_: S.op('act', lambda e: e.activation(junk2[s_][:], pO[:, s_, :], AF.Square, accum_out=r[:, 0:1]),
                                                             reads=[f'pO{s_}'], writes=[f'junk2{s_}', rn + 'a', f'bkB{s_}']))
                            A(lambda r=r, rn=rn: S.op('dve', lambda e: e.tensor_scalar(r[:, 1:2], r[:, 0:1], 1.0 / 256.0, EPS, ALU.mult, ALU.add),
                                                      reads=[rn + 'a'], writes=[rn + 'b']))
                            A(lambda r=r, rn=rn: S.op('act', lambda e: e.sqrt(r[:, 2:3], r[:, 1:2]), reads=[rn + 'b'], writes=[rn + 'c']))
                            A(lambda r=r, rn=rn: S.op('dve', lambda e: e.reciprocal(r[:, 3:4], r[:, 2:3]), reads=[rn + 'c'], writes=[rn + 'd']))
                            A(lambda r=r, rn=rn, gt=gt, gtn=gtn, h=h, c=c, s_=s_: S.op(
                                'dve', lambda e: e.scalar_tensor_tensor(yr[s_][:], pO[:, s_, :], r[:, 3:4], gt[:, c, h * 256:(h + 1) * 256], ALU.mult, ALU.mult),
                                reads=[f'pO{s_}', rn + 'd', gtn], writes=[f'yr{s_}', f'bkB{s_}']))
                            A(lambda s_=s_: S.mm([lambda e: e.transpose(pY[:, s_, 0, :], yr[s_][:, 0:128], ident[:]),
                                                  lambda e: e.transpose(pY[:, s_, 1, :], yr[s_][:, 128:256], ident[:])],
                                                 reads=[f'yr{s_}', 'ident'], writes=[f'pY{s_}', f'bkA{s_}']))
                            A(lambda ys=ys, ysn=ysn, h=h, c=c, csl=csl, s_=s_: copy_op('act', ys[:, h * 2:h * 2 + 2, csl], pY[:, s_, :, :], [f'pY{s_}'], [ysn + f'_{c}{h}', f'bkA{s_}']))
                            state_update(A, s_, 0, h, vt, vtn, c)
                            A(lambda h=h: copy_op('act', Sfb[:, h, :], St[:, 0, h, :], [f'St0{h}'], [f'Sfb{h}']))

                            def fin(blk=blk, ys=ys, ysn=ysn):
                                bdone[blk] = bdone.get(blk, 0) + 1
                                if bdone[blk] == 16:
                                    tsl = slice(blk * 512, (blk + 1) * 512)
                                    S.dma('sp', yrT_d.rearrange("(g f) t -> f g t", f=128)[:, :, tsl], ys[:],
                                          reads=[ysn + f'_{c2}{h2}' for c2 in range(4) for h2 in range(4)], writes=['yrT_d'])
                                    if blk + 2 < nblk:
                                        loadB(blk + 2, blk % 2)
                            A(fin)
                            yield ops

            cast_list = []
            for (src_, dst_) in ((I['peer_u'], uvb_d[:, 0:D]), (I['peer_v'], uvb_d[:, D:2 * D])):
                sv = src_.rearrange("(n g p) d -> n p g d", g=4, p=128)
                dv = dst_.rearrange("(n g p) d -> n p g d", g=4, p=128)
                for c in range(32):
                    cast_list.append((sv[c], dv[c]))

            def mixedB():
                for i, ch in enumerate(chainsB()):
                    n = i // 4
                    if n < len(cast_list):
                        sv_c, dv_c = cast_list[n]
                        bi = n % 2
                        if i % 4 == 0:
                            ch.insert(0, lambda bi=bi, sv_c=sv_c: S.dma('sp', cst[bi][:], sv_c, writes=[f'cst{bi}']))
                        if i % 4 == 3:
                            ch.insert(len(ch) - 1, lambda bi=bi, n=n: copy_op(('act', 'dve')[n % 2], cbf[bi][:], cst[bi][:], [f'cst{bi}'], [f'cbf{bi}']))
                            ch.insert(len(ch) - 1, lambda bi=bi, dv_c=dv_c: S.dma('sp', dv_c, cbf[bi][:], reads=[f'cbf{bi}'], writes=['tab']))
                    yield ch

            loadB(0, 0)
            if nblk > 1:
                loadB(1, 1)
            run_interleaved(mixedB(), KR)
            S.barrier()
        if stop_after in ('ret', 'retsmall'):
            return nc

        kern_d = scratch("kern", [D, 2 * L], BF16)
        ksp_d = scratch("ksp", [128, D, 2, 128], F32)
        u_d = scratch("u", [D, L], BF16)
        yconv_d = scratch("yconv", [D, L], F32)
        hyT_d = scratch("hyT", [D, L], BF16)
        invn_d = scratch("invn", [128, 8], F32)
        TWO_PI = 2.0 * math.pi
        MAGIC = 12582912.0
        with ExitStack() as ph:
            fw1 = sb(ph, "fw1", [33, 64]); fw2 = sb(ph, "fw2", [64, 64]); fw3 = sb(ph, "fw3", [64, 64])
            fw4 = sb(ph, "fw4", [64, 2 * D])
            fbc = sb(ph, "fbc", [64, 4]); fq2 = sb(ph, "fq2", [64, 1])
            dl = sb(ph, "dl", [128, 8]); nda = sb(ph, "nda", [128, 8])
            asum = sb(ph, "asum", [128, 8, 32]); invn = sb(ph, "invn", [128, 8]); atot = sb(ph, "atot", [128, 8])
            zb = [sb(ph, f"zb{i}", [33, 512]) for i in range(2)]
            lw = [sb(ph, f"lw{i}", [128, 512]) for i in range(2)]
            hu = [sb(ph, f"hu{i}", [64, 512]) for i in range(2)]; hk = [sb(ph, f"hk{i}", [64, 512]) for i in range(2)]
            hf = [sb(ph, f"hf{i}", [64, 512]) for i in range(2)]
            hh = [[sb(ph, f"hh{c}_{i}", [64, 512]) for i in range(3)] for c in range(2)]
            wn = [[sb(ph, f"wn{c}_{i}", [128, 512]) for i in range(2)] for c in range(2)]
            kf = [[sb(ph, f"kf{c}_{i}", [128, 512]) for i in range(2)] for c in range(2)]
            kbb = [[sb(ph, f"kbb{c}_{i}", [128, 512], BF16) for i in range(2)] for c in range(2)]
            pM = [ps(ph, f"pM{i}", [128, 512]) for i in range(2)]
            pG = [[ps(ph, f"pG{c}_{i}", [128, 512]) for i in range(2)] for c in range(2)]
            S.dma('sp', fw1[0:32, :], I['hy_fw1'][1:33, :], writes=['fw1'])
            S.dma('sp', fw1[32:33, :], I['hy_fw1'][0:1, :], writes=['fw1'])
            S.dma('sp', fw2[:], I['hy_fw2'], writes=['fw2'])
            S.dma('sp', fw3[:], I['hy_fw3'], writes=['fw3'])
            S.dma('sp', fw4[:], I['hy_fw4'], writes=['fw4'])
            for li, nme in enumerate(['hy_fb1', 'hy_fb2', 'hy_fb3', 'hy_sin_freq']):
                S.dma('sp', fbc[:, li:li + 1], I[nme].rearrange("o j -> j o"), writes=['fbc'], allow_slow_non_contiguous=True)
            S.dma('sp', dl[:], I['hy_deltas'].rearrange("o (g p) -> p (o g)", p=128), writes=['dl'], allow_slow_non_contiguous=True)
            S.op('dve', lambda e: e.tensor_scalar(fq2[:], fbc[:, 3:4], 1.0 / TWO_PI, None, ALU.mult), reads=['fbc'], writes=['fq2'])
            S.op('dve', lambda e: e.tensor_scalar(atot[:], dl[:], -1.0, None, ALU.mult), reads=['dl'], writes=['atot'])
            S.op('dve', lambda e: e.tensor_tensor(nda[:], dl[:], atot[:], ALU.min), reads=['dl', 'atot'], writes=['nda'])
            S.barrier()
            fws = [fw1, fw2, fw3]

            def fload(blk):
                cs = blk % 2
                nsl = slice(blk * 512, (blk + 1) * 512)
                S.dma('sp', zb[cs][:], C['zfeat'][:, nsl], writes=[f'zb{cs}'])
                S.dma('sp', lw[cs][:], C['lagw'][0:1, nsl].to_broadcast([128, 512]), writes=[f'lw{cs}'])

            def filt_chains():
                for blk in range(32):
                    cs = blk % 2
                    ops = []
                    A = ops.append
                    z, zn = zb[cs], f'zb{cs}'
                    lwt, lwn = lw[cs], f'lw{cs}'
                    nsl = slice(blk * 512, (blk + 1) * 512)
                    prev, prevn, kdim = z, zn, 33
                    pt, pn = pM[cs], f'pM{cs}'
                    HU, HK, HF = hu[cs], hk[cs], hf[cs]
                    for li in range(3):
                        A(lambda pt=pt, pn=pn, li=li, prev=prev, prevn=prevn, kdim=kdim: S.mm(
                            [lambda e: e.matmul(pt[0:64, :], fws[li][0:kdim, :], prev[0:kdim, :], start=True, stop=True)], reads=[prevn], writes=[pn]))
                        A(lambda pt=pt, pn=pn, li=li, HU=HU, cs=cs: S.op('dve', lambda e: e.tensor_scalar(HU[:], pt[0:64, :], fbc[:, li:li + 1], fq2[:, 0:1], ALU.add, ALU.mult),
                                                                   reads=[pn], writes=[f'hu{cs}']))
                        A(lambda HU=HU, HK=HK, cs=cs: S.op('dve', lambda e: e.tensor_scalar(HK[:], HU[:], MAGIC, MAGIC, ALU.add, ALU.subtract), reads=[f'hu{cs}'], writes=[f'hk{cs}']))
                        A(lambda HU=HU, HK=HK, HF=HF, cs=cs: S.op('dve', lambda e: e.tensor_tensor(HF[:], HU[:], HK[:], ALU.subtract), reads=[f'hu{cs}', f'hk{cs}'], writes=[f'hf{cs}']))
                        A(lambda HF=HF, li=li, cs=cs: S.op('act', lambda e: e.activation(hh[cs][li][:], HF[:], AF.Sin, scale=TWO_PI), reads=[f'hf{cs}'], writes=[f'hh{cs}_{li}']))
                        prev, prevn, kdim = hh[cs][li], f'hh{cs}_{li}', 64
                    dbase = 0 if blk < 16 else D
                    for g in range(8):
                        gi = g % 2
                        ptg, png = pG[cs][gi], f'pG{cs}_{gi}'
                        WN, KF, KB_ = wn[cs][gi], kf[cs][gi], kbb[cs][gi]
                        A(lambda ptg=ptg, png=png, g=g, cs=cs, dbase=dbase: S.mm(
                            [lambda e: e.matmul(ptg[:], fw4[:, dbase + g * 128:dbase + (g + 1) * 128], hh[cs][2][:], start=True, stop=True)],
                            reads=[f'hh{cs}_2'], writes=[png]))
                        A(lambda WN=WN, lwt=lwt, lwn=lwn, g=g, cs=cs, gi=gi: S.op('act', lambda e: e.activation(WN[:], lwt[:], AF.Exp, scale=nda[:, g:g + 1]),
                                                                                reads=[lwn], writes=[f'wn{cs}_{gi}']))
                        A(lambda KF=KF, WN=WN, ptg=ptg, png=png, cs=cs, gi=gi: S.op('dve', lambda e: e.tensor_tensor(KF[:], ptg[:], WN[:], ALU.mult),
                                                                                  reads=[png, f'wn{cs}_{gi}'], writes=[f'kf{cs}_{gi}']))
                        A(lambda KF=KF, g=g, blk=blk, cs=cs, gi=gi: S.op('dve', lambda e: e.tensor_reduce(asum[:, g, blk:blk + 1], KF[:], AX.X, ALU.add, apply_absolute_value=True),
                                                                       reads=[f'kf{cs}_{gi}'], writes=[f'asum{g}_{blk}']))
                        A(lambda KF=KF, KB_=KB_, cs=cs, gi=gi: copy_op('act', KB_[:], KF[:], [f'kf{cs}_{gi}'], [f'kbb{cs}_{gi}']))
                        A(lambda KB_=KB_, g=g, nsl=nsl, cs=cs, gi=gi: S.dma('sp', kern_d[g * 128:(g + 1) * 128, nsl], KB_[:], reads=[f'kbb{cs}_{gi}'], writes=['kern_d']))

                    def fin(blk=blk):
                        if blk + 2 < 32:
                            fload(blk + 2)
                    A(fin)
                    yield ops

            fload(0)
            fload(1)
            run_interleaved(filt_chains(), 2)
            S.op('dve', lambda e: e.tensor_reduce(atot[:], asum[:], AX.X, ALU.add), reads=[f'asum{g}_{b}' for g in range(8) for b in range(32)], writes=['atot'])
            S.op('dve', lambda e: e.tensor_scalar(atot[:], atot[:], EPS, None, ALU.add), reads=['atot'], writes=['atot'])
            S.op('dve', lambda e: e.reciprocal(invn[:], atot[:]), reads=['atot'], writes=['invn'])
            S.dma('sp', invn_d, invn[:], reads=['invn'], writes=['invn_d'])
            S.barrier()
        if stop_after == 'hfilt':
            return nc

        def conv3(e_out, zt, ztn, cw, cbb, s, g, outn):
            S.op('dve', lambda e: e.tensor_scalar(e_out[:], zt[:], cw[:, 1, s, g:g + 1], cbb[:, s, g:g + 1], ALU.mult, ALU.add),
                 reads=[ztn], writes=[outn])
            S.op('dve', lambda e: e.scalar_tensor_tensor(e_out[:, 1:L], zt[:, 0:L - 1], cw[:, 0, s, g:g + 1], e_out[:, 1:L], ALU.mult, ALU.add),
                 reads=[ztn, outn], writes=[outn])
            S.op('dve', lambda e: e.scalar_tensor_tensor(e_out[:, 0:L - 1], zt[:, 1:L], cw[:, 2, s, g:g + 1], e_out[:, 0:L - 1], ALU.mult, ALU.add),
                 reads=[ztn, outn], writes=[outn])

        with ExitStack() as ph:
            cw = sb(ph, "cw", [128, 3, 3, 8]); cbb = sb(ph, "cbb", [128, 3, 8])
            z1 = [sb(ph, f"z1{i}", [128, L], BF16) for i in range(2)]
            z2 = [sb(ph, f"z2{i}", [128, L], BF16) for i in range(2)]
            t1 = sb(ph, "t1", [128, L]); t2 = sb(ph, "t2", [128, L])
            ub = [sb(ph, f"ub{i}", [128, L], BF16) for i in range(2)]
            for k in range(3):
                S.dma('sp', cw[:, k, :, :], I['hy_conv_w'][k:k + 1, :].rearrange("o (s g p) -> p (o s) g", p=128, g=8),
                      writes=['cw'], allow_slow_non_contiguous=True)
            S.dma('sp', cbb[:], I['hy_conv_b'].rearrange("o (s g p) -> p (o s) g", p=128, g=8), writes=['cw'], allow_slow_non_contiguous=True)
            for g in range(8):
                a, an = z1[g % 2], f'z1{g % 2}'
                b_, bn = z2[g % 2], f'z2{g % 2}'
                S.dma('sp', a[:], zhy[D + g * 128:D + (g + 1) * 128, :], writes=[an])
                S.dma('sp', b_[:], zhy[2 * D + g * 128:2 * D + (g + 1) * 128, :], writes=[bn])
                conv3(t1, a, an, cw, cbb, 1, g, 't1')
                conv3(t2, b_, bn, cw, cbb, 2, g, 't2')
                S.op('dve', lambda e, g=g: e.tensor_tensor(ub[g % 2][:], t1[:], t2[:], ALU.mult), reads=['t1', 't2'], writes=[f'ub{g % 2}'])
                S.dma('sp', u_d[g * 128:(g + 1) * 128, :], ub[g % 2][:], reads=[f'ub{g % 2}'], writes=['u_d'])
            S.barrier()
        if stop_after == 'hu':
            return nc

        with ExitStack() as ph:
            fc1 = sb(ph, "fc1", [128, 256], BF16); fcj1 = sb(ph, "fcj1", [128, 256], BF16); fcj2 = sb(ph, "fcj2", [128, 256], BF16)
            fcj1n = sb(ph, "fcj1n", [128, 256], BF16)
            f3 = sb(ph, "f3", [128, 4, 128], BF16)
            tw = sb(ph, "tw", [128, 2, 128])
            src = [sb(ph, f"src{i}", [128, 16, 128], BF16) for i in range(2)]
            ksp = [sb(ph, f"ksp{i}", [128, 16, 2, 128]) for i in range(2)]
            yo = [sb(ph, f"yo{i}", [64, 16, 128]) for i in range(2)]
            KQ = 4
            p1t = [sb(ph, f"p1t{i}", [128, 2, 4, 128], BF16) for i in range(KQ)]
            p2t = [sb(ph, f"p2t{i}", [128, 2, 4, 128], BF16) for i in range(KQ)]
            m4 = [sb(ph, f"m4{i}", [128, 4, 4, 128]) for i in range(KQ)]
            Yt = [sb(ph, f"Yt{i}", [128, 2, 4, 128], BF16) for i in range(KQ)]
            q1t = [sb(ph, f"q1t{i}", [128, 2, 4, 128], BF16) for i in range(KQ)]
            q2t = [sb(ph, f"q2t{i}", [128, 2, 4, 128], BF16) for i in range(KQ)]
            pAC = [ps(ph, f"pAC{i}", [128, 4, 256]) for i in range(KQ)]
            pX = [pAC[i][:].rearrange("p c (r k) -> p r (c k)", r=2) if False else None for i in range(KQ)]
            S.dma('sp', fc1[:], C['fc1'], writes=['c']); S.dma('sp', fcj1[:], C['fcj1'], writes=['c'])
            S.dma('sp', fcj2[:], C['fcj2'], writes=['c']); S.dma('sp', fcj1n[:], C['fcj1n'], writes=['c'])
            S.dma('sp', f3[:], C['f3'].rearrange("p (a b) -> p a b", a=4), writes=['c'])
            S.dma('sp', tw[:], C['tw'].rearrange("p (a b) -> p a b", a=2), writes=['c'])
            S.barrier()
            Wre_b = tw[:, 0, :].unsqueeze(1).unsqueeze(1).to_broadcast([128, 4, 2, 128])
            Wim_b = tw[:, 1, :].unsqueeze(1).unsqueeze(1).to_broadcast([128, 4, 2, 128])
            FRE, FIM, NFIM, NFRE = 0, 1, 2, 3

            def twid_products(A, pin, pinn, P1, P1n, P2, P2n):
                pv = pin[:].rearrange("p c (r k) -> p c r k", r=2)
                A(lambda: S.op('dve', lambda e: e.tensor_tensor(P1[:].rearrange("p r c k -> p c r k"), pv, Wre_b, ALU.mult), reads=[pinn], writes=[P1n]))
                A(lambda: S.op('dve', lambda e: e.tensor_tensor(P2[:].rearrange("p r c k -> p c r k"), pv, Wim_b, ALU.mult), reads=[pinn], writes=[P2n]))

            def fl(t, r):
                return t[:, r, :, :].rearrange("p c k -> p (c k)")

            class _V:
                def __init__(self, t):
                    self.t = t

                def __getitem__(self, key):
                    return self.t[:].rearrange("p (r c) k -> p r (c k)", r=2)[key]

            def PXV(b):
                return _V(pAC[b])

            def fft_fwd(A, b, st, stn, q, kdim):
                pA, pAn = pAC[b], f'pAC{b}'
                pXb, pXn = PXV(b), f'pAC{b}'
                A(lambda: S.mm([(lambda e, ci=ci: e.matmul(pA[:, ci, :], st[0:kdim, q * 4 + ci, :], fc1[0:kdim, :], start=True, stop=True))
                                for ci in range(4)], reads=[stn], writes=[pAn]))
                P1, P2 = p1t[b], p2t[b]
                twid_products(A, pA, pAn, P1, f'p1t{b}', P2, f'p2t{b}')
                A(lambda: S.mm([lambda e: e.matmul(pXb[:, 0, :], f3[:, FRE, :], fl(P1, 0), start=True, stop=False),
                                lambda e: e.matmul(pXb[:, 0, :], f3[:, NFRE, :], fl(P2, 1), start=False, stop=False),
                                lambda e: e.matmul(pXb[:, 0, :], f3[:, NFIM, :], fl(P2, 0), start=False, stop=False),
                                lambda e: e.matmul(pXb[:, 0, :], f3[:, NFIM, :], fl(P1, 1), start=False, stop=True),
                                lambda e: e.matmul(pXb[:, 1, :], f3[:, FIM, :], fl(P1, 0), start=True, stop=False),
                                lambda e: e.matmul(pXb[:, 1, :], f3[:, NFIM, :], fl(P2, 1), start=False, stop=False),
                                lambda e: e.matmul(pXb[:, 1, :], f3[:, FRE, :], fl(P2, 0), start=False, stop=False),
                                lambda e: e.matmul(pXb[:, 1, :], f3[:, FRE, :], fl(P1, 1), start=False, stop=True)],
                               reads=[f'p1t{b}', f'p2t{b}'], writes=[pXn]))

            ngrp = 64 if stop_after != 'hsmall' else 2

            def kload(g):
                S.dma('sp', src[g % 2][:], kern_d[g * 16:(g + 1) * 16, :].rearrange("c (h l) -> h c l", l=128), writes=[f'src{g % 2}'])

            def cload(g):
                S.dma('sp', src[g % 2][0:64, :, :], u_d[g * 16:(g + 1) * 16, :].rearrange("c (h l) -> h c l", l=128), writes=[f'src{g % 2}'])
                S.dma('sp', ksp[g % 2][:], ksp_d[:, g * 16:(g + 1) * 16, :, :], writes=[f'ksp{g % 2}'])

            def ksp_chains():
                n = 0
                for gq in range(ngrp):
                    st, stn = src[gq % 2], f'src{gq % 2}'
                    kk_, kn = ksp[gq % 2], f'ksp{gq % 2}'
                    for q in range(4):
                        b = n % KQ
                        n += 1
                        ops = []
                        A = ops.append
                        fft_fwd(A, b, st, stn, q, 128)
                        pXb, pXn = PXV(b), f'pAC{b}'
                        A(lambda q=q, kk_=kk_, kn=kn, pXb=pXb, pXn=pXn: copy_op('act', kk_[:, q * 4:(q + 1) * 4, 0, :], pXb[:, 0, :].rearrange("p (c k) -> p c k", c=4), [pXn], [kn + f'_{q}r']))
                        A(lambda q=q, kk_=kk_, kn=kn, pXb=pXb, pXn=pXn: copy_op('act', kk_[:, q * 4:(q + 1) * 4, 1, :], pXb[:, 1, :].rearrange("p (c k) -> p c k", c=4), [pXn], [kn + f'_{q}i']))
                        def fin(gq=gq, kk_=kk_, kn=kn):
                            gdone[gq] = gdone.get(gq, 0) + 1
                            if gdone[gq] == 4:
                                S.dma('sp', ksp_d[:, gq * 16:(gq + 1) * 16, :, :], kk_[:],
                                      reads=[kn + f'_{q2}{x}' for q2 in range(4) for x in 'ri'], writes=['ksp_d'])
                                if gq + 2 < ngrp:
                                    kload(gq + 2)
                        A(fin)
                        yield ops

            gdone = {}
            kload(0)
            if ngrp > 1:
                kload(1)
            run_interleaved(ksp_chains(), KQ)
            S.barrier()
            if stop_after == 'hksp':
                return nc

            def conv_chains():
                n = 0
                for gq in range(ngrp):
                    st, stn = src[gq % 2], f'src{gq % 2}'
                    kk_, kn = ksp[gq % 2], f'ksp{gq % 2}'
                    yt, yn = yo[gq % 2], f'yo{gq % 2}'
                    for q in range(4):
                        b = n % KQ
                        n += 1
                        ops = []
                        A = ops.append
                        fft_fwd(A, b, st, stn, q, 64)
                        pXb, pXn = PXV(b), f'pAC{b}'
                        pC, pCn = pAC[b], f'pAC{b}'
                        M4, m4n = m4[b], f'm4{b}'
                        kq = kk_[:, q * 4:(q + 1) * 4, :, :]
                        Xre = pXb[:, 0, :].rearrange("p (c k) -> p c k", c=4)
                        Xim = pXb[:, 1, :].rearrange("p (c k) -> p c k", c=4)
                        A(lambda M4=M4, Xre=Xre, kq=kq, pXn=pXn, kn=kn, m4n=m4n: S.op('dve', lambda e: e.tensor_tensor(M4[:, 0], Xre, kq[:, :, 0, :], ALU.mult), reads=[pXn, kn], writes=[m4n + '0']))
                        A(lambda M4=M4, Xim=Xim, kq=kq, pXn=pXn, kn=kn, m4n=m4n: S.op('dve', lambda e: e.tensor_tensor(M4[:, 1], Xim, kq[:, :, 1, :], ALU.mult), reads=[pXn, kn], writes=[m4n + '1']))
                        A(lambda M4=M4, Xre=Xre, kq=kq, pXn=pXn, kn=kn, m4n=m4n: S.op('dve', lambda e: e.tensor_tensor(M4[:, 2], Xre, kq[:, :, 1, :], ALU.mult), reads=[pXn, kn], writes=[m4n + '2']))
                        A(lambda M4=M4, Xim=Xim, kq=kq, pXn=pXn, kn=kn, m4n=m4n: S.op('dve', lambda e: e.tensor_tensor(M4[:, 3], Xim, kq[:, :, 0, :], ALU.mult), reads=[pXn, kn], writes=[m4n + '3']))

                        Y, Yn = Yt[b], f'Yt{b}'
                        A(lambda M4=M4, Y=Y, Yn=Yn, m4n=m4n: S.op('pool', lambda e: e.tensor_tensor(Y[:, 0, :, :], M4[:, 0], M4[:, 1], ALU.subtract), reads=[m4n + '0', m4n + '1'], writes=[Yn + 'r']))
                        A(lambda M4=M4, Y=Y, Yn=Yn, m4n=m4n: S.op('pool', lambda e: e.tensor_tensor(Y[:, 1, :, :], M4[:, 2], M4[:, 3], ALU.add), reads=[m4n + '2', m4n + '3'], writes=[Yn + 'i']))

                        def step5(Y=Y, Yn=Yn, pC=pC, pCn=pCn):
                            fns = []
                            for ci in range(4):
                                fns.append(lambda e, ci=ci: e.matmul(pC[:, ci, :], Y[:, 0, ci, :], fcj1[:], start=True, stop=False))
                                fns.append(lambda e, ci=ci: e.matmul(pC[:, ci, :], Y[:, 1, ci, :], fcj2[:], start=False, stop=True))
                            S.mm(fns, reads=[Yn + 'r', Yn + 'i'], writes=[pCn])
                        A(step5)
                        Q1, Q2 = q1t[b], q2t[b]
                        twid_products(A, pC, pCn, Q1, f'q1t{b}', Q2, f'q2t{b}')
                        A(lambda pXb=pXb, pXn=pXn, Q1=Q1, Q2=Q2, b=b: S.mm(
                            [lambda e: e.matmul(pXb[0:64, 0, :], f3[:, FRE, 0:64], fl(Q1, 0), start=True, stop=False),
                             lambda e: e.matmul(pXb[0:64, 0, :], f3[:, FRE, 0:64], fl(Q2, 1), start=False, stop=False),
                             lambda e: e.matmul(pXb[0:64, 0, :], f3[:, FIM, 0:64], fl(Q1, 1), start=False, stop=False),
                             lambda e: e.matmul(pXb[0:64, 0, :], f3[:, NFIM, 0:64], fl(Q2, 0), start=False, stop=True)],
                            reads=[f'q1t{b}', f'q2t{b}'], writes=[pXn]))
                        A(lambda q=q, yt=yt, yn=yn, pXb=pXb, pXn=pXn: copy_op('act', yt[:, q * 4:(q + 1) * 4, :], pXb[0:64, 0, :].rearrange("p (c k) -> p c k", c=4),
                                                                            [pXn], [yn + f'_{q}'], scale=1.0 / 16384.0))
                        def fin(gq=gq, yt=yt, yn=yn):
                            gdone[gq] = gdone.get(gq, 0) + 1
                            if gdone[gq] == 4:
                                S.dma('sp', yconv_d[gq * 16:(gq + 1) * 16, :].rearrange("c (h l) -> h c l", l=128), yt[:],
                                      reads=[yn + f'_{q2}' for q2 in range(4)], writes=['yconv_d'])
                                if gq + 2 < ngrp:
                                    cload(gq + 2)
                        A(fin)
                        yield ops

            gdone = {}
            cload(0)
            if ngrp > 1:
                cload(1)
            run_interleaved(conv_chains(), KQ)
            S.barrier()
        if stop_after in ('hconv', 'hsmall'):
            return nc

        with ExitStack() as ph:
            cw = sb(ph, "cw", [128, 3, 3, 8]); cbb = sb(ph, "cbb", [128, 3, 8])
            hbias = sb(ph, "hbias", [128, 8]); invn = sb(ph, "invn", [128, 8])
            z0 = [sb(ph, f"z0{i}", [128, L], BF16) for i in range(2)]
            uu = [sb(ph, f"uu{i}", [128, L], BF16) for i in range(2)]
            yc = [sb(ph, f"yc{i}", [128, L]) for i in range(2)]
            t1 = sb(ph, "t1", [128, L])
            ho = [sb(ph, f"ho{i}", [128, L], BF16) for i in range(2)]
            for k in range(3):
                S.dma('sp', cw[:, k, :, :], I['hy_conv_w'][k:k + 1, :].rearrange("o (s g p) -> p (o s) g", p=128, g=8),
                      writes=['cw'], allow_slow_non_contiguous=True)
            S.dma('sp', cbb[:], I['hy_conv_b'].rearrange("o (s g p) -> p (o s) g", p=128, g=8), writes=['cw'], allow_slow_non_contiguous=True)
            S.dma('sp', hbias[:], I['hy_bias'].rearrange("o (g p) -> p (o g)", p=128), writes=['hbias'], allow_slow_non_contiguous=True)
            S.dma('sp', invn[:], invn_d, writes=['invn'])
            for g in range(8):
                gi = g % 2
                S.dma('sp', z0[gi][:], zhy[g * 128:(g + 1) * 128, :], writes=[f'z0{gi}'])
                S.dma('sp', uu[gi][:], u_d[g * 128:(g + 1) * 128, :], writes=[f'uu{gi}'])
                S.dma('sp', yc[gi][:], yconv_d[g * 128:(g + 1) * 128, :], writes=[f'yc{gi}'])
                conv3(t1, z0[gi], f'z0{gi}', cw, cbb, 0, g, 't1')
                S.op('dve', lambda e, g=g, gi=gi: e.tensor_scalar(yc[gi][:], yc[gi][:], invn[:, g:g + 1], None, ALU.mult),
                     reads=[f'yc{gi}', 'invn'], writes=[f'yc{gi}'])
                S.op('dve', lambda e, g=g, gi=gi: e.scalar_tensor_tensor(yc[gi][:], uu[gi][:], hbias[:, g:g + 1], yc[gi][:], ALU.mult, ALU.add),
                     reads=[f'yc{gi}', f'uu{gi}', 'hbias'], writes=[f'yc{gi}'])
                S.op('dve', lambda e, gi=gi: e.tensor_tensor(ho[gi][:], yc[gi][:], t1[:], ALU.mult), reads=[f'yc{gi}', 't1'], writes=[f'ho{gi}'])
                S.dma('sp', hyT_d[g * 128:(g + 1) * 128, :], ho[gi][:], reads=[f'ho{gi}'], writes=['hyT_d'])
            S.barrier()
        if stop_after == 'hy':
            return nc

        xl_d = scratch("xl", [L, D], F32)
        h2_d = scratch("h2", [L, D], F32)

        def load_cast_w(st, Wt, wname, src):
            for k in range(8):
                i = k % 2
                S.dma('sp', st[i][:], src[k * 128:(k + 1) * 128, :], writes=[f'wstg{i}'])
                copy_op(('act', 'dve')[k % 2], Wt[:, k, :], st[i][:], [f'wstg{i}'], [f'{wname}{k}'])

        def rms_rstd(r, rn, src, srcn, jk):
            S.op('act', lambda e: e.activation(jk[:], src[:], AF.Square, accum_out=r[:, 0:1]), reads=[srcn], writes=['jk', rn + 'a'])
            S.op('dve', lambda e: e.tensor_scalar(r[:, 1:2], r[:, 0:1], 1.0 / D, EPS, ALU.mult, ALU.add), reads=[rn + 'a'], writes=[rn + 'b'])
            S.op('act', lambda e: e.sqrt(r[:, 2:3], r[:, 1:2]), reads=[rn + 'b'], writes=[rn + 'c'])
            S.op('dve', lambda e: e.reciprocal(r[:, 3:4], r[:, 2:3]), reads=[rn + 'c'], writes=[rn + 'd'])

        with ExitStack() as pm_:
            Why = sb(pm_, "Why", [128, 8, D], BF16); Wret = sb(pm_, "Wret", [128, 8, D], BF16); Wo = sb(pm_, "Wo", [128, 8, D], BF16)
            G1 = sb(pm_, "G1", [128, D]); A2 = sb(pm_, "A2", [128, D]); SH2 = sb(pm_, "SH2", [128, D])
            wstg = [sb(pm_, f"wstg{i}", [128, D]) for i in range(2)]
            S.dma('sp', G1[:], mod_d[2:3, :].to_broadcast([128, D]), writes=['G1'])
            S.dma('sp', A2[:], mod_d[3:4, :].to_broadcast([128, D]), writes=['A2'])
            S.dma('sp', SH2[:], mod_d[4:5, :].to_broadcast([128, D]), writes=['SH2'])
            load_cast_w(wstg, Why, 'Why', I['w_hy_out'])
            load_cast_w(wstg, Wret, 'Wret', I['w_ret_out'])
            load_cast_w(wstg, Wo, 'Wo', I['w_o'])
            S.barrier()
            blkt = [[sb(pm_, f"blk{i}_{j}", [128, 8, 512], BF16) for j in range(4)] for i in range(2)]
            mT = sb(pm_, "mT", [128, 8, 512], BF16)
            ta = [sb(pm_, f"ta{i}", [128, 512]) for i in range(2)]
            tb = [sb(pm_, f"tb{i}", [128, 512]) for i in range(2)]
            xt = [sb(pm_, f"xt{i}", [128, D]) for i in range(4)]
            xl = [sb(pm_, f"xl{i}", [128, D]) for i in range(2)]
            h2t = [sb(pm_, f"h2t{i}", [128, D]) for i in range(2)]
            jk = sb(pm_, "jk", [128, D], BF16)
            rs = [sb(pm_, f"rs{i}", [128, 4]) for i in range(2)]
            pH = [ps(pm_, f"pH{i}", [128, 512]) for i in range(2)]
            pR = [ps(pm_, f"pR{i}", [128, 512]) for i in range(2)]
            pMx = [ps(pm_, f"pMx{i}", [128, 512]) for i in range(4)]
            srcs = [hyT_d, yrT_d, ahT_d, arT_d]
            nblk = NB if stop_after != 'mergesmall' else 2
            tcount = 0
            for b in range(nblk):
                tsl = slice(b * 512, (b + 1) * 512)
                bt = blkt[b % 2]
                bn = [f'blk{b % 2}_{j}' for j in range(4)]
                if b == 0:
                    for j in range(4):
                        S.dma('sp', bt[j][:], srcs[j].rearrange("(g f) t -> f g t", f=128)[:, :, tsl], writes=[bn[j]])
                if b + 1 < nblk:
                    tsl2 = slice((b + 1) * 512, (b + 2) * 512)
                    for j in range(4):
                        S.dma('sp', blkt[(b + 1) % 2][j][:], srcs[j].rearrange("(g f) t -> f g t", f=128)[:, :, tsl2],
                              writes=[f'blk{(b + 1) % 2}_{j}'])
                for ti in range(4):
                    S.dma('sp', xt[ti][:], I['x'][b * 512 + ti * 128:b * 512 + (ti + 1) * 128, :], writes=[f'xt{ti}'])
                for n_ in range(8):
                    i2 = n_ % 2
                    S.mm([(lambda e, k=k: e.matmul(pH[i2][:], Why[:, k, n_ * 128:(n_ + 1) * 128], bt[0][:, k, :], start=(k == 0), stop=(k == 7)))
                          for k in range(8)], reads=[bn[0]], writes=[f'pH{i2}'])
                    S.mm([(lambda e, k=k: e.matmul(pR[i2][:], Wret[:, k, n_ * 128:(n_ + 1) * 128], bt[1][:, k, :], start=(k == 0), stop=(k == 7)))
                          for k in range(8)], reads=[bn[1]], writes=[f'pR{i2}'])
                    S.op('dve', lambda e: e.tensor_tensor(ta[i2][:], pH[i2][:], bt[2][:, n_, :], ALU.mult), reads=[f'pH{i2}', bn[2]], writes=[f'ta{i2}'])
                    S.op('dve', lambda e: e.tensor_tensor(tb[i2][:], pR[i2][:], bt[3][:, n_, :], ALU.mult), reads=[f'pR{i2}', bn[3]], writes=[f'tb{i2}'])
                    S.op('dve', lambda e: e.tensor_tensor(mT[:, n_, :], ta[i2][:], tb[i2][:], ALU.add), reads=[f'ta{i2}', f'tb{i2}'], writes=[f'mT{n_}'])
                mreads = [f'mT{n_}' for n_ in range(8)]
                for ti in range(4):
                    r0 = b * 512 + ti * 128
                    i2 = tcount % 2
                    tcount += 1
                    for hf in range(2):
                        pp = pMx[(tcount * 2 + hf) % 4]
                        ppn = f'pMx{(tcount * 2 + hf) % 4}'
                        S.mm([(lambda e, k=k: e.matmul(pp[:], mT[:, k, ti * 128:(ti + 1) * 128], Wo[:, k, hf * 512:(hf + 1) * 512],
                                                       start=(k == 0), stop=(k == 7))) for k in range(8)], reads=mreads, writes=[ppn])
                        S.op('dve', lambda e: e.tensor_tensor(xl[i2][:, hf * 512:(hf + 1) * 512], pp[:], G1[:, hf * 512:(hf + 1) * 512], ALU.mult),
                             reads=[ppn], writes=[f'xl{i2}_{hf}'])
                    S.op('pool', lambda e: e.tensor_tensor(xl[i2][:], xl[i2][:], xt[ti][:], ALU.add),
                         reads=[f'xl{i2}_0', f'xl{i2}_1', f'xt{ti}'], writes=[f'xl{i2}'])
                    S.dma('sp', xl_d[r0:r0 + 128, :], xl[i2][:], reads=[f'xl{i2}'], writes=['xl_d'])
                    rms_rstd(rs[i2], f'rs{i2}', xl[i2], f'xl{i2}', jk)
                    S.op('dve', lambda e: e.scalar_tensor_tensor(h2t[i2][:], xl[i2][:], rs[i2][:, 3:4], A2[:], ALU.mult, ALU.mult),
                         reads=[f'xl{i2}', f'rs{i2}d'], writes=[f'h2t{i2}'])
                    S.op('pool', lambda e: e.tensor_tensor(h2t[i2][:], h2t[i2][:], SH2[:], ALU.add), reads=[f'h2t{i2}'], writes=[f'h2t{i2}'])
                    S.dma('sp', h2_d[r0:r0 + 128, :], h2t[i2][:], reads=[f'h2t{i2}'], writes=['h2_d'])
            S.barrier()
        if stop_after in ('merge', 'mergesmall'):
            return nc

        NEG = -1.0e30
        import os as _os
        ABL = _os.environ.get('PEER_ABL', '')
        with ExitStack() as pp_:
            Wq = sb(pp_, "Wq", [128, 8, D], BF16)
            KB = sb(pp_, "KB", [128, 8, 256])
            G2 = sb(pp_, "G2", [128, D]); FN = sb(pp_, "FN", [128, D])
            iota16 = sb(pp_, "iota16", [128, 16])
            S.dma('sp', G2[:], mod_d[5:6, :].to_broadcast([128, D]), writes=['G2'])
            S.dma('sp', FN[:], mod_d[6:7, :].to_broadcast([128, D]), writes=['FN'])
            S.dma('sp', iota16[:], C['iota16'], writes=['iota16'])
            with ExitStack() as pq_:
                wstg = [sb(pq_, f"wstg{i}", [128, D]) for i in range(2)]
                SK = sb(pq_, "SK", [128, 16, 64])
                pk = ps(pq_, "pk", [128, 128])
                load_cast_w(wstg, Wq, 'Wq', I['peer_w_query'])
                S.dma('sp', SK[:], I['peer_sub_keys'].rearrange("h c n d -> n (h c) d"), writes=['SK'])
                S.op('pool', lambda e: e.memset(KB[:], 0.0), writes=['KB'])
                for h in range(8):
                    S.mm([lambda e, h=h: e.transpose(pk[:], SK[:, 2 * h:2 * h + 2, :].rearrange("p a d -> p (a d)"), identf[:])],
                         reads=['SK', 'identf'], writes=['pk'])
                    copy_op('act', KB[0:64, h, 0:128], pk[0:64, :], ['pk'], ['KB'])
                    copy_op('act', KB[64:128, h, 128:256], pk[64:128, :], ['pk'], ['KB'])
                S.barrier()
            NG = 20
            gb = [sb(pp_, f"gb{i}", [128, 2 * D], BF16) for i in range(NG)]
            gl = sb(pp_, "gl", [128, 128])
            ND = 10
            Dg = [sb(pp_, f"Dg{i}", [128, 128], BF16) for i in range(ND)]
            h2 = [sb(pp_, f"h2_{i}", [128, D]) for i in range(2)]
            xlp = [sb(pp_, f"xlp{i}", [128, D]) for i in range(2)]
            h2b = sb(pp_, "h2b", [128, D], BF16)
            h2T = sb(pp_, "h2T", [128, 8, 128], BF16)
            qTs = sb(pp_, "qTs", [128, 4, 128])
            sall = sb(pp_, "sall", [128, 8, 2, 128])
            srep = sb(pp_, "srep", [128, 128])
            vv = sb(pp_, "vv", [128, 8, 2, 16]); iu = sb(pp_, "iu", [128, 8, 2, 16], U32); idf = sb(pp_, "idf", [128, 8, 2, 16])
            cand = sb(pp_, "cand", [128, 8, 16, 16]); crep = sb(pp_, "crep", [128, 256])
            tops = sb(pp_, "tops", [128, 8, 16]); pos = sb(pp_, "pos", [128, 8, 16], U32)
            pab = sb(pp_, "pab", [128, 2, 128], U32); pabf = sb(pp_, "pabf", [128, 2, 128])
            oh = sb(pp_, "oh", [128, 128, 16]); isel = sb(pp_, "isel", [128, 2, 128])
            eidf = sb(pp_, "eidf", [128, 128]); EIDX = [sb(pp_, f"EIDX{i}", [128, 128], I32) for i in range(2)]
            GATE = [sb(pp_, f"GATE{i}", [128, 8, 16]) for i in range(2)]
            ex = sb(pp_, "ex", [128, 8, 16]); sm = sb(pp_, "sm", [128, 2, 8])
            actp = sb(pp_, "actp", [128, 128]); coef = sb(pp_, "coef", [128, 128])
            jk2 = [sb(pp_, f"jk2{i}", [128, D], BF16) for i in range(4)]; jk3 = sb(pp_, "jk3", [128, D], BF16)
            xo = sb(pp_, "xo", [128, D]); ot = [sb(pp_, f"ot{i}", [128, D]) for i in range(2)]
            rs = sb(pp_, "rsp", [128, 4])
            pT2 = ps(pp_, "pT2", [128, 8, 128], BF16)
            pq = ps(pp_, "pq", [128, 4, 128])
            psc = ps(pp_, "psc", [128, 4, 256])
            pacc = ps(pp_, "pacc", [128, D])
            ntile = NT if stop_after != 'peersmall' else 1
            ntile = int(_os.environ.get('PEER_TILES', ntile))

            def front_ops(t):
                r0 = t * 128
                i2 = t % 2
                hh_, hn = h2[i2], f'h2_{i2}'
                EI, EIn = EIDX[i2], f'EIDX{i2}'
                GT, GTn = GATE[i2], f'GATE{i2}'
                ops = []
                A = ops.append
                A(lambda: S.dma('sp', hh_[:], h2_d[r0:r0 + 128, :], writes=[hn]))
                A(lambda: S.dma('sp', xlp[i2][:], xl_d[r0:r0 + 128, :], writes=[f'xlp{i2}']))
                A(lambda: copy_op('act', h2b[:], hh_[:], [hn], ['h2b']))
                A(lambda: S.mm([(lambda e, k=k: e.transpose(pT2[:, k, :], h2b[:, k * 128:(k + 1) * 128], ident[:])) for k in range(8)],
                               reads=['h2b', 'ident'], writes=['pT2']))
                A(lambda: copy_op('act', h2T[:], pT2[:], ['pT2'], ['h2T']))
                for hg in range(2):
                    def qproj(hg=hg):
                        fns = []
                        for hh in range(4):
                            h = hg * 4 + hh
                            for k in range(8):
                                fns.append(lambda e, h=h, hh=hh, k=k: e.matmul(pq[:, hh, :], Wq[:, k, h * 128:(h + 1) * 128], h2T[:, k, :],
                                                                             start=(k == 0), stop=(k == 7)))
                        S.mm(fns, reads=['h2T'], writes=['pq'])
                    A(qproj)
                    A(lambda: copy_op('act', qTs[:], pq[:], ['pq'], ['qTs']))
                    A(lambda hg=hg: S.mm([(lambda e, hh=hh: e.matmul(psc[:, hh, :], qTs[:, hh, :], KB[:, hg * 4 + hh, :], start=True, stop=True))
                                          for hh in range(4)], reads=['qTs'], writes=['psc']))
                    A(lambda hg=hg: copy_op('act', sall[:, hg * 4:(hg + 1) * 4, :, :].rearrange("p h c n -> p h (c n)"), psc[:], ['psc'], [f'sall{hg}']))
                for h in range(8):
                    sn = f'sall{h // 4}'
                    for c in range(2):
                        sc_ = sall[:, h, c, :]
                        A(lambda h=h, c=c, sc_=sc_, sn=sn: S.op('dve', lambda e: e.max(out=vv[:, h, c, 0:8], in_=sc_), reads=[sn], writes=[f'vv{h}{c}a']))
                        A(lambda h=h, c=c, sc_=sc_, sn=sn: S.op('dve', lambda e: e.match_replace(out=srep[:], in_to_replace=vv[:, h, c, 0:8], in_values=sc_, imm_value=NEG),
                                                                reads=[sn, f'vv{h}{c}a'], writes=['srep']))
                        A(lambda h=h, c=c: S.op('dve', lambda e: e.max(out=vv[:, h, c, 8:16], in_=srep[:]), reads=['srep'], writes=[f'vv{h}{c}b']))
                        A(lambda h=h, c=c, sc_=sc_, sn=sn: S.op('dve', lambda e: e.max_index(out=iu[:, h, c, 0:8], in_max=vv[:, h, c, 0:8], in_values=sc_),
                                                                reads=[sn, f'vv{h}{c}a'], writes=[f'iu{h}{c}a']))
                        A(lambda h=h, c=c: S.op('dve', lambda e: e.max_index(out=iu[:, h, c, 8:16], in_max=vv[:, h, c, 8:16], in_values=srep[:]),
                                                reads=['srep', f'vv{h}{c}b'], writes=[f'iu{h}{c}b']))
                vall = [f'vv{h}{c}{x}' for h in range(8) for c in range(2) for x in 'ab']
                iall = [f'iu{h}{c}{x}' for h in range(8) for c in range(2) for x in 'ab']
                A(lambda: S.op('dve', lambda e: e.tensor_copy(idf[:], iu[:]), reads=iall, writes=['idf']))
                A(lambda: S.op('dve', lambda e: e.tensor_tensor(cand[:], vv[:, :, 0, :].unsqueeze(3).to_broadcast([128, 8, 16, 16]),
                                                                vv[:, :, 1, :].unsqueeze(2).to_broadcast([128, 8, 16, 16]), ALU.add),
                               reads=vall, writes=['cand']))
                for h in range(8):
                    cf = cand[:, h, :, :].rearrange("p a b -> p (a b)")
                    A(lambda h=h, cf=cf: S.op('dve', lambda e: e.max(out=tops[:, h, 0:8], in_=cf), reads=['cand'], writes=[f'tops{h}a']))
                    A(lambda h=h, cf=cf: S.op('dve', lambda e: e.match_replace(out=crep[:], in_to_replace=tops[:, h, 0:8], in_values=cf, imm_value=NEG),
                                              reads=['cand', f'tops{h}a'], writes=['crep']))
                    A(lambda h=h: S.op('dve', lambda e: e.max(out=tops[:, h, 8:16], in_=crep[:]), reads=['crep'], writes=[f'tops{h}b']))
                    A(lambda h=h, cf=cf: S.op('dve', lambda e: e.max_index(out=pos[:, h, 0:8], in_max=tops[:, h, 0:8], in_values=cf),
                                              reads=['cand', f'tops{h}a'], writes=[f'pos{h}a']))
                    A(lambda h=h: S.op('dve', lambda e: e.max_index(out=pos[:, h, 8:16], in_max=tops[:, h, 8:16], in_values=crep[:]),
                                       reads=['crep', f'tops{h}b'], writes=[f'pos{h}b']))
                tall = [f'tops{h}{x}' for h in range(8) for x in 'ab']
                pall = [f'pos{h}{x}' for h in range(8) for x in 'ab']
                pf = pos[:].rearrange("p h k -> p (h k)")
                A(lambda: S.op('dve', lambda e: e.tensor_single_scalar(pab[:, 0, :], pf, 4, ALU.logical_shift_right), reads=pall, writes=['pab0']))
                A(lambda: S.op('dve', lambda e: e.tensor_single_scalar(pab[:, 1, :], pf, 15, ALU.bitwise_and), reads=pall, writes=['pab1']))
                A(lambda: S.op('dve', lambda e: e.tensor_copy(pabf[:], pab[:]), reads=['pab0', 'pab1'], writes=['pabf']))
                for ab in range(2):
                    A(lambda ab=ab: S.op('dve', lambda e: e.tensor_tensor(oh[:], pabf[:, ab, :].unsqueeze(2).to_broadcast([128, 128, 16]),
                                                                          iota16[:].unsqueeze(1).to_broadcast([128, 128, 16]), ALU.is_equal),
                                         reads=['pabf'], writes=['oh']))
                    A(lambda ab=ab: S.op('dve', lambda e: e.tensor_tensor(oh[:].rearrange("p (h k) a -> p h k a", h=8),
                                                                          oh[:].rearrange("p (h k) a -> p h k a", h=8),
                                                                          idf[:, :, ab, :].unsqueeze(2).to_broadcast([128, 8, 16, 16]), ALU.mult),
                                         reads=['oh', 'idf'], writes=['oh']))
                    A(lambda ab=ab: S.op('dve', lambda e: e.tensor_reduce(isel[:, ab, :], oh[:], AX.X, ALU.add), reads=['oh'], writes=[f'isel{ab}']))
                A(lambda: S.op('dve', lambda e: e.scalar_tensor_tensor(eidf[:], isel[:, 0, :], 128.0, isel[:, 1, :], ALU.mult, ALU.add),
                               reads=['isel0', 'isel1'], writes=['eidf']))
                A(lambda: S.op('dve', lambda e: e.tensor_copy(EI[:], eidf[:]), reads=['eidf'], writes=[EIn]))
                A(lambda: S.op('dve', lambda e: e.tensor_tensor(ex[:], tops[:], tops[:, :, 0:1].to_broadcast([128, 8, 16]), ALU.subtract),
                               reads=tall, writes=['ex']))
                A(lambda: S.op('act', lambda e: e.activation(ex[:], ex[:], AF.Exp), reads=['ex'], writes=['ex']))
                A(lambda: S.op('dve', lambda e: e.tensor_reduce(sm[:, 0, :], ex[:], AX.X, ALU.add), reads=['ex'], writes=['sm0']))
                A(lambda: S.op('dve', lambda e: e.reciprocal(sm[:, 1, :], sm[:, 0, :]), reads=['sm0'], writes=['sm1']))
                A(lambda: S.op('dve', lambda e: e.tensor_tensor(GT[:], ex[:], sm[:, 1, :].unsqueeze(2).to_broadcast([128, 8, 16]), ALU.mult),
                               reads=['ex', 'sm1'], writes=[GTn]))
                return ops

            pending = front_ops(0)
            for op_ in pending:
                op_()
            gcnt = 0
            dcnt = 0
            for t in range(ntile):
                r0 = t * 128
                i2 = t % 2
                hh_, hn = h2[i2], f'h2_{i2}'
                EI, EIn = EIDX[i2], f'EIDX{i2}'
                GT, GTn = GATE[i2], f'GATE{i2}'
                nxt = front_ops(t + 1) if t + 1 < ntile else []
                ni = 0
                per = (len(nxt) + 127) // 128 if nxt else 0
                GTf = GT[:].rearrange("p h k -> p (h k)")
                for j in range(128):
                    g_ = gcnt % NG
                    gcnt += 1
                    d_ = dcnt % ND
                    dcnt += 1
                    if 'nog' not in ABL:
                        S.raw_dma('pool', lambda e, g_=g_, j=j: e.indirect_dma_start(
                            out=gb[g_][:], out_offset=None, in_=uvb_d,
                            in_offset=bass.IndirectOffsetOnAxis(ap=EI[:, j:j + 1], axis=0)), reads=[EIn], writes=[f'gb{g_}'])
                    if 'nod' not in ABL:
                        S.op('dve', lambda e, g_=g_, j=j: e.scalar_tensor_tensor(jk2[j % 4][:], gb[g_][:, 0:D], 1.0, hh_[:], ALU.mult, ALU.mult,
                                                                                 accum_out=actp[:, j:j + 1]),
                             reads=[f'gb{g_}', hn], writes=[f'jk2{j % 4}', f'actp{j % 4}'])
                    S.op('act', lambda e, j=j: e.activation(gl[:, j:j + 1], actp[:, j:j + 1], AF.Gelu), reads=[f'actp{j % 4}'], writes=[f'gl{j % 4}'])
                    S.op('act', lambda e, j=j: e.mul(coef[:, j:j + 1], gl[:, j:j + 1], GTf[:, j:j + 1]), reads=[f'gl{j % 4}', GTn], writes=[f'coef{j % 4}'])
                    S.op('act', lambda e, d_=d_, j=j: e.mul(Dg[d_][:], ident[:], coef[:, j:j + 1]), reads=[f'coef{j % 4}'], writes=[f'Dg{d_}'])
                    S.mm([lambda e, d_=d_, g_=g_, j=j: e.matmul(pacc[:, 0:512], Dg[d_][:], gb[g_][:, D:D + 512], start=(j == 0), stop=(j == 127)),
                          lambda e, d_=d_, g_=g_, j=j: e.matmul(pacc[:, 512:1024], Dg[d_][:], gb[g_][:, D + 512:2 * D], start=(j == 0), stop=(j == 127))],
                         reads=[f'Dg{d_}', f'gb{g_}'], writes=['pacc'])
                    for _ in range(per):
                        if ni < len(nxt):
                            nxt[ni]()
                            ni += 1
                while ni < len(nxt):
                    nxt[ni]()
                    ni += 1
                S.op('dve', lambda e: e.tensor_tensor(xo[:], pacc[:], G2[:], ALU.mult), reads=['pacc'], writes=['xo'])
                S.op('pool', lambda e: e.tensor_tensor(xo[:], xo[:], xlp[i2][:], ALU.add), reads=['xo', f'xlp{i2}'], writes=['xo'])
                rms_rstd(rs, 'rsp', xo, 'xo', jk3)
                S.op('dve', lambda e: e.scalar_tensor_tensor(ot[i2][:], xo[:], rs[:, 3:4], FN[:], ALU.mult, ALU.mult),
                     reads=['xo', 'rspd'], writes=[f'ot{i2}'])
                S.dma('sp', out_ap[r0:r0 + 128, :], ot[i2][:], reads=[f'ot{i2}'], writes=['out'])
            S.barrier()

        S.barrier()
        print("ops", S.nops, "waits", S.nwaits, flush=True)
    return nc


def make_in_maps(inputs, consts):
    maps = []
    shared = {}
    for k in INPUT_NAMES:
        if k in ('x', 'c', 'ctx'):
            continue
        a = np.asarray(inputs[k], dtype=np.float32)
        if k in ('c_ctx', 'final_norm'):
            a = a.reshape(1, D)
        elif a.shape[0] == 1:
            a = a[0]
        a = a.reshape(PER_CORE_SHAPES[k])
        shared[k] = np.ascontiguousarray(a)
    for k, v in consts.items():
        shared["k_" + k] = v
    for b in range(NCORES):
        m = dict(shared)
        m['x'] = np.ascontiguousarray(inputs['x'][b])
        m['c'] = np.ascontiguousarray(inputs['c'][b:b + 1])
        m['ctx'] = np.ascontiguousarray(inputs['ctx'][b])
        maps.append(m)
    return maps


def kernel(**inputs):
    consts = make_consts()
    nc = build_nc(consts)
    maps = make_in_maps(inputs, consts)
    res = run_bass_kernel_spmd(nc, maps, core_ids=list(range(NCORES)))
    return np.stack([r["out"] for r in res.results], axis=0).astype(np.float32)
```
